# Optimizing a Trainium2 kernel written in Bass

```python
import math
import jax
import jax.numpy as jnp
from jax import lax
import numpy as np

D_MODEL = 1024
BATCH = 32
SEQ = 256
DEPTH = 1
DEC_BATCH = 4
DEC_SEQ = 4096
PAST_LEN = 512

GRID_W = 64
MIX_WIDTH = D_MODEL
S5_WIDTH = MIX_WIDTH // 2
S5_GROUP = 16
S5_GROUPS = S5_WIDTH // S5_GROUP
S5_STATE = 64
ATTN_WIDTH = MIX_WIDTH - S5_WIDTH
DIFF_HEAD_DIM = 64
VALUE_DIM = 2 * DIFF_HEAD_DIM
N_DIFF_HEADS = ATTN_WIDTH // VALUE_DIM
IN_WIDTH = S5_WIDTH + 3 * ATTN_WIDTH
ROT_PAIRS = DIFF_HEAD_DIM // 4
ROPE_THETA = 10000.0
Q_BLOCK = 128
N_EXPERTS = 64
TOP_K = 8
N_EXPERT_GROUPS = 8
TOPK_GROUPS = 4
EXPERT_FF = D_MODEL // 4
ROUTED_SCALE = 2.5
NORM_EPS = 1e-6

kernel_name = 'hymba_s5_diffattn_moe_dit_step'


def rmsnorm(x, w):
    x32 = x.astype(jnp.float32)
    y = x32 * lax.rsqrt(jnp.mean(x32 * x32, axis=-1, keepdims=True) + NORM_EPS)
    return (y * w.astype(jnp.float32)).astype(x.dtype)


def swiglu(t, w_gate, w_up, w_down):
    return (jax.nn.silu(t @ w_gate) * (t @ w_up)) @ w_down


def axial_rope_angles(n_tokens):
    rows = n_tokens // GRID_W
    row = jnp.repeat(jnp.arange(rows, dtype=jnp.float32), GRID_W)
    col = jnp.tile(jnp.arange(GRID_W, dtype=jnp.float32), rows)
    freqs = ROPE_THETA ** (-jnp.arange(ROT_PAIRS, dtype=jnp.float32) / ROT_PAIRS)
    return row[:, None] * freqs, col[:, None] * freqs


def rotate_pairs(x, ang):
    cos = jnp.cos(ang)[:, None, None, :].astype(x.dtype)
    sin = jnp.sin(ang)[:, None, None, :].astype(x.dtype)
    x1, x2 = x[..., :ROT_PAIRS], x[..., ROT_PAIRS:]
    return jnp.concatenate([x1 * cos - x2 * sin, x2 * cos + x1 * sin], axis=-1)


def axial_rope(x, ang_row, ang_col):
    half = DIFF_HEAD_DIM // 2
    return jnp.concatenate([rotate_pairs(x[..., :half], ang_row),
                            rotate_pairs(x[..., half:], ang_col)], axis=-1)


def ssm_combine(e1, e2):
    a1, b1 = e1
    a2, b2 = e2
    return a1 * a2, a2 * b1 + b2


def to_complex(re, im):
    return lax.complex(re.astype(jnp.float32), im.astype(jnp.float32))


def s5_direction(u_c, lam_re, lam_im, log_dt, b_re, b_im, c_re, c_im, h0, reverse):
    lam = to_complex(lam_re, lam_im)
    dt = jnp.exp(log_dt.astype(jnp.float32))[:, None]
    lam_bar = jnp.exp(lam * dt)
    b_bar = ((lam_bar - 1.0) / lam)[:, :, None] * to_complex(b_re, b_im)
    bu = jnp.einsum('blgh,gnh->blgn', u_c, b_bar)
    a = jnp.broadcast_to(lam_bar, bu.shape)
    a_cum, h = lax.associative_scan(ssm_combine, (a, bu), axis=1, reverse=reverse)
    if h0 is not None:
        h = h + a_cum * h0[:, None]
    y = jnp.einsum('blgn,ghn->blgh', h, to_complex(c_re, c_im)).real
    h_final = h[:, 0] if reverse else h[:, -1]
    return y, h_final


def s5_mixer(u, p, h0_re, h0_im):
    bsz, seq_len, _ = u.shape
    u32 = u.astype(jnp.float32)
    u_c = u32.reshape(bsz, seq_len, S5_GROUPS, S5_GROUP).astype(jnp.complex64)
    y = u32 * p['ssm_d'].astype(jnp.float32)
    finals = []
    for d, reverse in enumerate((False, True)):
        h0 = None if h0_re is None else to_complex(h0_re[:, d], h0_im[:, d])
        y_d, h_fin = s5_direction(u_c, p['ssm_lambda_re'][d], p['ssm_lambda_im'][d], p['ssm_log_dt'][d],
                                  p['ssm_b_re'][d], p['ssm_b_im'][d], p['ssm_c_re'][d], p['ssm_c_im'][d],
                                  h0, reverse)
        y = y + y_d.reshape(bsz, seq_len, S5_WIDTH)
        finals.append(h_fin)
    g = jax.nn.gelu(y)
    out = g * jax.nn.sigmoid(g @ p['ssm_w_glu'].astype(jnp.float32))
    h_fin = jnp.stack(finals, axis=1)
    return out.astype(u.dtype), h_fin.real, h_fin.imag


def diff_attn_block(q, k, v, lam):
    s = jnp.einsum('bqhmd,bkhmd->bhmqk', q, k, preferred_element_type=jnp.float32) * (DIFF_HEAD_DIM ** -0.5)
    pr = jax.nn.softmax(s, axis=-1)
    w = pr[:, :, 0] - lam * pr[:, :, 1]
    return jnp.einsum('bhqk,bkhe->bqhe', w.astype(v.dtype), v)


def mixing(h, p, lambda_init, ctx_k, ctx_v, h0_re, h0_im):
    bsz, seq_len, _ = h.shape
    proj = h @ p['w_in']
    u, q, k, v = jnp.split(proj, [S5_WIDTH, S5_WIDTH + ATTN_WIDTH, S5_WIDTH + 2 * ATTN_WIDTH], axis=-1)
    q = q.reshape(bsz, seq_len, N_DIFF_HEADS, 2, DIFF_HEAD_DIM)
    k = k.reshape(bsz, seq_len, N_DIFF_HEADS, 2, DIFF_HEAD_DIM)
    v = v.reshape(bsz, seq_len, N_DIFF_HEADS, VALUE_DIM)
    s5_out, h_re, h_im = s5_mixer(u, p, h0_re, h0_im)
    lq = p['diff_lambda_q'].astype(jnp.float32)
    lk = p['diff_lambda_k'].astype(jnp.float32)
    lam = jnp.exp(jnp.sum(lq[0] * lk[0])) - jnp.exp(jnp.sum(lq[1] * lk[1])) + lambda_init
    k_ctx_layout = k.reshape(bsz, seq_len, N_DIFF_HEADS, VALUE_DIM)
    if ctx_k is None:
        o = diff_attn_block(q, k, v, lam)
    else:
        ang_row, ang_col = axial_rope_angles(seq_len)
        q = axial_rope(q, ang_row, ang_col)
        k = axial_rope(k, ang_row, ang_col)
        ctx_len = ctx_k.shape[1]
        k_all = jnp.concatenate(
            [k, ctx_k.reshape(bsz, ctx_len, N_DIFF_HEADS, 2, DIFF_HEAD_DIM).astype(k.dtype)], axis=1)
        v_all = jnp.concatenate([v, ctx_v.astype(v.dtype)], axis=1)
        n_blocks = seq_len // Q_BLOCK
        q_blocks = jnp.moveaxis(q.reshape(bsz, n_blocks, Q_BLOCK, N_DIFF_HEADS, 2, DIFF_HEAD_DIM), 1, 0)
        o = lax.map(lambda qb: diff_attn_block(qb, k_all, v_all, lam), q_blocks)
        o = jnp.moveaxis(o, 0, 1).reshape(bsz, seq_len, N_DIFF_HEADS, VALUE_DIM)
    o = rmsnorm(o, p['diff_subln_w']) * (1.0 - lambda_init)
    mixed = jnp.concatenate([s5_out, o.reshape(bsz, seq_len, ATTN_WIDTH)], axis=-1) @ p['w_out']
    return mixed, (k_ctx_layout, v, h_re, h_im)


def moe(h, p):
    bsz, seq_len, dm = h.shape
    t = h.reshape(bsz * seq_len, dm)
    scores = jax.nn.sigmoid((t @ p['w_router']).astype(jnp.float32))
    biased = scores + p['router_bias'].astype(jnp.float32)
    grouped = biased.reshape(-1, N_EXPERT_GROUPS, N_EXPERTS // N_EXPERT_GROUPS)
    group_score = lax.top_k(grouped, 2)[0].sum(-1)
    _, top_groups = lax.top_k(group_score, TOPK_GROUPS)
    group_mask = jax.nn.one_hot(top_groups, N_EXPERT_GROUPS, dtype=jnp.float32).sum(-2)
    expert_mask = jnp.repeat(group_mask, N_EXPERTS // N_EXPERT_GROUPS, axis=-1)
    masked = jnp.where(expert_mask > 0, biased, -jnp.inf)
    _, top_idx = lax.top_k(masked, TOP_K)
    sel = jnp.take_along_axis(scores, top_idx, axis=-1)
    wts = sel / jnp.sum(sel, axis=-1, keepdims=True) * ROUTED_SCALE
    gates = jnp.einsum('nk,nke->ne', wts, jax.nn.one_hot(top_idx, N_EXPERTS, dtype=jnp.float32))

    def expert_step(acc, e):
        wg, wu, wd, g = e
        return acc + g[:, None].astype(t.dtype) * swiglu(t, wg, wu, wd), None

    routed, _ = lax.scan(expert_step, jnp.zeros_like(t),
                         (p['w_exp_gate'], p['w_exp_up'], p['w_exp_down'], gates.T))
    out = routed + swiglu(t, p['w_sh_gate'], p['w_sh_up'], p['w_sh_down'])
    return out.reshape(bsz, seq_len, dm)


def block(x, cond, p, lambda_init, ctx_k, ctx_v, h0_re, h0_im):
    mod = jax.nn.silu(cond) @ p['w_ada'] + p['b_ada']
    shift1, scale1, gate1, shift2, scale2, gate2 = jnp.split(mod[:, None, :], 6, axis=-1)
    h = rmsnorm(x, p['norm1_w']) * (1.0 + scale1) + shift1
    mixed, ctx_state = mixing(h, p, lambda_init, ctx_k, ctx_v, h0_re, h0_im)
    x = x + gate1 * mixed
    h = rmsnorm(x, p['norm2_w']) * (1.0 + scale2) + shift2
    x = x + gate2 * moe(h, p)
    return x, ctx_state


def setup_inputs(seed: int = 0) -> dict:
    key = jax.random.key(seed)
    ks = jax.random.split(key, 40)
    f32 = jnp.float32

    def nrm(k, shape, scale):
        return jax.random.normal(k, shape, f32) * scale

    lam_im_base = jnp.pi * jnp.arange(S5_STATE, dtype=f32)
    return {
        'x_prompt': nrm(ks[0], (BATCH, SEQ, D_MODEL), 1.0),
        'x_sample': nrm(ks[1], (DEC_BATCH, DEC_SEQ, D_MODEL), 1.0),
        'c': nrm(ks[2], (DEC_BATCH, D_MODEL), 1.0),
        'cache_k': nrm(ks[3], (DEC_BATCH, DEPTH, PAST_LEN, N_DIFF_HEADS, VALUE_DIM), 1.0),
        'cache_v': nrm(ks[4], (DEC_BATCH, DEPTH, PAST_LEN, N_DIFF_HEADS, VALUE_DIM), 1.0),
        'state_ssm_re': nrm(ks[5], (DEC_BATCH, DEPTH, 2, S5_GROUPS, S5_STATE), 0.5),
        'state_ssm_im': nrm(ks[6], (DEC_BATCH, DEPTH, 2, S5_GROUPS, S5_STATE), 0.5),
        'c_ctx': nrm(ks[7], (D_MODEL,), 1.0),
        'w_ada': nrm(ks[8], (DEPTH, D_MODEL, 6 * D_MODEL), 0.5 * D_MODEL ** -0.5),
        'b_ada': nrm(ks[9], (DEPTH, 6 * D_MODEL), 0.02),
        'norm1_w': 1.0 + nrm(ks[10], (DEPTH, D_MODEL), 0.02),
        'w_in': nrm(ks[11], (DEPTH, D_MODEL, IN_WIDTH), D_MODEL ** -0.5),
        'ssm_lambda_re': -0.5 + nrm(ks[12], (DEPTH, 2, S5_GROUPS, S5_STATE), 0.02),
        'ssm_lambda_im': lam_im_base + nrm(ks[13], (DEPTH, 2, S5_GROUPS, S5_STATE), 0.02),
        'ssm_log_dt': jax.random.uniform(ks[14], (DEPTH, 2, S5_GROUPS), f32, math.log(1e-3), math.log(1e-1)),
        'ssm_b_re': nrm(ks[15], (DEPTH, 2, S5_GROUPS, S5_STATE, S5_GROUP), S5_GROUP ** -0.5),
        'ssm_b_im': nrm(ks[16], (DEPTH, 2, S5_GROUPS, S5_STATE, S5_GROUP), S5_GROUP ** -0.5),
        'ssm_c_re': nrm(ks[17], (DEPTH, 2, S5_GROUPS, S5_GROUP, S5_STATE), S5_STATE ** -0.5),
        'ssm_c_im': nrm(ks[18], (DEPTH, 2, S5_GROUPS, S5_GROUP, S5_STATE), S5_STATE ** -0.5),
        'ssm_d': nrm(ks[19], (DEPTH, S5_WIDTH), 1.0),
        'ssm_w_glu': nrm(ks[20], (DEPTH, S5_WIDTH, S5_WIDTH), S5_WIDTH ** -0.5),
        'diff_lambda_q': nrm(ks[21], (DEPTH, 2, DIFF_HEAD_DIM), 0.1),
        'diff_lambda_k': nrm(ks[22], (DEPTH, 2, DIFF_HEAD_DIM), 0.1),
        'diff_subln_w': 1.0 + nrm(ks[23], (DEPTH, VALUE_DIM), 0.02),
        'w_out': nrm(ks[24], (DEPTH, MIX_WIDTH, D_MODEL), MIX_WIDTH ** -0.5),
        'norm2_w': 1.0 + nrm(ks[25], (DEPTH, D_MODEL), 0.02),
        'w_router': nrm(ks[26], (DEPTH, D_MODEL, N_EXPERTS), D_MODEL ** -0.5),
        'router_bias': nrm(ks[27], (DEPTH, N_EXPERTS), 0.01),
        'w_exp_gate': nrm(ks[28], (DEPTH, N_EXPERTS, D_MODEL, EXPERT_FF), D_MODEL ** -0.5),
        'w_exp_up': nrm(ks[29], (DEPTH, N_EXPERTS, D_MODEL, EXPERT_FF), D_MODEL ** -0.5),
        'w_exp_down': nrm(ks[30], (DEPTH, N_EXPERTS, EXPERT_FF, D_MODEL), EXPERT_FF ** -0.5),
        'w_sh_gate': nrm(ks[31], (DEPTH, D_MODEL, EXPERT_FF), D_MODEL ** -0.5),
        'w_sh_up': nrm(ks[32], (DEPTH, D_MODEL, EXPERT_FF), D_MODEL ** -0.5),
        'w_sh_down': nrm(ks[33], (DEPTH, EXPERT_FF, D_MODEL), EXPERT_FF ** -0.5),
        'final_norm_w': 1.0 + nrm(ks[34], (D_MODEL,), 0.02),
    }


def reference(x_prompt, x_sample, c, cache_k, cache_v, state_ssm_re, state_ssm_im,
              c_ctx, w_ada, b_ada, norm1_w, w_in, ssm_lambda_re, ssm_lambda_im, ssm_log_dt,
              ssm_b_re, ssm_b_im, ssm_c_re, ssm_c_im, ssm_d, ssm_w_glu,
              diff_lambda_q, diff_lambda_k, diff_subln_w, w_out, norm2_w,
              w_router, router_bias, w_exp_gate, w_exp_up, w_exp_down,
              w_sh_gate, w_sh_up, w_sh_down, final_norm_w):
    xp, xs = x_prompt, x_sample
    ks, vs, hres, hims = [], [], [], []
    for l in range(DEPTH):
        p = {
            'w_ada': w_ada[l], 'b_ada': b_ada[l], 'norm1_w': norm1_w[l], 'w_in': w_in[l],
            'ssm_lambda_re': ssm_lambda_re[l], 'ssm_lambda_im': ssm_lambda_im[l], 'ssm_log_dt': ssm_log_dt[l],
            'ssm_b_re': ssm_b_re[l], 'ssm_b_im': ssm_b_im[l], 'ssm_c_re': ssm_c_re[l], 'ssm_c_im': ssm_c_im[l],
            'ssm_d': ssm_d[l], 'ssm_w_glu': ssm_w_glu[l],
            'diff_lambda_q': diff_lambda_q[l], 'diff_lambda_k': diff_lambda_k[l], 'diff_subln_w': diff_subln_w[l],
            'w_out': w_out[l], 'norm2_w': norm2_w[l], 'w_router': w_router[l], 'router_bias': router_bias[l],
            'w_exp_gate': w_exp_gate[l], 'w_exp_up': w_exp_up[l], 'w_exp_down': w_exp_down[l],
            'w_sh_gate': w_sh_gate[l], 'w_sh_up': w_sh_up[l], 'w_sh_down': w_sh_down[l],
        }
        lambda_init = 0.8 - 0.6 * math.exp(-0.3 * l)
        xp, (k_new, v_new, h_re, h_im) = block(xp, c_ctx[None, :], p, lambda_init, None, None, None, None)
        xs, _ = block(xs, c, p, lambda_init, cache_k[:, l], cache_v[:, l], state_ssm_re[:, l], state_ssm_im[:, l])
        ks.append(k_new)
        vs.append(v_new)
        hres.append(h_re)
        hims.append(h_im)
    y_prompt = rmsnorm(xp, final_norm_w)
    y_sample = rmsnorm(xs, final_norm_w)
    new_cache_k = jnp.stack(ks, axis=1)
    new_cache_v = jnp.stack(vs, axis=1)
    new_state_ssm_re = jnp.stack(hres, axis=1)
    new_state_ssm_im = jnp.stack(hims, axis=1)
    return (y_prompt, y_sample, new_cache_k, new_cache_v, new_state_ssm_re, new_state_ssm_im)
```

```python
import contextlib
import os
STAGE = int(os.environ.get('KSTAGE', '9'))
import numpy as np
import ml_dtypes
import concourse.bass as bass
import concourse.mybir as mybir
from concourse.bass_utils import run_bass_kernel_spmd

F32 = mybir.dt.float32
BF16 = mybir.dt.bfloat16
ALU = mybir.AluOpType
AF = mybir.ActivationFunctionType
AX = mybir.AxisListType

NCORES = 8
D = 1024
KC = 8
NP_TOK = 1024
NS_TOK = 2048
NT_MOE = int(os.environ.get("NT_MOE", "24"))
NE_MOE = int(os.environ.get("NE_MOE", "65"))
EPS = 1e-6


class Tk:
    __slots__ = ("sem", "val", "eng")

    def __init__(self, sem, val, eng):
        self.sem, self.val, self.eng = sem, val, eng


class Buf:
    __slots__ = ("name", "w", "r")

    def __init__(self, name=""):
        self.name, self.w, self.r = name, None, {}


class Sched:
    def __init__(self, nc, stack):
        self.nc = nc
        self.engs = {"pe": nc.tensor, "act": nc.scalar, "dve": nc.vector,
                     "pool": nc.gpsimd, "sp": nc.sync}
        self.sem = {}
        self.cnt = {}
        for k in ("pe", "act", "dve", "pool"):
            self.sem[k] = stack.enter_context(nc.semaphore("c_" + k))
            self.cnt[k] = 0
        self.seen = {k: {} for k in self.engs}
        self.dsem = {}
        self.dcnt = {}
        self.bsem = {}
        self.free = []
        self.stack = stack

    def _wait(self, e, tk):
        if tk is None:
            return
        if tk.eng == "pe" and e == "pe":
            return
        key = tk.sem.num
        if self.seen[e].get(key, 0) >= tk.val:
            return
        self.engs[e].wait_ge(tk.sem, tk.val)
        self.seen[e][key] = tk.val

    def _deps(self, e, reads, writes):
        need = {}

        def add(tk):
            if tk is None or (tk.eng == "pe" and e == "pe"):
                return
            k = tk.sem.num
            if k not in need or need[k].val < tk.val:
                need[k] = tk
        for b in reads:
            add(b.w)
        for b in writes:
            add(b.w)
            for t in b.r.values():
                add(t)
        for tk in need.values():
            self._wait(e, tk)

    def _mark(self, tk, reads, writes):
        for b in reads:
            b.r[tk.sem.num] = tk
        for b in writes:
            b.w = tk
            b.r = {}

    dead = False
    nops = 0
    KCUT = int(os.environ.get('KCUT', '100000000'))

    def op(self, e, fn, reads=(), writes=()):
        Sched.nops += 1
        if self.dead or Sched.nops > Sched.KCUT:
            return
        self._deps(e, reads, writes)
        ins = fn(self.engs[e])
        self.cnt[e] += 1
        ins.then_inc(self.sem[e], 1)
        self._mark(Tk(self.sem[e], self.cnt[e], e), reads, writes)

    def dma(self, q, out, in_, reads=(), writes=(), stream="ld", **kw):
        Sched.nops += 1
        if self.dead or Sched.nops > Sched.KCUT:
            return
        key = id(writes[0]) if writes else id(reads[0])
        if key not in self.bsem:
            if self.free:
                num = self.free.pop()
            else:
                h = self.stack.enter_context(self.nc.semaphore("d%d" % len(self.dsem)))
                num = h.num
                self.dsem[num] = h
                self.dcnt[num] = 0
            self.bsem[key] = num
        num = self.bsem[key]
        self._deps(q, reads, writes)
        ins = self.engs[q].dma_start(out=out, in_=in_, **kw)
        self.dcnt[num] += 16
        ins.then_inc(self.dsem[num], 16)
        self._mark(Tk(self.dsem[num], self.dcnt[num], "dma"), reads, writes)

    def barrier(self):
        es = ("pe", "act", "dve", "pool", "sp")
        for e in es:
            for k in ("pe", "act", "dve", "pool"):
                if k != e and self.cnt[k]:
                    self._wait(e, Tk(self.sem[k], self.cnt[k], k))
            for s_, sem in self.dsem.items():
                if self.dcnt[s_]:
                    self._wait(e, Tk(sem, self.dcnt[s_], "dma"))
        self.free = list(self.dsem.keys())
        self.bsem = {}

    def finish(self):
        for s, sem in self.dsem.items():
            if self.dcnt[s]:
                self.engs["sp"].wait_ge(sem, self.dcnt[s])


def build_program():
    nc = bass.Bass("TRN2", target_bir_lowering=False)
    dt = nc.dram_tensor

    def din(name, shape, dtype=F32):
        return dt(name, list(shape), dtype, kind="ExternalInput").ap()

    def dout(name, shape, dtype=F32):
        return dt(name, list(shape), dtype, kind="ExternalOutput").ap()

    xp = din("xp", [NP_TOK, D])
    condT = din("condT", [128, KC, 2])
    w_ada = din("w_ada", [D, 6 * D])
    b_ada_bc = din("b_ada_bc", [128, 6 * D])
    n1w = din("n1w", [128, KC])
    w_in = din("w_in", [D, 2048])
    ident_in = din("ident", [128, 128])
    w_out_d = din("w_out", [D, D])
    w_glu_d = din("w_glu", [512, 512])
    lam_re_l = din("lam_re_l", [128, 32]); lam_im_l = din("lam_im_l", [128, 32]); logdt_l = din("logdt_l", [128, 32])
    bre_l = din("bre_l", [128, 32, 16]); bim_l = din("bim_l", [128, 32, 16])
    cre_l = din("cre_l", [128, 32, 16]); cim_l = din("cim_l", [128, 32, 16])
    dfold_l = din("dfold_l", [128, 32]); mle_l = din("mle_l", [128, 128]); mge_l = din("mge_l", [128, 128])
    xs_all = din("xs_all", [4096, D]); pos_rc = din("pos_rc", [128, 32, 2]); fidx_bc = din("fidx_bc", [128, 16])
    ck_in = din("ck_in", [512, 512]); cv_in = din("cv_in", [512, 512]); h0_l = din("h0_l", [128, 2, 32])
    n2w = din("n2w", [128, KC]); w_router_d = din("w_router", [D, 64]); rbias_bc = din("rbias_bc", [128, 64])
    fw_bc = din("fw_bc", [128, D])
    weg = din("weg", [65, D, 256]); weu = din("weu", [65, D, 256]); wed = din("wed", [65, 256, D])
    lq_bc = din("lq_bc", [128, 2, 64]); lk_bc = din("lk_bc", [128, 2, 64]); subw_bc = din("subw_bc", [128, 128])

    o_ck = dout("o_ck", [NP_TOK, 512])
    o_cv = dout("o_cv", [NP_TOK, 512])
    o_st = dout("o_st", [256, 128])
    o_y = dout("o_y", [NP_TOK + NS_TOK, D])
    x1_d = dt("x1_scratch", [NP_TOK + NS_TOK, D], F32, kind="Internal").ap()

    with contextlib.ExitStack() as st:
        S = Sched(nc, st)

        cur = [st]

        def sb(name, shape, dtype=F32):
            return cur[0].enter_context(nc.sbuf_tensor(name, list(shape), dtype))

        _stk = []

        def scope_begin():
            _stk.append(cur[0])
            cur[0] = contextlib.ExitStack()

        def scope_end():
            S.barrier()
            cur[0].close()
            cur[0] = _stk.pop()

        def psum(name, shape, dtype=F32):
            return st.enter_context(nc.psum_tensor(name, list(shape), dtype))

        banks = [psum(f"bank{i}", [128, 512], F32) for i in range(8)]
        bbuf = [Buf(f"bank{i}") for i in range(8)]

        ident_f = sb("ident_f", [128, 128]); b_ident_f = Buf()
        ident_b = sb("ident_b", [128, 128], BF16); b_ident_b = Buf()
        S.dma("sp", ident_f[:], ident_in, writes=[b_ident_f], stream="c")
        S.op("dve", lambda e: e.tensor_copy(out=ident_b[:], in_=ident_f[:]),
             reads=[b_ident_f], writes=[b_ident_b])

        eps_t = sb("eps_t", [128, 1]); b_eps = Buf()
        S.op("dve", lambda e: e.memset(eps_t[:], EPS), writes=[b_eps])
        n1w_sb = sb("n1w_sb", [128, KC]); b_n1w = Buf()
        S.dma("sp", n1w_sb[:], n1w, writes=[b_n1w], stream="c")

        w_in_v = w_in.rearrange("(kc p) n -> p kc n", p=128)

        gate_bc = sb("gate_bc", [128, 2, 2, D]); b_gate = Buf()
        modT = sb("modT", [128, 4, KC, 2]); b_modT = Buf()
        sc1 = sb("sc1", [128, KC, 2]); b_sc1 = Buf()
        scope_begin()
        cT = sb("cT", [128, KC, 2]); b_cT = Buf()
        S.dma("sp", cT[:], condT, writes=[b_cT], stream="c")
        sil = sb("sil", [128, KC, 2]); b_sil = Buf()
        S.op("act", lambda e: e.activation(out=sil[:], in_=cT[:], func=AF.Silu),
             reads=[b_cT], writes=[b_sil])
        silrep = sb("silrep", [128, 2, KC, 128], BF16); b_silrep = Buf()
        for r in range(2):
            S.op("dve", lambda e, r=r: e.tensor_copy(
                out=silrep[:, r, :, :],
                in_=sil[:, :, r:r + 1].to_broadcast([128, KC, 128])),
                reads=[b_sil], writes=[b_silrep])

        wada_sb = [sb(f"wada{i}", [128, KC, 512], BF16) for i in range(2)]
        b_wada = [Buf(), Buf()]
        bada_sb = [sb(f"bada{i}", [128, 512]) for i in range(2)]
        b_bada = [Buf(), Buf()]
        w_ada_v = w_ada.rearrange("(kc p) n -> p kc n", p=128)
        modrow = sb("modrow", [128, 512]); b_modrow = Buf()
        vec_of_chunk = {0: 0, 1: 1, 3: 2, 4: 3}
        gate_of_chunk = {2: 0, 5: 1}
        for cb in range(12):
            chunk, hf = cb // 2, cb % 2
            wb = cb % 2
            S.dma("pool", wada_sb[wb][:], w_ada_v[:, :, cb * 512:(cb + 1) * 512],
                  writes=[b_wada[wb]], stream="w")
            S.dma("sp", bada_sb[wb][:], b_ada_bc[:, cb * 512:(cb + 1) * 512],
                  writes=[b_bada[wb]], stream="c")
            for r in range(2):
                pb = (cb * 2 + r) % 2
                def mm(e, r=r, wb=wb, pb=pb):
                    for kc in range(KC):
                        ins = e.matmul(banks[pb][:, :], lhsT=silrep[:, r, kc, :],
                                       rhs=wada_sb[wb][:, kc, :],
                                       start=(kc == 0), stop=(kc == KC - 1))
                    return ins
                S.op("pe", mm, reads=[b_silrep, b_wada[wb]], writes=[bbuf[pb]])
                if chunk in gate_of_chunk:
                    g = gate_of_chunk[chunk]
                    S.op("dve", lambda e, r=r, g=g, hf=hf, pb=pb, wb=wb: e.tensor_tensor(
                        out=gate_bc[:, r, g, hf * 512:(hf + 1) * 512],
                        in0=banks[pb][:, :], in1=bada_sb[wb][:], op=ALU.add),
                        reads=[bbuf[pb], b_bada[wb]], writes=[b_gate])
                else:
                    v = vec_of_chunk[chunk]
                    S.op("dve", lambda e, pb=pb, wb=wb: e.tensor_tensor(
                        out=modrow[:], in0=banks[pb][:, :], in1=bada_sb[wb][:], op=ALU.add),
                        reads=[bbuf[pb], b_bada[wb]], writes=[b_modrow])
                    tb = 2 + (cb * 2 + r) % 2
                    def tp(e, tb=tb):
                        for j in range(4):
                            ins = e.transpose(out=banks[tb][:, j * 128:(j + 1) * 128],
                                              in_=modrow[:, j * 128:(j + 1) * 128],
                                              identity=ident_f[:])
                        return ins
                    S.op("pe", tp, reads=[b_modrow, b_ident_f], writes=[bbuf[tb]])
                    S.op("dve", lambda e, tb=tb, v=v, hf=hf, r=r: e.tensor_copy(
                        out=modT[:, v, hf * 4:(hf + 1) * 4, r],
                        in_=banks[tb][:, :].rearrange("p (j m) -> p j m", m=128)[:, :, 0]),
                        reads=[bbuf[tb]], writes=[b_modT])
        scope_end()
        n2w_sb = sb("n2w_sb", [128, KC]); b_n2w = Buf()
        S.dma("sp", n2w_sb[:], n2w, writes=[b_n2w], stream="c")
        sc2 = sb("sc2", [128, KC, 2]); b_sc2 = Buf()
        S.op("dve", lambda e: e.tensor_scalar(out=sc2[:], in0=modT[:, 3, :, :], scalar1=1.0,
                                              scalar2=None, op0=ALU.add),
             reads=[b_modT], writes=[b_sc2])
        S.op("dve", lambda e: e.tensor_tensor(out=sc2[:], in0=sc2[:],
                                              in1=n2w_sb[:, :].unsqueeze(2).to_broadcast([128, KC, 2]),
                                              op=ALU.mult),
             reads=[b_sc2, b_n2w], writes=[b_sc2])
        S.op("dve", lambda e: e.tensor_scalar(out=sc1[:], in0=modT[:, 1, :, :], scalar1=1.0,
                                              scalar2=None, op0=ALU.add),
             reads=[b_modT], writes=[b_sc1])
        S.op("dve", lambda e: e.tensor_tensor(out=sc1[:], in0=sc1[:],
                                              in1=n1w_sb[:, :].unsqueeze(2).to_broadcast([128, KC, 2]),
                                              op=ALU.mult),
             reads=[b_sc1, b_n1w], writes=[b_sc1])

        class TT:
            def __init__(s, ap, buf):
                s.ap, s.buf = ap, buf
            def __getitem__(s, k):
                return TT(s.ap[k], s.buf)
            def v(s, fn):
                return TT(fn(s.ap), s.buf)

        _cnt = [0]
        def newT(shape, dtype=F32, name=None):
            _cnt[0] += 1
            t = sb(f"{name or 't'}_{_cnt[0]}", shape, dtype)
            return TT(t[tuple(slice(None) for _ in shape)], Buf())

        def _rb(*xs):
            return [x.buf for x in xs if isinstance(x, TT)]
        def _a(x):
            return x.ap if isinstance(x, TT) else x
        def tt(o, a, b, op, eng="dve"):
            S.op(eng, lambda e: e.tensor_tensor(out=o.ap, in0=a.ap, in1=b.ap, op=op),
                 reads=_rb(a, b), writes=[o.buf])
        def ts(o, a, s1, s2, op0, op1=None, eng="dve"):
            if op1 is None:
                S.op(eng, lambda e: e.tensor_scalar(out=o.ap, in0=a.ap, scalar1=_a(s1), scalar2=None, op0=op0),
                     reads=_rb(a, s1), writes=[o.buf])
            else:
                S.op(eng, lambda e: e.tensor_scalar(out=o.ap, in0=a.ap, scalar1=_a(s1), scalar2=_a(s2),
                                                    op0=op0, op1=op1),
                     reads=_rb(a, s1, s2), writes=[o.buf])
        def stt(o, a, sc, b, op0, op1, eng="dve"):
            S.op(eng, lambda e: e.scalar_tensor_tensor(out=o.ap, in0=a.ap, scalar=_a(sc), in1=b.ap, op0=op0, op1=op1),
                 reads=_rb(a, sc, b), writes=[o.buf])
        def cp(o, a, eng="dve"):
            if eng == "act":
                S.op("act", lambda e: e.copy(out=o.ap, in_=a.ap), reads=[a.buf], writes=[o.buf])
            else:
                S.op(eng, lambda e: e.tensor_copy(out=o.ap, in_=a.ap), reads=[a.buf], writes=[o.buf])
        def act(o, a, func, scale=1.0, bias=None, accum=None):
            kw = {}
            if bias is not None:
                kw["bias"] = _a(bias)
            if accum is not None:
                kw["accum_out"] = accum.ap
            S.op("act", lambda e: e.activation(out=o.ap, in_=a.ap, func=func, scale=_a(scale), **kw),
                 reads=_rb(a, scale, bias), writes=[o.buf] + ([accum.buf] if accum is not None else []))
        def memset(o, val, eng="dve"):
            S.op(eng, lambda e: e.memset(o.ap, val), writes=[o.buf])
        def recip(o, a):
            S.op("dve", lambda e: e.reciprocal(out=o.ap, in_=a.ap), reads=[a.buf], writes=[o.buf])
        def mmg(o, pairs):
            def f(e):
                n = len(pairs)
                for i, (l, r) in enumerate(pairs):
                    ins = e.matmul(o.ap, lhsT=l.ap, rhs=r.ap, start=(i == 0), stop=(i == n - 1))
                return ins
            rd = []
            for l, r in pairs:
                rd += [l.buf, r.buf]
            S.op("pe", f, reads=rd, writes=[o.buf])
        def tpose(o, a, idt):
            S.op("pe", lambda e: e.transpose(out=o.ap, in_=a.ap, identity=idt.ap),
                 reads=[a.buf, idt.buf], writes=[o.buf])
        def load(dram_ap, shape, dtype=F32, q="sp", stream="c"):
            t = newT(shape, dtype)
            S.dma(q, t.ap, dram_ap, writes=[t.buf], stream=stream)
            return t
        _bk = [0]
        def nbank():
            _bk[0] = (_bk[0] + 1) % 8
            i = _bk[0]
            return TT(banks[i][:, :], bbuf[i])
        def bf(bank):
            return TT(bank.ap.bitcast(BF16), bank.buf)
        _ae = [0]
        def evac_eng():
            _ae[0] ^= 1
            return "act" if _ae[0] else "dve"

        identF = TT(ident_f[:], b_ident_f); identB = TT(ident_b[:], b_ident_b)
        epsT = TT(eps_t[:], b_eps)
        sc2T = TT(sc2[:], b_sc2)
        sc1T = TT(sc1[:], b_sc1); modTT = TT(modT[:], b_modT); gateT = TT(gate_bc[:], b_gate)
        def load_win():
            w = newT([128, KC, 2048], BF16, "w_in_sb")
            for kc in range(KC):
                S.dma("pool", w.ap[:, kc, :], w_in_v[:, kc, :], writes=[w.buf], stream="w")
            return w

        scope_begin()
        wout_v = w_out_d.rearrange("(kc p) n -> p kc n", p=128)
        wglu_sb = newT([128, 4, 512], BF16, "wglu_sb")
        S.dma("pool", wglu_sb.ap, w_glu_d.rearrange("(kc p) n -> p kc n", p=128), writes=[wglu_sb.buf], stream="w")

        def g128(x, g):
            return x.v(lambda a: a[:, g, :, :].rearrange("p j h -> p (j h)"))

        def build_s5_consts():
            Qr = newT([128, 32, 8, 16], BF16, "Qr"); QiN = newT([128, 32, 8, 16], BF16, "QiN")
            Mt = newT([128, 32, 128], BF16, "Mt")
            PT = newT([128, 32, 2, 128], BF16, "PT")
            A1 = newT([128, 2, 32]); A2 = newT([128, 2, 32])
            scope_begin()
            lamre = load(lam_re_l, [128, 32]); lamim = load(lam_im_l, [128, 32]); logdt = load(logdt_l, [128, 32])
            Bre = load(bre_l, [128, 32, 16]); Bim = load(bim_l, [128, 32, 16])
            Cre = load(cre_l, [128, 32, 16]); Cim = load(cim_l, [128, 32, 16])
            Dfold = load(dfold_l, [128, 32]); mle = load(mle_l, [128, 128]); mge = load(mge_l, [128, 128])
            halfpi = newT([128, 1]); memset(halfpi, float(np.pi / 2))

            def s32():
                return newT([128, 32])
            def cmul(a, b):
                (ar_, ai_), (br_, bi_) = a, b
                t1, t2, t3, t4, cr, ci = s32(), s32(), s32(), s32(), s32(), s32()
                tt(t1, ar_, br_, ALU.mult); tt(t2, ai_, bi_, ALU.mult); tt(cr, t1, t2, ALU.subtract)
                tt(t3, ar_, bi_, ALU.mult); tt(t4, ai_, br_, ALU.mult); tt(ci, t3, t4, ALU.add)
                return (cr, ci)

            dtt = s32(); act(dtt, logdt, AF.Exp)
            ar = s32(); ai = s32(); tt(ar, lamre, dtt, ALU.mult); tt(ai, lamim, dtt, ALU.mult)
            mag = s32(); act(mag, ar, AF.Exp, scale=1.0 / 32)
            sn = s32(); act(sn, ai, AF.Sin, scale=1.0 / 32)
            cs = s32(); act(cs, ai, AF.Sin, scale=1.0 / 32, bias=halfpi)
            zr = s32(); zi = s32(); tt(zr, mag, cs, ALU.mult); tt(zi, mag, sn, ALU.mult)
            z = (zr, zi)
            for _ in range(5):
                z = cmul(z, z)
            lb = z
            one = s32(); zero = s32(); memset(one, 1.0); memset(zero, 0.0)
            pw = [(one, zero), lb]
            for e_ in range(2, 9):
                pw.append(cmul(pw[-1], lb))
            m2 = s32(); t_ = s32(); tt(m2, lb[0], lb[0], ALU.mult); tt(t_, lb[1], lb[1], ALU.mult)
            tt(m2, m2, t_, ALU.add); rinv = s32(); recip(rinv, m2)
            ilr = s32(); ili = s32(); tt(ilr, lb[0], rinv, ALU.mult); tt(ili, lb[1], rinv, ALU.mult)
            ts(ili, ili, -1.0, None, ALU.mult)
            ilb = (ilr, ili)
            ipw = [(one, zero), ilb]
            for e_ in range(2, 8):
                ipw.append(cmul(ipw[-1], ilb))
            nr = s32(); ts(nr, lb[0], -1.0, None, ALU.add)
            den = s32(); t2_ = s32(); tt(den, lamre, lamre, ALU.mult); tt(t2_, lamim, lamim, ALU.mult)
            tt(den, den, t2_, ALU.add); rden = s32(); recip(rden, den)
            t5 = s32(); t6 = s32(); kr = s32(); ki = s32()
            tt(t5, nr, lamre, ALU.mult); tt(t6, lb[1], lamim, ALU.mult); tt(kr, t5, t6, ALU.add)
            tt(t5, lb[1], lamre, ALU.mult); tt(t6, nr, lamim, ALU.mult); tt(ki, t5, t6, ALU.subtract)
            tt(kr, kr, rden, ALU.mult); tt(ki, ki, rden, ALU.mult)
            selPr = newT([128, 32, 8]); selPi = newT([128, 32, 8]); selQr = newT([128, 32, 8]); selQi = newT([128, 32, 8])
            for j in range(8):
                for (dst, src, comp) in ((selPr, pw, 0), (selPi, pw, 1), (selQr, ipw, 0), (selQi, ipw, 1)):
                    cp(dst[0:64, :, j], src[7 - j][comp][0:64, :])
                    cp(dst[64:128, :, j], src[j][comp][64:128, :])
            def bc8(x):
                return x.v(lambda a: a.unsqueeze(2).to_broadcast([128, 32, 8]))
            wr = newT([128, 32, 8]); wi = newT([128, 32, 8]); ta = newT([128, 32, 8]); tb_ = newT([128, 32, 8])
            tt(ta, selPr, bc8(kr), ALU.mult); tt(tb_, selPi, bc8(ki), ALU.mult); tt(wr, ta, tb_, ALU.subtract)
            tt(ta, selPr, bc8(ki), ALU.mult); tt(tb_, selPi, bc8(kr), ALU.mult); tt(wi, ta, tb_, ALU.add)
            def bj(x):
                return x.v(lambda a: a.unsqueeze(3).to_broadcast([128, 32, 8, 16]))
            def bh(x):
                return x.v(lambda a: a.unsqueeze(2).to_broadcast([128, 32, 8, 16]))
            big1 = newT([128, 32, 8, 16]); big2 = newT([128, 32, 8, 16])
            Pr = newT([128, 32, 8, 16], BF16, "Pr"); Pi = newT([128, 32, 8, 16], BF16, "Pi")
            tt(big1, bj(wr), bh(Bre), ALU.mult); tt(big2, bj(wi), bh(Bim), ALU.mult); tt(Pr, big1, big2, ALU.subtract)
            tt(big1, bj(wr), bh(Bim), ALU.mult); tt(big2, bj(wi), bh(Bre), ALU.mult); tt(Pi, big1, big2, ALU.add)
            tt(big1, bj(selQr), bh(Cre), ALU.mult); tt(big2, bj(selQi), bh(Cim), ALU.mult); tt(Qr, big1, big2, ALU.subtract)
            tt(big1, bj(selQr), bh(Cim), ALU.mult); tt(big2, bj(selQi), bh(Cre), ALU.mult); tt(big1, big1, big2, ALU.add)
            ts(QiN, big1, -1.0, None, ALU.mult)
            def g128(x, g):
                return x.v(lambda a: a[:, g, :, :].rearrange("p j h -> p (j h)"))
            mtmp = newT([128, 4, 128])
            qa = newT([128, 32, 8, 16], BF16); qb = newT([128, 32, 8, 16], BF16)
            for d_ in range(2):
                cp(qa, Qr); cp(qb, QiN, eng="act")
                z = slice(64, 128) if d_ == 0 else slice(0, 64)
                memset(qa[z], 0.0); memset(qb[z], 0.0)
                msk = (mle if d_ == 0 else mge).v(lambda a: a.unsqueeze(1).to_broadcast([128, 4, 128]))
                for g0 in range(0, 32, 4):
                    bk = nbank()
                    for gg in range(4):
                        g = g0 + gg
                        mmg(bk[:, gg * 128:(gg + 1) * 128], [(g128(Pr, g), g128(qa, g)), (g128(Pi, g), g128(qb, g))])
                    bv = bk.v(lambda a: a.rearrange("p (g c) -> p g c", g=4))
                    tt(mtmp, bv, msk, ALU.mult)
                    for gg in range(4):
                        g = g0 + gg
                        if d_ == 0:
                            stt(Mt[:, g, :], identF, Dfold[:, g:g + 1], mtmp[:, gg, :], ALU.mult, ALU.add)
                        else:
                            tt(Mt[:, g, :], Mt[:, g, :], mtmp[:, gg, :], ALU.add)
            for g0 in range(0, 32, 4):
                bk = bf(nbank())
                for gg in range(4):
                    for ri, src in enumerate((Pr, Pi)):
                        col = (gg * 2 + ri) * 128
                        tpose(bk[:, col:col + 128], g128(src, g0 + gg), identB)
                cp(PT[:, g0:g0 + 4, :, :], bk.v(lambda a: a.rearrange("p (g r c) -> p g r c", g=4, r=2)), eng=evac_eng())
            cp(A1[:, 0, :], pw[8][0]); cp(A1[:, 1, :], pw[8][0])
            ts(A2[:, 0, :], pw[8][1], -1.0, None, ALU.mult); cp(A2[:, 1, :], pw[8][1])
            scope_end()

            return Qr, QiN, Mt, PT, A1, A2

        print("NOPS at end of consts", Sched.nops)
        if STAGE < 1:
            S.dead = True
        class _Gen:
            pass
        gen = _Gen()

        def alloc_gen(kv=True):
            gen.xt = [newT([128, D]) for i in range(2)]
            gen.xs = [newT([128, D], BF16) for i in range(2)]
            gen.junk = newT([128, D], BF16)
            gen.ssq = [newT([128, 1]) for i in range(2)]
            gen.rstd = [newT([128, 1]) for i in range(2)]
            if kv == "one":
                _kv2 = newT([128, 1024])
                gen.kv_sb = [_kv2, _kv2]
            elif kv:
                gen.kv_sb = [newT([128, 1024]) for i in range(2)]
            else:
                _kv1 = newT([128, 512])
                gen.kv_sb = [_kv1, _kv1]
        _nt = [0]

        def norm_tile(x_in, hT_out, scT, shT):
            _nt[0] += 1
            i = _nt[0] % 2
            act(gen.junk, x_in, AF.Square, accum=gen.ssq[i])
            act(gen.rstd[i], gen.ssq[i], AF.Sqrt, scale=1.0 / D, bias=epsT[:, 0:1])
            recip(gen.rstd[i], gen.rstd[i])
            ts(gen.xs[i], x_in, gen.rstd[i][:, 0:1], None, ALU.mult)
            bk = bf(nbank())
            for kc in range(KC):
                tpose(bk[:, kc * 128:(kc + 1) * 128], gen.xs[i][:, kc * 128:(kc + 1) * 128], identB)
            for kc in range(KC):
                act(hT_out[:, kc, :], bk[:, kc * 128:(kc + 1) * 128], AF.Identity,
                    scale=scT[:, kc:kc + 1], bias=shT[:, kc:kc + 1])

        lq = load(lq_bc, [128, 2, 64]); lk = load(lk_bc, [128, 2, 64])
        lprod = newT([128, 2, 64]); tt(lprod, lq, lk, ALU.mult)
        lsum = newT([128, 2])
        S.op("dve", lambda e: e.tensor_reduce(out=lsum.ap, in_=lprod.ap, axis=AX.X, op=ALU.add),
             reads=[lprod.buf], writes=[lsum.buf])
        lexp = newT([128, 2]); act(lexp, lsum, AF.Exp)
        lamneg = newT([128, 1])
        tt(lamneg, lexp[:, 1:2], lexp[:, 0:1], ALU.subtract)
        ts(lamneg, lamneg, -0.2, None, ALU.add)
        subw = load(subw_bc, [128, 128]); ts(subw, subw, 0.8, None, ALU.mult)

        catS = newT([128, KC, NS_TOK], BF16, "catS")
        pos = load(pos_rc, [128, 32, 2]); fidx = load(fidx_bc, [128, 16])
        freq = newT([128, 16]); act(freq, fidx, AF.Exp, scale=-float(np.log(10000.0)) / 16)
        scope_begin()
        Qr, QiN, Mt, PT, A1, A2 = build_s5_consts()
        scope_begin()
        Uf = newT([128, 32, 128], BF16, "Uf")
        yf = newT([128, 32, 128], BF16, "yf")
        qT = newT([128, 4, NP_TOK], BF16, "qT"); kT = newT([128, 4, NP_TOK], BF16, "kT")
        Vaug = newT([128, 8, 4, 129], BF16, "Vaug"); memset(Vaug, 1.0)
        scope_begin()
        winT = load_win()
        hT = newT([128, KC, NP_TOK], BF16, "hT")
        scope_begin()
        alloc_gen(kv="one")
        _qk1 = newT([128, 1024], BF16, name="qkbf")
        qk_bf = [_qk1, _qk1]
        for t in range(8):
            tok0 = t * 128
            i = t % 2
            S.dma("sp", gen.xt[i].ap, xp[tok0:tok0 + 128, :], writes=[gen.xt[i].buf], stream="x")
            norm_tile(gen.xt[i], hT[:, :, tok0:tok0 + 128], sc1T[:, :, 0], modTT[:, 0, :, 0])
            bq, bkk, bv_ = nbank(), nbank(), nbank()
            for bnk, c0 in ((bq, 512), (bkk, 1024), (bv_, 1536)):
                mmg(bnk, [(hT[:, kc, tok0:tok0 + 128], winT[:, kc, c0:c0 + 512]) for kc in range(KC)])
            cp(gen.kv_sb[i][:, 0:512], bkk, eng="act"); cp(gen.kv_sb[i][:, 512:1024], bv_, eng="dve")
            S.dma("sp", o_ck[tok0:tok0 + 128, :], gen.kv_sb[i].ap[:, 0:512], reads=[gen.kv_sb[i].buf], stream="o")
            S.dma("sp", o_cv[tok0:tok0 + 128, :], gen.kv_sb[i].ap[:, 512:1024], reads=[gen.kv_sb[i].buf], stream="o")
            cp(qk_bf[i][:, 0:512], bq, eng="act"); cp(qk_bf[i][:, 512:1024], gen.kv_sb[i][:, 0:512], eng="dve")
            cp(Vaug[:, t, :, 0:128], gen.kv_sb[i][:, 512:1024].v(lambda a: a.rearrange("p (h e) -> p h e", h=4)), eng="dve")
            bt = bf(nbank())
            for k8 in range(8):
                tpose(bt[:, k8 * 128:(k8 + 1) * 128], qk_bf[i][:, k8 * 128:(k8 + 1) * 128], identB)
            cp(qT[:, :, tok0:tok0 + 128], bt[:, 0:512].v(lambda a: a.rearrange("p (h c) -> p h c", h=4)), eng="act")
            cp(kT[:, :, tok0:tok0 + 128], bt[:, 512:1024].v(lambda a: a.rearrange("p (h c) -> p h c", h=4)), eng="act")

        scope_end()
        hT_cj = hT.v(lambda a: a.rearrange("p k (c j) -> p k c j", j=8))
        u_cm = newT([128, 32, 8, 16], BF16, "u_cm")
        for j in range(8):
            bk = nbank()
            mmg(bk, [(hT_cj[:, kc, :, j], winT[:, kc, 0:512]) for kc in range(KC)])
            cp(u_cm[:, :, j, :], bk.v(lambda a: a.rearrange("p (g h) -> p g h", h=16)), eng=evac_eng())
        for g0 in range(0, 32, 8):
            bk = bf(nbank())
            for gg in range(8):
                g = g0 + gg
                tpose(bk[:, gg * 128:(gg + 1) * 128], g128(u_cm, g), identB)
            cp(Uf[:, g0:g0 + 8, :], bk.v(lambda a: a.rearrange("p (g c) -> p g c", g=8)), eng=evac_eng())
        scope_end()
        scope_begin()
        Sp = newT([128, 128, 2, 32], BF16, "Sp")
        if STAGE < 2:
            S.dead = True
        for g0 in range(0, 32, 2):
            bk = nbank()
            for gg in range(2):
                for ri in range(2):
                    col = (gg * 2 + ri) * 128
                    mmg(bk[:, col:col + 128], [(PT[:, g0 + gg, ri, :], Uf[:, g0 + gg, :])])
            cp(Sp[:, :, :, g0:g0 + 2], bk.v(lambda a: a.rearrange("p (g r c) -> p c r g", g=2, r=2)), eng=evac_eng())
        if STAGE < 3:
            S.dead = True
        Gs = newT([128, 4, 2, 32], F32, "Gs"); memset(Gs, 0.0)
        Tt_ = newT([128, 4, 2, 32], F32, "Tt"); X1 = newT([128, 4, 2, 32]); X2 = newT([128, 4, 2, 32])
        Gst = newT([128, 2, 32, 128], BF16, "Gst")
        Sp_sl = Sp.v(lambda a: a.rearrange("p (s l) r g -> p s l r g", l=32))
        Gst_sl = Gst.v(lambda a: a.rearrange("p r g (s l) -> p s r g l", l=32))
        A1b = A1.v(lambda a: a.unsqueeze(1).to_broadcast([128, 4, 2, 32]))
        for step in range(32):
            for half, l in ((slice(0, 64), step), (slice(64, 128), 31 - step)):
                cp(Gst_sl[half, :, :, :, l], Gs[half])
                tt(Tt_[half], Gs[half], Sp_sl[half, :, l, :, :], ALU.add)
                tt(X1[half], Tt_[half], A1b[half], ALU.mult)
                for ri in range(2):
                    tt(X2[half, :, ri, :], Tt_[half, :, 1 - ri, :],
                       A2.v(lambda a: a[:, ri, :].unsqueeze(1).to_broadcast([128, 4, 32]))[half], ALU.mult)
                tt(Gs[half], X1[half], X2[half], ALU.add)
        if STAGE < 4:
            S.dead = True
        hfin = newT([128, 2, 128], F32, "hfin")
        Tt_flat = Tt_.v(lambda a: a.rearrange("p s r g -> p (s r g)"))
        bk = nbank()
        for k2 in range(2):
            tpose(bk[:, k2 * 128:(k2 + 1) * 128], Tt_flat[:, k2 * 128:(k2 + 1) * 128], identF)
        cp(hfin, bk[:, 0:256].v(lambda a: a.rearrange("p (k c) -> p k c", k=2)))
        S.dma("sp", o_st.rearrange("(k p) c -> p k c", p=128), hfin.ap, reads=[hfin.buf], stream="o")
        for g0 in range(0, 32, 4):
            bk = nbank()
            for gg in range(4):
                g = g0 + gg
                mmg(bk[:, gg * 128:(gg + 1) * 128],
                    [(Mt[:, g, :], Uf[:, g, :]), (g128(Qr, g), Gst[:, 0, g, :]), (g128(QiN, g), Gst[:, 1, g, :])])
            cp(yf[:, g0:g0 + 4, :], bk.v(lambda a: a.rearrange("p (g c) -> p g c", g=4)), eng=evac_eng())
        scope_end()
        catT = newT([128, KC, NP_TOK], BF16, "catT")
        scope_begin()
        y_cm = newT([128, 8, 512], F32, "y_cm")
        for g0 in range(0, 32, 8):
            bk = bf(nbank())
            for gg in range(8):
                tpose(bk[:, gg * 128:(gg + 1) * 128], yf[:, g0 + gg, :], identB)
            cp(y_cm[:, :, 16 * g0:16 * g0 + 128].v(lambda a: a.rearrange("p i (g h) -> p g i h", g=8)),
               bk.v(lambda a: a.rearrange("p (g i h) -> p g i h", g=8, i=8)), eng=evac_eng())
        if STAGE < 5:
            S.dead = True
        g_cm = newT([128, 8, 512], BF16, "g_cm")
        gt1 = newT([128, 4, 512]); gt2 = newT([128, 4, 512])
        for hf in range(2):
            ysl = y_cm[:, hf * 4:(hf + 1) * 4, :]
            act(gt1, ysl, AF.Square)
            ts(gt1, gt1, 0.044715, 1.0, ALU.mult, ALU.add)
            tt(gt1, gt1, ysl, ALU.mult)
            act(gt2, gt1, AF.Sigmoid, scale=1.5957691216057308)
            tt(g_cm[:, hf * 4:(hf + 1) * 4, :], ysl, gt2, ALU.mult)
        gT = newT([128, 4, NP_TOK], BF16, "gT")
        gT_v = gT.v(lambda a: a.rearrange("p k (c j) -> p k c j", j=8))
        for i0 in range(0, 8, 2):
            bk = bf(nbank())
            for ii in range(2):
                for k4 in range(4):
                    col = (ii * 4 + k4) * 128
                    tpose(bk[:, col:col + 128], g_cm[:, i0 + ii, k4 * 128:(k4 + 1) * 128], identB)
            for ii in range(2):
                cp(gT_v[:, :, :, i0 + ii],
                   bk[:, ii * 512:(ii + 1) * 512].v(lambda a: a.rearrange("p (k c) -> p k c", k=4)), eng="act")
        sg = [newT([128, 512], name=f"sg{i}") for i in range(2)]
        for m4 in range(4):
            for tb2 in range(2):
                bk = nbank()
                tsl = slice(tb2 * 512, (tb2 + 1) * 512)
                mmg(bk, [(wglu_sb[:, k4, m4 * 128:(m4 + 1) * 128], gT[:, k4, tsl]) for k4 in range(4)])
                act(sg[tb2], bk, AF.Sigmoid)
                tt(catT[:, m4, tsl], gT[:, m4, tsl], sg[tb2], ALU.mult)
        scope_end()

        scope_begin()
        kTm = [newT([128, 4, NP_TOK], BF16, f"kTm{m}") for m in range(2)]
        for m in range(2):
            cp(kTm[m], kT, eng=("act" if m else "dve"))
            z = slice(64, 128) if m == 0 else slice(0, 64)
            memset(kTm[m][z], 0.0)
        PTs = [newT([128, 512], BF16, name=f"PTs{m}") for m in range(2)]
        o_tok = [newT([128, 4, 128], name=f"otok{q}") for q in range(2)]
        rr = newT([128, 2]); sq = newT([128, 4, 128]); ss4 = newT([128, 4]); on = newT([128, 4, 128], BF16)
        for s_ in range(4):
            for hd in range(4):
                for m in range(2):
                    bk = nbank()
                    for kt in range(2):
                        k0 = s_ * 256 + kt * 128
                        mmg(bk[:, kt * 256:(kt + 1) * 256],
                            [(kTm[m][:, hd, k0:k0 + 128], qT[:, hd, s_ * 256:(s_ + 1) * 256])])
                    act(PTs[m], bk, AF.Exp, scale=0.125)
                for qt in range(2):
                    bk = nbank()
                    for m in range(2):
                        mmg(bk[:, m * 129:(m + 1) * 129],
                            [(PTs[m][:, kt * 256 + qt * 128:kt * 256 + qt * 128 + 128], Vaug[:, 2 * s_ + kt, hd, :])
                             for kt in range(2)])
                    recip(rr[:, 0:1], bk[:, 128:129]); recip(rr[:, 1:2], bk[:, 257:258])
                    tt(rr[:, 1:2], rr[:, 1:2], lamneg, ALU.mult)
                    ts(o_tok[qt][:, hd, :], bk[:, 0:128], rr[:, 0:1], None, ALU.mult)
                    stt(o_tok[qt][:, hd, :], bk[:, 129:257], rr[:, 1:2], o_tok[qt][:, hd, :], ALU.mult, ALU.add)
            for qt in range(2):
                tok0 = s_ * 256 + qt * 128
                tt(sq, o_tok[qt], o_tok[qt], ALU.mult)
                S.op("dve", lambda e: e.tensor_reduce(out=ss4.ap, in_=sq.ap, axis=AX.X, op=ALU.add),
                     reads=[sq.buf], writes=[ss4.buf])
                act(ss4, ss4, AF.Sqrt, scale=1.0 / 128, bias=epsT[:, 0:1])
                recip(ss4, ss4)
                tt(sq, o_tok[qt], ss4.v(lambda a: a.unsqueeze(2).to_broadcast([128, 4, 128])), ALU.mult)
                tt(on, sq, subw.v(lambda a: a.unsqueeze(1).to_broadcast([128, 4, 128])), ALU.mult)
                bk = bf(nbank())
                for hd in range(4):
                    tpose(bk[:, hd * 128:(hd + 1) * 128], on[:, hd, :], identB)
                cp(catT[:, 4:8, tok0:tok0 + 128], bk[:, 0:512].v(lambda a: a.rearrange("p (h c) -> p h c", h=4)), eng="act")
        scope_end()

        scope_begin()
        alloc_gen()
        wout_sb = newT([128, KC, D], BF16, "wout_sb")
        for kc in range(KC):
            S.dma("pool", wout_sb.ap[:, kc, :], wout_v[:, kc, :], writes=[wout_sb.buf], stream="w")
        x1t = [newT([128, D], name=f"x1t{i}") for i in range(2)]
        wtmp = newT([128, 512])
        for t in range(8):
            tok0 = t * 128
            i = t % 2
            S.dma("sp", gen.xt[i].ap, xp[tok0:tok0 + 128, :], writes=[gen.xt[i].buf], stream="x")
            for cb in range(2):
                csl = slice(cb * 512, (cb + 1) * 512)
                bk = nbank()
                mmg(bk, [(catT[:, kc, tok0:tok0 + 128], wout_sb[:, kc, csl]) for kc in range(KC)])
                tt(wtmp, bk, gateT[:, 0, 0, csl], ALU.mult)
                tt(x1t[i][:, csl], wtmp, gen.xt[i][:, csl], ALU.add)
            S.dma("sp", x1_d[tok0:tok0 + 128, :], x1t[i].ap, reads=[x1t[i].buf], stream="o")
        S.dead = False
        scope_end()
        scope_end()
        if STAGE < 6:
            S.dead = True
        H0, H1 = slice(0, 64), slice(64, 128)

        yf2 = newT([128, 2, 32, 128], BF16, "yf2")
        scope_begin()
        UfA = newT([128, 32, 512], BF16, "UfA")
        scope_begin()
        alloc_gen()
        winu = newT([128, KC, 512], BF16, "winu")
        for kc in range(KC):
            S.dma("pool", winu.ap[:, kc, :], w_in_v[:, kc, 0:512], writes=[winu.buf], stream="w")
        hTb = newT([128, KC, 1024], BF16, "hTb"); u_cm = newT([128, 32, 8, 16], BF16, "u_cm_s")
        hTb_cj = hTb.v(lambda a: a.rearrange("p k (c j) -> p k c j", j=8))
        for blk in range(4):
            for t in range(8):
                i = t % 2
                r0 = blk * 1024 + t * 128
                S.dma("sp", gen.xt[i].ap, xs_all[r0:r0 + 128, :], writes=[gen.xt[i].buf], stream="x")
                norm_tile(gen.xt[i], hTb[:, :, t * 128:(t + 1) * 128], sc1T[:, :, 1], modTT[:, 0, :, 1])
            for j in range(8):
                bk = nbank()
                mmg(bk, [(hTb_cj[:, kc, :, j], winu[:, kc, :]) for kc in range(KC)])
                cp(u_cm[:, :, j, :], bk.v(lambda a: a.rearrange("p (g h) -> p g h", h=16)), eng=evac_eng())
            for g0 in range(0, 32, 8):
                bk = bf(nbank())
                for gg in range(8):
                    tpose(bk[:, gg * 128:(gg + 1) * 128], g128(u_cm, g0 + gg), identB)
                cp(UfA[:, g0:g0 + 8, blk * 128:(blk + 1) * 128],
                   bk.v(lambda a: a.rearrange("p (g c) -> p g c", g=8)), eng="act")
        scope_end()
        GstO = newT([128, 2, 32, 256], BF16, "GstO")
        SpA = newT([128, 64, 2, 32], BF16, "SpA"); SpB = newT([128, 64, 2, 32], BF16, "SpB")
        h0 = load(h0_l, [128, 2, 32])
        Gs = newT([128, 2, 32]); Tt2 = newT([128, 2, 32]); X1s = newT([128, 2, 32]); X2s = newT([128, 2, 32])

        def compute_Sp(dst, c0):
            for g0 in range(0, 32, 4):
                bk = nbank()
                for gg in range(4):
                    for ri in range(2):
                        col = (gg * 2 + ri) * 64
                        mmg(bk[:, col:col + 64], [(PT[:, g0 + gg, ri, :], UfA[:, g0 + gg, c0:c0 + 64])])
                cp(dst[:, :, :, g0:g0 + 4], bk.v(lambda a: a.rearrange("p (g r c) -> p c r g", g=4, r=2)), eng=evac_eng())

        X1p = newT([128, 2, 32]); Tt2p = newT([128, 2, 32]); Gsp = newT([128, 2, 32])
        X2dT = newT([128, 2, 32]); X2qT = newT([128, 2, 32])
        X2d = [TT(X2dT.ap[:, ri, :], Buf()) for ri in range(2)]
        X2q = [TT(X2qT.ap[:, ri, :], Buf()) for ri in range(2)]

        def a8mul(dst, src, half, eng="dve"):
            xa, xb, xw = (X1s, X2d, X2dT) if eng == "dve" else (X1p, X2q, X2qT)
            tt(xa[half], src[half], A1[half], ALU.mult, eng=eng)
            for ri in range(2):
                tt(xb[ri][half], src[half, 1 - ri, :], A2[half, ri, :], ALU.mult, eng=eng)
            S.op(eng, lambda e: e.tensor_tensor(out=dst[half].ap, in0=xa[half].ap, in1=xw[half].ap, op=ALU.add),
                 reads=[xa.buf, xb[0].buf, xb[1].buf], writes=[dst.buf])

        a8mul(Gsp, h0, H0, eng="pool"); a8mul(Gs, h0, H1)
        for k in range(256):
            if k % 64 == 0:
                compute_Sp(SpA, k); compute_Sp(SpB, 448 - k)
            cp(GstO[H0, :, :, k], Gsp[H0], eng="act")
            tt(Tt2p[H0], Gsp[H0], SpA[H0, k % 64, :, :], ALU.add, eng="pool")
            a8mul(Gsp, Tt2p, H0, eng="pool")
            tt(Tt2[H1], Gs[H1], SpB[H1, 63 - k % 64, :, :], ALU.add)
            a8mul(Gs, Tt2, H1)
        for k in range(256, 512):
            cb_ = 511 - k
            if k % 64 == 0:
                compute_Sp(SpB, 448 - k)
            cp(GstO[H1, :, :, cb_], Gs[H1], eng="act")
            tt(Tt2[H1], Gs[H1], SpB[H1, 63 - k % 64, :, :], ALU.add)
            a8mul(Gs, Tt2, H1)
        for blk in range(2):
            csl = slice(blk * 128, (blk + 1) * 128)
            for g0 in range(0, 32, 4):
                bk = nbank()
                for gg in range(4):
                    g = g0 + gg
                    mmg(bk[:, gg * 128:(gg + 1) * 128],
                        [(Mt[:, g, :], UfA[:, g, csl]), (g128(Qr, g), GstO[:, 0, g, csl]), (g128(QiN, g), GstO[:, 1, g, csl])])
                cp(yf2[:, blk, g0:g0 + 4, :], bk.v(lambda a: a.rearrange("p (g c) -> p g c", g=4)), eng=evac_eng())
        scope_end()
        scope_begin()
        y_cm = newT([128, 8, 512], F32, "y_cm_s"); g_cm = newT([128, 8, 512], BF16, "g_cm_s")
        gt1 = newT([128, 4, 512]); gt2 = newT([128, 4, 512])
        gT = newT([128, 4, 1024], BF16, "gT_s")
        gT_v = gT.v(lambda a: a.rearrange("p k (c j) -> p k c j", j=8))
        sg = [newT([128, 512]) for i in range(2)]
        for blk in range(2):
            for g0 in range(0, 32, 8):
                bk = bf(nbank())
                for gg in range(8):
                    tpose(bk[:, gg * 128:(gg + 1) * 128], yf2[:, blk, g0 + gg, :], identB)
                cp(y_cm[:, :, 16 * g0:16 * g0 + 128].v(lambda a: a.rearrange("p i (g h) -> p g i h", g=8)),
                   bk.v(lambda a: a.rearrange("p (g i h) -> p g i h", g=8, i=8)), eng="act")
            for hf in range(2):
                ysl = y_cm[:, hf * 4:(hf + 1) * 4, :]
                act(gt1, ysl, AF.Square)
                ts(gt1, gt1, 0.044715, 1.0, ALU.mult, ALU.add)
                tt(gt1, gt1, ysl, ALU.mult)
                act(gt2, gt1, AF.Sigmoid, scale=1.5957691216057308)
                tt(g_cm[:, hf * 4:(hf + 1) * 4, :], ysl, gt2, ALU.mult)
            for i0 in range(0, 8, 2):
                bk = bf(nbank())
                for ii in range(2):
                    for k4 in range(4):
                        col = (ii * 4 + k4) * 128
                        tpose(bk[:, col:col + 128], g_cm[:, i0 + ii, k4 * 128:(k4 + 1) * 128], identB)
                for ii in range(2):
                    cp(gT_v[:, :, :, i0 + ii],
                       bk[:, ii * 512:(ii + 1) * 512].v(lambda a: a.rearrange("p (k c) -> p k c", k=4)), eng="act")
            for m4 in range(4):
                for tb2 in range(2):
                    bk = nbank()
                    tsl = slice(tb2 * 512, (tb2 + 1) * 512)
                    osl = slice(blk * 1024 + tb2 * 512, blk * 1024 + (tb2 + 1) * 512)
                    mmg(bk, [(wglu_sb[:, k4, m4 * 128:(m4 + 1) * 128], gT[:, k4, tsl]) for k4 in range(4)])
                    act(sg[tb2], bk, AF.Sigmoid)
                    tt(catS[:, m4, osl], gT[:, m4, tsl], sg[tb2], ALU.mult)
        scope_end()
        scope_end()

        if STAGE < 7:
            S.dead = True
        scope_begin()
        kTa = newT([128, 4, 4608], BF16, "kTa"); Vs = newT([128, 36, 4, 129], BF16, "Vs"); memset(Vs, 1.0)
        qTs = newT([128, 4, NS_TOK], BF16, "qTs")
        scope_begin()
        alloc_gen(kv=False)
        winq = newT([128, KC, 1536], BF16, "winq")
        for kc in range(KC):
            S.dma("pool", winq.ap[:, kc, :], w_in_v[:, kc, 512:2048], writes=[winq.buf], stream="w")
        hTt2 = [newT([128, KC, 128], BF16, "hTt") for _ in range(2)]
        qkf2 = [newT([128, 2, 512], F32, "qkf") for _ in range(2)]
        qkb2 = [newT([128, 2, 512], BF16, "qkb") for _ in range(2)]
        ang4 = newT([128, 2, 2, 16]); kf4 = newT([128, 2, 2, 16])
        SC2 = [newT([128, 2, 2, 16]) for _ in range(2)]
        r1 = newT([128, 16, 2, 16]); r2 = newT([128, 16, 2, 16])
        MAGIC = 12582912.0
        TWO_PI = float(2 * np.pi)
        vbank = {}

        def stA(tile):
            own = tile < 16
            hTt, qkf, qkb = hTt2[tile % 2], qkf2[tile % 2], qkb2[tile % 2]
            if tile < 32:
                i = tile % 2
                S.dma("sp", gen.xt[i].ap, xs_all[tile * 128:(tile + 1) * 128, :], writes=[gen.xt[i].buf], stream="x")
                SC = SC2[tile % 2]
                tt(ang4[:, 0, :, :], pos[:, tile, :].v(lambda a: a.unsqueeze(2).to_broadcast([128, 2, 16])),
                   freq.v(lambda a: a.unsqueeze(1).to_broadcast([128, 2, 16])), ALU.mult)
                ts(ang4[:, 1, :, :], ang4[:, 0, :, :], float(np.pi / 2), None, ALU.add)
                ts(kf4, ang4, 1.0 / TWO_PI, MAGIC, ALU.mult, ALU.add)
                ts(kf4, kf4, -MAGIC, None, ALU.add)
                stt(kf4, kf4, -TWO_PI, ang4, ALU.mult, ALU.add)
                ts(kf4, kf4, -3.1415925, 3.1415925, ALU.max, ALU.min)
                act(SC, kf4, AF.Sin)
                norm_tile(gen.xt[i], hTt, sc1T[:, :, 1], modTT[:, 0, :, 1])
                bkk, bv_ = nbank(), nbank()
                mmg(bkk, [(hTt[:, kc, :], winq[:, kc, 512:1024]) for kc in range(KC)])
                mmg(bv_, [(hTt[:, kc, :], winq[:, kc, 1024:1536]) for kc in range(KC)])
                cp(qkf[:, 1, :], bkk, eng="act")
                vbank[tile] = bv_
                if own:
                    bq = nbank()
                    mmg(bq, [(hTt[:, kc, :], winq[:, kc, 0:512]) for kc in range(KC)])
                    cp(qkf[:, 0, :], bq, eng="act")
            else:
                ct = tile - 32
                S.dma("sp", qkf.ap[:, 1, :], ck_in[ct * 128:(ct + 1) * 128, :], writes=[qkf.buf], stream="x")
                S.dma("sp", gen.xt[tile % 2].ap[:, 0:512], cv_in[ct * 128:(ct + 1) * 128, :], writes=[gen.xt[tile % 2].buf], stream="x")

        def stB(tile):
            own = tile < 16
            hTt, qkf, qkb = hTt2[tile % 2], qkf2[tile % 2], qkb2[tile % 2]
            if tile < 32:
                SC = SC2[tile % 2]
                cp(Vs[:, tile, :, 0:128], vbank.pop(tile).v(lambda a: a.rearrange("p (h e) -> p h e", h=4)), eng="dve")
                a0 = 0 if own else 1
                na = 2 - a0
                xv = qkf[:, a0:2, :].v(lambda a: a.rearrange("p a (b r x f) -> p (a b) r x f", r=2, x=2, f=16))
                ov = qkb[:, a0:2, :].v(lambda a: a.rearrange("p a (b r x f) -> p (a b) r x f", r=2, x=2, f=16))
                A_ = na * 8
                COS = SC[:, 1, :, :].v(lambda a: a.unsqueeze(1).to_broadcast([128, A_, 2, 16]))
                SIN = SC[:, 0, :, :].v(lambda a: a.unsqueeze(1).to_broadcast([128, A_, 2, 16]))
                x1 = xv[:, :, :, 0, :]; x2 = xv[:, :, :, 1, :]
                tt(r1[:, 0:A_], x1, COS, ALU.mult); tt(r2[:, 0:A_], x2, SIN, ALU.mult)
                tt(ov[:, :, :, 0, :], r1[:, 0:A_], r2[:, 0:A_], ALU.subtract)
                tt(r1[:, 0:A_], x2, COS, ALU.mult); tt(r2[:, 0:A_], x1, SIN, ALU.mult)
                tt(ov[:, :, :, 1, :], r1[:, 0:A_], r2[:, 0:A_], ALU.add)
            else:
                cp(qkb[:, 1, :], qkf[:, 1, :])
                cp(Vs[:, tile, :, 0:128], gen.xt[tile % 2][:, 0:512].v(lambda a: a.rearrange("p (h e) -> p h e", h=4)))
            bt = bf(nbank())
            for k8 in range(4 if not own else 8):
                src = qkb[:, 1, (k8 % 4) * 128:(k8 % 4 + 1) * 128] if k8 < 4 else qkb[:, 0, (k8 - 4) * 128:(k8 - 3) * 128]
                tpose(bt[:, k8 * 128:(k8 + 1) * 128], src, identB)
            cp(kTa[:, :, tile * 128:(tile + 1) * 128], bt[:, 0:512].v(lambda a: a.rearrange("p (h c) -> p h c", h=4)), eng="act")
            if own:
                cp(qTs[:, :, tile * 128:(tile + 1) * 128], bt[:, 512:1024].v(lambda a: a.rearrange("p (h c) -> p h c", h=4)), eng="act")

        stA(0)
        for tile in range(36):
            if tile + 1 < 36:
                stA(tile + 1)
            stB(tile)
        scope_end()
        qTm = [newT([128, 512], BF16, name=f"qTm{m}") for m in range(2)]
        PTs = [newT([128, 512], BF16, name=f"PTss{i}") for i in range(4)]
        Osb = [newT([128, 4, 129], name=f"Osb{m}") for m in range(2)]
        o_tok = newT([128, 4, 4, 128], F32, "o_tok_s")
        rr = newT([128, 2]); sq = newT([128, 4, 128]); ss4 = newT([128, 4]); on = newT([128, 4, 128], BF16)
        obank = [TT(banks[i][:, :], bbuf[i]) for i in range(4)]
        _sbk = [0]
        def sbank():
            _sbk[0] = (_sbk[0] + 1) % 4
            i = 4 + _sbk[0]
            return TT(banks[i][:, :], bbuf[i])
        _pt = [0]
        for qb in range(4):
            for hd in range(4):
                for m in range(2):
                    cp(qTm[m], qTs[:, hd, qb * 512:(qb + 1) * 512], eng=("act" if m else "dve"))
                    z = H1 if m == 0 else H0
                    memset(qTm[m][z], 0.0)
                    sbks = {}
                    Pbuf = {}
                    for it in range(36 + 3):
                        if it < 36:
                            kt = it
                            sbks[kt] = sbank()
                            mmg(sbks[kt], [(kTa[:, hd, kt * 128:(kt + 1) * 128], qTm[m])])
                        if 0 <= it - 2 < 36:
                            kt = it - 2
                            _pt[0] = (_pt[0] + 1) % 4
                            Pbuf[kt] = PTs[_pt[0]]
                            act(Pbuf[kt], sbks[kt], AF.Exp, scale=0.125)
                        if 0 <= it - 3 < 36:
                            kt = it - 3
                            P_ = Pbuf[kt]
                            for qt in range(4):
                                S.op("pe", lambda e, qt=qt, P_=P_, kt=kt: e.matmul(
                                    obank[qt].ap[:, 0:129], lhsT=P_.ap[:, qt * 128:(qt + 1) * 128],
                                    rhs=Vs.ap[:, kt, hd, :], start=(kt == 0), stop=(kt == 35)),
                                    reads=[P_.buf, Vs.buf], writes=[obank[qt].buf])
                    for qt in range(4):
                        cp(Osb[m][:, qt, :], obank[qt][:, 0:129], eng=("act" if qt % 2 else "dve"))
                for qt in range(4):
                    recip(rr[:, 0:1], Osb[0][:, qt, 128:129]); recip(rr[:, 1:2], Osb[1][:, qt, 128:129])
                    tt(rr[:, 1:2], rr[:, 1:2], lamneg, ALU.mult)
                    ts(o_tok[:, qt, hd, :], Osb[0][:, qt, 0:128], rr[:, 0:1], None, ALU.mult)
                    stt(o_tok[:, qt, hd, :], Osb[1][:, qt, 0:128], rr[:, 1:2], o_tok[:, qt, hd, :], ALU.mult, ALU.add)
            for qt in range(4):
                tok0 = qb * 512 + qt * 128
                ot = o_tok[:, qt, :, :]
                tt(sq, ot, ot, ALU.mult)
                S.op("dve", lambda e: e.tensor_reduce(out=ss4.ap, in_=sq.ap, axis=AX.X, op=ALU.add),
                     reads=[sq.buf], writes=[ss4.buf])
                act(ss4, ss4, AF.Sqrt, scale=1.0 / 128, bias=epsT[:, 0:1])
                recip(ss4, ss4)
                tt(sq, ot, ss4.v(lambda a: a.unsqueeze(2).to_broadcast([128, 4, 128])), ALU.mult)
                tt(on, sq, subw.v(lambda a: a.unsqueeze(1).to_broadcast([128, 4, 128])), ALU.mult)
                bk = TT(banks[4 + qt][:, :].bitcast(BF16), bbuf[4 + qt])
                for hd in range(4):
                    tpose(bk[:, hd * 128:(hd + 1) * 128], on[:, hd, :], identB)
                cp(catS[:, 4:8, tok0:tok0 + 128], bk[:, 0:512].v(lambda a: a.rearrange("p (h c) -> p h c", h=4)), eng="act")
        scope_end()

        scope_begin()
        alloc_gen()
        wout_sb = newT([128, KC, D], BF16, "wout_sb2")
        for kc in range(KC):
            S.dma("pool", wout_sb.ap[:, kc, :], wout_v[:, kc, :], writes=[wout_sb.buf], stream="w")
        x1t = [newT([128, D]) for i in range(2)]
        wtmp = newT([128, 512])
        for t in range(16):
            tok0 = t * 128
            i = t % 2
            S.dma("sp", gen.xt[i].ap, xs_all[tok0:tok0 + 128, :], writes=[gen.xt[i].buf], stream="x")
            for cb in range(2):
                csl = slice(cb * 512, (cb + 1) * 512)
                bk = nbank()
                mmg(bk, [(catS[:, kc, tok0:tok0 + 128], wout_sb[:, kc, csl]) for kc in range(KC)])
                tt(wtmp, bk, gateT[:, 1, 0, csl], ALU.mult)
                tt(x1t[i][:, csl], wtmp, gen.xt[i][:, csl], ALU.add)
            S.dma("sp", x1_d[NP_TOK + tok0:NP_TOK + tok0 + 128, :], x1t[i].ap, reads=[x1t[i].buf], stream="o")
        S.dead = False
        scope_end()
        scope_end()

        NT = NT_MOE
        scope_begin()
        h2T = newT([128, KC, NT * 128], BF16, "h2T")
        acc = newT([128, NT, D], F32, "acc")
        gates = newT([128, NT, 65], F32, "gates"); memset(gates, 1.0)
        _xt1 = newT([128, D])
        gen.xt = [_xt1, _xt1]
        _xs1 = newT([128, D], BF16)
        gen.xs = [_xs1, _xs1]
        gen.junk = newT([128, D], BF16)
        gen.ssq = [newT([128, 1]) for i in range(2)]
        gen.rstd = [newT([128, 1]) for i in range(2)]
        wr_sb = newT([128, KC, 64], BF16, "wr_sb")
        S.dma("pool", wr_sb.ap, w_router_d.rearrange("(kc p) n -> p kc n", p=128), writes=[wr_sb.buf], stream="w")
        rbias = load(rbias_bc, [128, 64])
        sco = newT([128, 64]); bia = newT([128, 64]); eq = newT([128, 64]); mk1 = newT([128, 64])
        gm1 = newT([128, 8]); gm2 = newT([128, 8]); gsc = newT([128, 8]); top8 = newT([128, 8])
        gmask = newT([128, 8]); pen = newT([128, 8]); sel = newT([128, 64]); den = newT([128, 1])
        def g88(x):
            return x.v(lambda a: a.rearrange("p (g e) -> p g e", e=8))
        def b88(x):
            return x.v(lambda a: a.unsqueeze(2).to_broadcast([128, 8, 8]))
        def prologue(t):
            cond = 0 if t < 8 else 1
            i = t % 2
            S.dma("sp", gen.xt[i].ap, x1_d[t * 128:(t + 1) * 128, :], writes=[gen.xt[i].buf], stream="x")
            norm_tile(gen.xt[i], h2T[:, :, t * 128:(t + 1) * 128], sc2T[:, :, cond], modTT[:, 2, :, cond])
            bk = nbank()
            mmg(bk[:, 0:64], [(h2T[:, kc, t * 128:(t + 1) * 128], wr_sb[:, kc, :]) for kc in range(KC)])
            act(sco, bk[:, 0:64], AF.Sigmoid)
            tt(bia, sco, rbias, ALU.add)
            S.op("dve", lambda e: e.tensor_reduce(out=gm1.ap, in_=g88(bia).ap, axis=AX.X, op=ALU.max),
                 reads=[bia.buf], writes=[gm1.buf])
            tt(g88(eq), g88(bia), b88(gm1), ALU.is_equal)
            stt(mk1, eq, -1e9, bia, ALU.mult, ALU.add)
            S.op("dve", lambda e: e.tensor_reduce(out=gm2.ap, in_=g88(mk1).ap, axis=AX.X, op=ALU.max),
                 reads=[mk1.buf], writes=[gm2.buf])
            tt(gsc, gm1, gm2, ALU.add)
            S.op("dve", lambda e: e.max(out=top8.ap, in_=gsc.ap), reads=[gsc.buf], writes=[top8.buf])
            ts(gmask, gsc, top8[:, 3:4], None, ALU.is_ge)
            ts(pen, gmask, -1.0, 1e9, ALU.add, ALU.mult)
            tt(g88(mk1), g88(bia), b88(gmask), ALU.mult)
            tt(g88(mk1), g88(mk1), b88(pen), ALU.add)
            S.op("dve", lambda e: e.max(out=top8.ap, in_=mk1.ap), reads=[mk1.buf], writes=[top8.buf])
            ts(sel, mk1, top8[:, 7:8], None, ALU.is_ge)
            tt(sel, sel, sco, ALU.mult)
            S.op("dve", lambda e: e.tensor_reduce(out=den.ap, in_=sel.ap, axis=AX.X, op=ALU.add),
                 reads=[sel.buf], writes=[den.buf])
            recip(den, den)
            ts(gates[:, t, 0:64], sel, den[:, 0:1], 2.5, ALU.mult, ALU.mult)
        scope_begin()
        wgu = [newT([128, KC, 512], BF16, f"wgu{i}") for i in range(2)]
        wd = [newT([128, 2, D], BF16, f"wd{i}") for i in range(2)]
        sgl = [newT([128, 512], BF16, name=f"sgl{i}") for i in range(2)]
        actT = [newT([128, 512], BF16, name=f"actT{i}") for i in range(2)]
        NE = NE_MOE
        NB_ = NT // 2
        items = [(e_, blk) for e_ in range(NE) for blk in range(NB_)]

        def load_expert(e_):
            wb = e_ % 2
            S.dma("pool", wgu[wb].ap[:, :, 0:256], weg[e_].rearrange("(kc p) f -> p kc f", p=128),
                  writes=[wgu[wb].buf], stream="w")
            S.dma("pool", wgu[wb].ap[:, :, 256:512], weu[e_].rearrange("(kc p) f -> p kc f", p=128),
                  writes=[wgu[wb].buf], stream="w")
            S.dma("pool", wd[wb].ap, wed[e_].rearrange("(c p) f -> p c f", p=128),
                  writes=[wd[wb].buf], stream="w")

        ugb = {}

        def UG(idx):
            e_, blk = items[idx]
            wb = e_ % 2
            tsl = slice(blk * 256, (blk + 1) * 256)
            bA, bB = nbank(), nbank()
            for ffc in range(2):
                mmg(bA[:, ffc * 256:(ffc + 1) * 256],
                    [(wgu[wb][:, kc, ffc * 128:(ffc + 1) * 128], h2T[:, kc, tsl]) for kc in range(KC)])
            for ffc in range(2):
                mmg(bB[:, ffc * 256:(ffc + 1) * 256],
                    [(wgu[wb][:, kc, 256 + ffc * 128:256 + (ffc + 1) * 128], h2T[:, kc, tsl]) for kc in range(KC)])
            ugb[idx] = (bA, bB)

        def MID(idx):
            bA, bB = ugb.pop(idx)
            i = idx % 2
            act(sgl[i], bA, AF.Silu)
            tt(actT[i], sgl[i], bB, ALU.mult)

        def DOWN(idx):
            e_, blk = items[idx]
            wb = e_ % 2
            i = idx % 2
            for t2 in range(2):
                tile_ = blk * 2 + t2
                for cb in range(2):
                    csl = slice(cb * 512, (cb + 1) * 512)
                    bC = nbank()
                    mmg(bC, [(actT[i][:, ffc * 256 + t2 * 128:ffc * 256 + t2 * 128 + 128], wd[wb][:, ffc, csl])
                             for ffc in range(2)])
                    if e_ == 0:
                        ts(acc[:, tile_, csl], bC, gates[:, tile_, e_:e_ + 1], None, ALU.mult)
                    else:
                        stt(acc[:, tile_, csl], bC, gates[:, tile_, e_:e_ + 1], acc[:, tile_, csl], ALU.mult, ALU.add)

        load_expert(0)
        if NE > 1:
            load_expert(1)
        prologue(0); prologue(1)
        UG(0)
        for idx in range(len(items)):
            e_, blk = items[idx]
            MID(idx)
            if idx + 1 < len(items):
                if items[idx + 1][0] == 0:
                    prologue(2 * items[idx + 1][1]); prologue(2 * items[idx + 1][1] + 1)
                UG(idx + 1)
            DOWN(idx)
            if blk == NB_ - 1 and e_ + 2 < NE:
                load_expert(e_ + 2)
        scope_end()
        fwb = load(fw_bc, [128, D])
        _yt1 = newT([128, D], name="ytile")
        ytile = [_yt1, _yt1]
        for t in range(NT):
            cond = 0 if t < 8 else 1
            i = t % 2
            S.dma("sp", gen.xt[i].ap, x1_d[t * 128:(t + 1) * 128, :], writes=[gen.xt[i].buf], stream="x")
            tt(ytile[i], acc[:, t, :], gateT[:, cond, 1, :], ALU.mult)
            tt(gen.xt[i], gen.xt[i], ytile[i], ALU.add)
            act(gen.junk, gen.xt[i], AF.Square, accum=gen.ssq[i])
            act(gen.rstd[i], gen.ssq[i], AF.Sqrt, scale=1.0 / D, bias=epsT[:, 0:1])
            recip(gen.rstd[i], gen.rstd[i])
            stt(ytile[i], gen.xt[i], gen.rstd[i][:, 0:1], fwb, ALU.mult, ALU.mult)
            S.dma("sp", o_y[t * 128:(t + 1) * 128, :], ytile[i].ap, reads=[ytile[i].buf], stream="o")
        scope_end()
        S.finish()
    return nc


_NC_CACHE = {}
_DBG = {}


def kernel(x_prompt, x_sample, c, cache_k, cache_v, state_ssm_re, state_ssm_im,
           c_ctx, w_ada, b_ada, norm1_w, w_in, ssm_lambda_re, ssm_lambda_im, ssm_log_dt,
           ssm_b_re, ssm_b_im, ssm_c_re, ssm_c_im, ssm_d, ssm_w_glu,
           diff_lambda_q, diff_lambda_k, diff_subln_w, w_out, norm2_w,
           w_router, router_bias, w_exp_gate, w_exp_up, w_exp_down,
           w_sh_gate, w_sh_up, w_sh_down, final_norm_w):
    f32 = np.float32
    A = lambda a: np.ascontiguousarray(np.asarray(a, dtype=f32))
    x_prompt = A(x_prompt); x_sample = A(x_sample); c = A(c); c_ctx = A(c_ctx)

    if "nc" not in _NC_CACHE:
        _NC_CACHE["nc"] = build_program()
    nc = _NC_CACHE["nc"]

    def fm(v):
        return np.ascontiguousarray(np.asarray(v, f32).reshape(KC, 128).T)

    jj = np.arange(128) // 16
    shared = {
        "w_ada": A(w_ada)[0],
        "b_ada_bc": np.ascontiguousarray(np.broadcast_to(A(b_ada)[0][None, :], (128, 6 * D))),
        "n1w": fm(A(norm1_w)[0]),
        "w_in": A(w_in)[0],
        "ident": np.eye(128, dtype=f32),
        "w_out": A(w_out)[0], "w_glu": A(ssm_w_glu)[0],
        "dfold_l": np.ascontiguousarray(np.tile(A(ssm_d)[0].reshape(32, 16).T, (8, 1))),
        "mle_l": (jj[:, None] <= jj[None, :]).astype(f32), "mge_l": (jj[:, None] >= jj[None, :]).astype(f32),
        "n2w": fm(A(norm2_w)[0]), "w_router": A(w_router)[0],
        "rbias_bc": np.ascontiguousarray(np.broadcast_to(A(router_bias)[0][None], (128, 64))),
        "fw_bc": np.ascontiguousarray(np.broadcast_to(A(final_norm_w)[None], (128, D))),
        "weg": np.concatenate([A(w_exp_gate)[0], A(w_sh_gate)], axis=0),
        "weu": np.concatenate([A(w_exp_up)[0], A(w_sh_up)], axis=0),
        "wed": np.concatenate([A(w_exp_down)[0], A(w_sh_down)], axis=0),
        "lq_bc": np.ascontiguousarray(np.broadcast_to(A(diff_lambda_q)[0][None], (128, 2, 64))),
        "lk_bc": np.ascontiguousarray(np.broadcast_to(A(diff_lambda_k)[0][None], (128, 2, 64))),
        "subw_bc": np.ascontiguousarray(np.broadcast_to(A(diff_subln_w)[0][None], (128, 128))),
    }
    def s5l(rev):
        ds = [1, 0] if rev else [0, 1]
        C = np.ascontiguousarray
        o = {}
        o["lam_re_l"] = C(A(ssm_lambda_re)[0][ds].transpose(0, 2, 1).reshape(128, 32))
        o["lam_im_l"] = C(A(ssm_lambda_im)[0][ds].transpose(0, 2, 1).reshape(128, 32))
        o["logdt_l"] = C(np.repeat(A(ssm_log_dt)[0][ds][:, None, :], 64, axis=1).reshape(128, 32))
        o["bre_l"] = C(A(ssm_b_re)[0][ds].transpose(0, 2, 1, 3).reshape(128, 32, 16))
        o["bim_l"] = C(A(ssm_b_im)[0][ds].transpose(0, 2, 1, 3).reshape(128, 32, 16))
        o["cre_l"] = C(A(ssm_c_re)[0][ds].transpose(0, 3, 1, 2).reshape(128, 32, 16))
        o["cim_l"] = C(A(ssm_c_im)[0][ds].transpose(0, 3, 1, 2).reshape(128, 32, 16))
        return o
    s5maps = [s5l(False), s5l(True)]
    in_maps = []
    for i in range(NCORES):
        b, rev = i // 2, (i % 2 == 1)
        xpi = x_prompt[4 * i:4 * i + 4]
        if rev:
            xpi = xpi[:, ::-1]
        cond2 = np.stack([c_ctx, c[b]], axis=0)
        condT = np.ascontiguousarray(cond2.reshape(2, KC, 128).transpose(2, 1, 0))
        m = dict(shared)
        m["xp"] = np.ascontiguousarray(xpi.reshape(NP_TOK, D))
        m["condT"] = condT
        m.update(s5maps[i % 2])
        xsb = x_sample[b]
        idx = np.arange(4096)
        if rev:
            xsb = xsb[::-1]
            idx = idx[::-1]
        m["xs_all"] = np.ascontiguousarray(xsb)
        rc = np.stack([(idx // 64).astype(f32), (idx % 64).astype(f32)], axis=-1)
        m["pos_rc"] = np.ascontiguousarray(rc.reshape(32, 128, 2).transpose(1, 0, 2))
        m["fidx_bc"] = np.ascontiguousarray(np.broadcast_to(np.arange(16, dtype=f32)[None], (128, 16)))
        m["ck_in"] = np.ascontiguousarray(A(cache_k)[b, 0].reshape(512, 512))
        m["cv_in"] = np.ascontiguousarray(A(cache_v)[b, 0].reshape(512, 512))
        ds_ = [1, 0] if rev else [0, 1]
        hr = A(state_ssm_re)[b, 0][ds_].transpose(0, 2, 1).reshape(128, 32)
        hi = A(state_ssm_im)[b, 0][ds_].transpose(0, 2, 1).reshape(128, 32)
        m["h0_l"] = np.ascontiguousarray(np.stack([hr, hi], axis=1))
        in_maps.append(m)

    res = run_bass_kernel_spmd(nc, in_maps, core_ids=list(range(NCORES)))
    R = res.results

    y_prompt = np.zeros((32, 256, D), f32)
    y_sample = np.zeros((4, 4096, D), f32)
    new_ck = np.zeros((32, 1, 256, 4, 128), f32)
    new_cv = np.zeros((32, 1, 256, 4, 128), f32)
    new_re = np.zeros((32, 1, 2, 32, 64), f32)
    new_im = np.zeros((32, 1, 2, 32, 64), f32)
    for i in range(NCORES):
        rev = (i % 2 == 1)
        ck = np.asarray(R[i]["o_ck"]).reshape(4, 256, 4, 128)
        cv = np.asarray(R[i]["o_cv"]).reshape(4, 256, 4, 128)
        if rev:
            ck, cv = ck[:, ::-1], cv[:, ::-1]
        new_ck[4 * i:4 * i + 4, 0] = ck
        new_cv[4 * i:4 * i + 4, 0] = cv
        stt_ = np.asarray(R[i]["o_st"]).reshape(4, 2, 32, 2, 64)
        if rev:
            stt_ = stt_[:, :, :, ::-1]
        new_re[4 * i:4 * i + 4, 0] = stt_[:, 0].transpose(0, 2, 1, 3)
        new_im[4 * i:4 * i + 4, 0] = stt_[:, 1].transpose(0, 2, 1, 3)
        yy = np.asarray(R[i]["o_y"])
        yp = yy[:NP_TOK].reshape(4, 256, D)
        if rev:
            yp = yp[:, ::-1]
        y_prompt[4 * i:4 * i + 4] = yp
        ys = yy[NP_TOK:]
        if rev:
            y_sample[i // 2, 2048:] = ys[::-1]
        else:
            y_sample[i // 2, :2048] = ys
    return (y_prompt, y_sample, new_ck, new_cv, new_re, new_im)
```

```python
import contextlib
import os
STAGE = int(os.environ.get('KSTAGE', '9'))
import numpy as np
import ml_dtypes
import concourse.bass as bass
import concourse.mybir as mybir
from concourse.bass_utils import run_bass_kernel_spmd

F32 = mybir.dt.float32
BF16 = mybir.dt.bfloat16
ALU = mybir.AluOpType
AF = mybir.ActivationFunctionType
AX = mybir.AxisListType

NCORES = 8
D = 1024
KC = 8
NP_TOK = 1024
NS_TOK = 2048
NT_MOE = int(os.environ.get("NT_MOE", "24"))
NE_MOE = int(os.environ.get("NE_MOE", "65"))
EPS = 1e-6


class Tk:
    __slots__ = ("sem", "val", "eng")

    def __init__(self, sem, val, eng):
        self.sem, self.val, self.eng = sem, val, eng


class Buf:
    __slots__ = ("name", "w", "r")

    def __init__(self, name=""):
        self.name, self.w, self.r = name, None, {}


class Sched:
    def __init__(self, nc, stack):
        self.nc = nc
        self.engs = {"pe": nc.tensor, "act": nc.scalar, "dve": nc.vector,
                     "pool": nc.gpsimd, "sp": nc.sync}
        self.sem = {}
        self.cnt = {}
        for k in ("pe", "act", "dve", "pool"):
            self.sem[k] = stack.enter_context(nc.semaphore("c_" + k))
            self.cnt[k] = 0
        self.seen = {k: {} for k in self.engs}
        self.dsem = {}
        self.dcnt = {}
        self.bsem = {}
        self.free = {"sw": [], "hw": []}
        self.kind = {}
        self.stack = stack

    def _wait(self, e, tk):
        if tk is None:
            return
        if tk.eng == "pe" and e == "pe":
            return
        key = tk.sem.num
        if self.seen[e].get(key, 0) >= tk.val:
            return
        self.engs[e].wait_ge(tk.sem, tk.val)
        self.seen[e][key] = tk.val

    def _deps(self, e, reads, writes):
        need = {}

        def add(tk):
            if tk is None or (tk.eng == "pe" and e == "pe"):
                return
            k = tk.sem.num
            if k not in need or need[k].val < tk.val:
                need[k] = tk
        for b in reads:
            add(b.w)
        for b in writes:
            add(b.w)
            for t in b.r.values():
                add(t)
        for tk in need.values():
            self._wait(e, tk)

    def _mark(self, tk, reads, writes):
        for b in reads:
            b.r[tk.sem.num] = tk
        for b in writes:
            b.w = tk
            b.r = {}

    dead = False
    nops = 0
    KCUT = int(os.environ.get('KCUT', '100000000'))

    def op(self, e, fn, reads=(), writes=()):
        Sched.nops += 1
        if self.dead or Sched.nops > Sched.KCUT:
            return
        self._deps(e, reads, writes)
        ins = fn(self.engs[e])
        self.cnt[e] += 1
        ins.then_inc(self.sem[e], 1)
        self._mark(Tk(self.sem[e], self.cnt[e], e), reads, writes)

    def dma(self, q, out, in_, reads=(), writes=(), stream="ld", **kw):
        Sched.nops += 1
        if self.dead or Sched.nops > Sched.KCUT:
            return
        kind = "sw" if q == "pool" else "hw"
        key = (id(writes[0]) if writes else id(reads[0]), kind)
        if key not in self.bsem:
            if self.free[kind]:
                num = self.free[kind].pop()
            else:
                h = self.stack.enter_context(self.nc.semaphore("d%d" % len(self.dsem)))
                num = h.num
                self.dsem[num] = h
                self.dcnt[num] = 0
                self.kind[num] = kind
            self.bsem[key] = num
        num = self.bsem[key]
        self._deps(q, reads, writes)
        ins = self.engs[q].dma_start(out=out, in_=in_, **kw)
        self.dcnt[num] += 16
        ins.then_inc(self.dsem[num], 16)
        self._mark(Tk(self.dsem[num], self.dcnt[num], "dma"), reads, writes)

    def barrier(self):
        es = ("pe", "act", "dve", "pool", "sp")
        for e in es:
            for k in ("pe", "act", "dve", "pool"):
                if k != e and self.cnt[k]:
                    self._wait(e, Tk(self.sem[k], self.cnt[k], k))
            for s_, sem in self.dsem.items():
                if self.dcnt[s_]:
                    self._wait(e, Tk(sem, self.dcnt[s_], "dma"))
        self.free = {"sw": [n for n in self.dsem if self.kind[n] == "sw"],
                     "hw": [n for n in self.dsem if self.kind[n] == "hw"]}
        self.bsem = {}

    def finish(self):
        for s, sem in self.dsem.items():
            if self.dcnt[s]:
                self.engs["sp"].wait_ge(sem, self.dcnt[s])


def build_program():
    nc = bass.Bass("TRN2", target_bir_lowering=False)
    dt = nc.dram_tensor

    def din(name, shape, dtype=F32):
        return dt(name, list(shape), dtype, kind="ExternalInput").ap()

    def dout(name, shape, dtype=F32):
        return dt(name, list(shape), dtype, kind="ExternalOutput").ap()

    xp = din("xp", [NP_TOK, D])
    condT = din("condT", [128, KC, 2])
    w_ada = din("w_ada", [D, 6 * D])
    b_ada_bc = din("b_ada_bc", [128, 6 * D])
    n1w = din("n1w", [128, KC])
    w_in = din("w_in", [D, 2048])
    ident_in = din("ident", [128, 128])
    w_out_d = din("w_out", [D, D])
    w_glu_d = din("w_glu", [512, 512])
    lam_re_l = din("lam_re_l", [128, 32]); lam_im_l = din("lam_im_l", [128, 32]); logdt_l = din("logdt_l", [128, 32])
    bre_l = din("bre_l", [128, 32, 16]); bim_l = din("bim_l", [128, 32, 16])
    cre_l = din("cre_l", [128, 32, 16]); cim_l = din("cim_l", [128, 32, 16])
    dfold_l = din("dfold_l", [128, 32]); mle_l = din("mle_l", [128, 128]); mge_l = din("mge_l", [128, 128])
    xs_all = din("xs_all", [4096, D]); pos_rc = din("pos_rc", [128, 32, 2]); fidx_bc = din("fidx_bc", [128, 16])
    ck_in = din("ck_in", [512, 512]); cv_in = din("cv_in", [512, 512]); h0_l = din("h0_l", [128, 2, 32])
    n2w = din("n2w", [128, KC]); w_router_d = din("w_router", [D, 64]); rbias_bc = din("rbias_bc", [128, 64])
    fw_bc = din("fw_bc", [128, D])
    weg = din("weg", [65, D, 256]); weu = din("weu", [65, D, 256]); wed = din("wed", [65, 256, D])
    lq_bc = din("lq_bc", [128, 2, 64]); lk_bc = din("lk_bc", [128, 2, 64]); subw_bc = din("subw_bc", [128, 128])

    o_ck = dout("o_ck", [NP_TOK, 512])
    o_cv = dout("o_cv", [NP_TOK, 512])
    o_st = dout("o_st", [256, 128])
    o_y = dout("o_y", [NP_TOK + NS_TOK, D])
    x1_d = dt("x1_scratch", [NP_TOK + NS_TOK, D], F32, kind="Internal").ap()

    with contextlib.ExitStack() as st:
        S = Sched(nc, st)

        cur = [st]

        def sb(name, shape, dtype=F32):
            return cur[0].enter_context(nc.sbuf_tensor(name, list(shape), dtype))

        _stk = []

        def scope_begin():
            _stk.append(cur[0])
            cur[0] = contextlib.ExitStack()

        def scope_end():
            S.barrier()
            cur[0].close()
            cur[0] = _stk.pop()

        def psum(name, shape, dtype=F32):
            return st.enter_context(nc.psum_tensor(name, list(shape), dtype))

        banks = [psum(f"bank{i}", [128, 512], F32) for i in range(8)]
        bbuf = [Buf(f"bank{i}") for i in range(8)]

        ident_f = sb("ident_f", [128, 128]); b_ident_f = Buf()
        ident_b = sb("ident_b", [128, 128], BF16); b_ident_b = Buf()
        S.dma("sp", ident_f[:], ident_in, writes=[b_ident_f], stream="c")
        S.op("dve", lambda e: e.tensor_copy(out=ident_b[:], in_=ident_f[:]),
             reads=[b_ident_f], writes=[b_ident_b])

        eps_t = sb("eps_t", [128, 1]); b_eps = Buf()
        S.op("dve", lambda e: e.memset(eps_t[:], EPS), writes=[b_eps])
        n1w_sb = sb("n1w_sb", [128, KC]); b_n1w = Buf()
        S.dma("sp", n1w_sb[:], n1w, writes=[b_n1w], stream="c")

        w_in_v = w_in.rearrange("(kc p) n -> p kc n", p=128)

        gate_bc = sb("gate_bc", [128, 2, 2, D]); b_gate = Buf()
        modT = sb("modT", [128, 4, KC, 2]); b_modT = Buf()
        sc1 = sb("sc1", [128, KC, 2]); b_sc1 = Buf()
        scope_begin()
        cT = sb("cT", [128, KC, 2]); b_cT = Buf()
        S.dma("sp", cT[:], condT, writes=[b_cT], stream="c")
        sil = sb("sil", [128, KC, 2]); b_sil = Buf()
        S.op("act", lambda e: e.activation(out=sil[:], in_=cT[:], func=AF.Silu),
             reads=[b_cT], writes=[b_sil])
        silrep = sb("silrep", [128, 2, KC, 128], BF16); b_silrep = Buf()
        for r in range(2):
            S.op("dve", lambda e, r=r: e.tensor_copy(
                out=silrep[:, r, :, :],
                in_=sil[:, :, r:r + 1].to_broadcast([128, KC, 128])),
                reads=[b_sil], writes=[b_silrep])

        wada_sb = [sb(f"wada{i}", [128, KC, 512], BF16) for i in range(2)]
        b_wada = [Buf(), Buf()]
        bada_sb = [sb(f"bada{i}", [128, 512]) for i in range(2)]
        b_bada = [Buf(), Buf()]
        w_ada_v = w_ada.rearrange("(kc p) n -> p kc n", p=128)
        modrow = sb("modrow", [128, 512]); b_modrow = Buf()
        vec_of_chunk = {0: 0, 1: 1, 3: 2, 4: 3}
        gate_of_chunk = {2: 0, 5: 1}
        for cb in range(12):
            chunk, hf = cb // 2, cb % 2
            wb = cb % 2
            S.dma("pool", wada_sb[wb][:], w_ada_v[:, :, cb * 512:(cb + 1) * 512],
                  writes=[b_wada[wb]], stream="w")
            S.dma("sp", bada_sb[wb][:], b_ada_bc[:, cb * 512:(cb + 1) * 512],
                  writes=[b_bada[wb]], stream="c")
            for r in range(2):
                pb = (cb * 2 + r) % 2
                def mm(e, r=r, wb=wb, pb=pb):
                    for kc in range(KC):
                        ins = e.matmul(banks[pb][:, :], lhsT=silrep[:, r, kc, :],
                                       rhs=wada_sb[wb][:, kc, :],
                                       start=(kc == 0), stop=(kc == KC - 1))
                    return ins
                S.op("pe", mm, reads=[b_silrep, b_wada[wb]], writes=[bbuf[pb]])
                if chunk in gate_of_chunk:
                    g = gate_of_chunk[chunk]
                    S.op("dve", lambda e, r=r, g=g, hf=hf, pb=pb, wb=wb: e.tensor_tensor(
                        out=gate_bc[:, r, g, hf * 512:(hf + 1) * 512],
                        in0=banks[pb][:, :], in1=bada_sb[wb][:], op=ALU.add),
                        reads=[bbuf[pb], b_bada[wb]], writes=[b_gate])
                else:
                    v = vec_of_chunk[chunk]
                    S.op("dve", lambda e, pb=pb, wb=wb: e.tensor_tensor(
                        out=modrow[:], in0=banks[pb][:, :], in1=bada_sb[wb][:], op=ALU.add),
                        reads=[bbuf[pb], b_bada[wb]], writes=[b_modrow])
                    tb = 2 + (cb * 2 + r) % 2
                    def tp(e, tb=tb):
                        for j in range(4):
                            ins = e.transpose(out=banks[tb][:, j * 128:(j + 1) * 128],
                                              in_=modrow[:, j * 128:(j + 1) * 128],
                                              identity=ident_f[:])
                        return ins
                    S.op("pe", tp, reads=[b_modrow, b_ident_f], writes=[bbuf[tb]])
                    S.op("dve", lambda e, tb=tb, v=v, hf=hf, r=r: e.tensor_copy(
                        out=modT[:, v, hf * 4:(hf + 1) * 4, r],
                        in_=banks[tb][:, :].rearrange("p (j m) -> p j m", m=128)[:, :, 0]),
                        reads=[bbuf[tb]], writes=[b_modT])
        scope_end()
        n2w_sb = sb("n2w_sb", [128, KC]); b_n2w = Buf()
        S.dma("sp", n2w_sb[:], n2w, writes=[b_n2w], stream="c")
        sc2 = sb("sc2", [128, KC, 2]); b_sc2 = Buf()
        S.op("dve", lambda e: e.tensor_scalar(out=sc2[:], in0=modT[:, 3, :, :], scalar1=1.0,
                                              scalar2=None, op0=ALU.add),
             reads=[b_modT], writes=[b_sc2])
        S.op("dve", lambda e: e.tensor_tensor(out=sc2[:], in0=sc2[:],
                                              in1=n2w_sb[:, :].unsqueeze(2).to_broadcast([128, KC, 2]),
                                              op=ALU.mult),
             reads=[b_sc2, b_n2w], writes=[b_sc2])
        S.op("dve", lambda e: e.tensor_scalar(out=sc1[:], in0=modT[:, 1, :, :], scalar1=1.0,
                                              scalar2=None, op0=ALU.add),
             reads=[b_modT], writes=[b_sc1])
        S.op("dve", lambda e: e.tensor_tensor(out=sc1[:], in0=sc1[:],
                                              in1=n1w_sb[:, :].unsqueeze(2).to_broadcast([128, KC, 2]),
                                              op=ALU.mult),
             reads=[b_sc1, b_n1w], writes=[b_sc1])

        class TT:
            def __init__(s, ap, buf):
                s.ap, s.buf = ap, buf
            def __getitem__(s, k):
                return TT(s.ap[k], s.buf)
            def v(s, fn):
                return TT(fn(s.ap), s.buf)

        _cnt = [0]
        def newT(shape, dtype=F32, name=None):
            _cnt[0] += 1
            t = sb(f"{name or 't'}_{_cnt[0]}", shape, dtype)
            return TT(t[tuple(slice(None) for _ in shape)], Buf())

        def _rb(*xs):
            return [x.buf for x in xs if isinstance(x, TT)]
        def _a(x):
            return x.ap if isinstance(x, TT) else x
        def tt(o, a, b, op, eng="dve"):
            S.op(eng, lambda e: e.tensor_tensor(out=o.ap, in0=a.ap, in1=b.ap, op=op),
                 reads=_rb(a, b), writes=[o.buf])
        def ts(o, a, s1, s2, op0, op1=None, eng="dve"):
            if op1 is None:
                S.op(eng, lambda e: e.tensor_scalar(out=o.ap, in0=a.ap, scalar1=_a(s1), scalar2=None, op0=op0),
                     reads=_rb(a, s1), writes=[o.buf])
            else:
                S.op(eng, lambda e: e.tensor_scalar(out=o.ap, in0=a.ap, scalar1=_a(s1), scalar2=_a(s2),
                                                    op0=op0, op1=op1),
                     reads=_rb(a, s1, s2), writes=[o.buf])
        def stt(o, a, sc, b, op0, op1, eng="dve"):
            S.op(eng, lambda e: e.scalar_tensor_tensor(out=o.ap, in0=a.ap, scalar=_a(sc), in1=b.ap, op0=op0, op1=op1),
                 reads=_rb(a, sc, b), writes=[o.buf])
        def cp(o, a, eng="dve"):
            if eng == "act":
                S.op("act", lambda e: e.copy(out=o.ap, in_=a.ap), reads=[a.buf], writes=[o.buf])
            else:
                S.op(eng, lambda e: e.tensor_copy(out=o.ap, in_=a.ap), reads=[a.buf], writes=[o.buf])
        def act(o, a, func, scale=1.0, bias=None, accum=None):
            kw = {}
            if bias is not None:
                kw["bias"] = _a(bias)
            if accum is not None:
                kw["accum_out"] = accum.ap
            S.op("act", lambda e: e.activation(out=o.ap, in_=a.ap, func=func, scale=_a(scale), **kw),
                 reads=_rb(a, scale, bias), writes=[o.buf] + ([accum.buf] if accum is not None else []))
        def memset(o, val, eng="dve"):
            S.op(eng, lambda e: e.memset(o.ap, val), writes=[o.buf])
        def recip(o, a):
            S.op("dve", lambda e: e.reciprocal(out=o.ap, in_=a.ap), reads=[a.buf], writes=[o.buf])
        def mmg(o, pairs):
            def f(e):
                n = len(pairs)
                for i, (l, r) in enumerate(pairs):
                    ins = e.matmul(o.ap, lhsT=l.ap, rhs=r.ap, start=(i == 0), stop=(i == n - 1))
                return ins
            rd = []
            for l, r in pairs:
                rd += [l.buf, r.buf]
            S.op("pe", f, reads=rd, writes=[o.buf])
        def tpose(o, a, idt):
            S.op("pe", lambda e: e.transpose(out=o.ap, in_=a.ap, identity=idt.ap),
                 reads=[a.buf, idt.buf], writes=[o.buf])
        def load(dram_ap, shape, dtype=F32, q="sp", stream="c"):
            t = newT(shape, dtype)
            S.dma(q, t.ap, dram_ap, writes=[t.buf], stream=stream)
            return t
        _bk = [0]
        def nbank():
            _bk[0] = (_bk[0] + 1) % 8
            i = _bk[0]
            return TT(banks[i][:, :], bbuf[i])
        def bf(bank):
            return TT(bank.ap.bitcast(BF16), bank.buf)
        _ae = [0]
        def evac_eng():
            _ae[0] ^= 1
            return "act" if _ae[0] else "dve"

        identF = TT(ident_f[:], b_ident_f); identB = TT(ident_b[:], b_ident_b)
        epsT = TT(eps_t[:], b_eps)
        sc2T = TT(sc2[:], b_sc2)
        sc1T = TT(sc1[:], b_sc1); modTT = TT(modT[:], b_modT); gateT = TT(gate_bc[:], b_gate)
        def load_win():
            w = newT([128, KC, 2048], BF16, "w_in_sb")
            for kc in range(KC):
                S.dma("pool", w.ap[:, kc, :], w_in_v[:, kc, :], writes=[w.buf], stream="w")
            return w

        scope_begin()
        wout_v = w_out_d.rearrange("(kc p) n -> p kc n", p=128)
        wglu_sb = newT([128, 4, 512], BF16, "wglu_sb")
        S.dma("pool", wglu_sb.ap, w_glu_d.rearrange("(kc p) n -> p kc n", p=128), writes=[wglu_sb.buf], stream="w")

        def g128(x, g):
            return x.v(lambda a: a[:, g, :, :].rearrange("p j h -> p (j h)"))

        def build_s5_consts():
            Qr = newT([128, 32, 8, 16], BF16, "Qr"); QiN = newT([128, 32, 8, 16], BF16, "QiN")
            Mt = newT([128, 32, 128], BF16, "Mt")
            PT = newT([128, 32, 2, 128], BF16, "PT")
            A1 = newT([128, 2, 32]); A2 = newT([128, 2, 32])
            scope_begin()
            lamre = load(lam_re_l, [128, 32]); lamim = load(lam_im_l, [128, 32]); logdt = load(logdt_l, [128, 32])
            Bre = load(bre_l, [128, 32, 16]); Bim = load(bim_l, [128, 32, 16])
            Cre = load(cre_l, [128, 32, 16]); Cim = load(cim_l, [128, 32, 16])
            Dfold = load(dfold_l, [128, 32]); mle = load(mle_l, [128, 128]); mge = load(mge_l, [128, 128])
            halfpi = newT([128, 1]); memset(halfpi, float(np.pi / 2))

            def s32():
                return newT([128, 32])
            def cmul(a, b):
                (ar_, ai_), (br_, bi_) = a, b
                t1, t2, t3, t4, cr, ci = s32(), s32(), s32(), s32(), s32(), s32()
                tt(t1, ar_, br_, ALU.mult); tt(t2, ai_, bi_, ALU.mult); tt(cr, t1, t2, ALU.subtract)
                tt(t3, ar_, bi_, ALU.mult); tt(t4, ai_, br_, ALU.mult); tt(ci, t3, t4, ALU.add)
                return (cr, ci)

            dtt = s32(); act(dtt, logdt, AF.Exp)
            ar = s32(); ai = s32(); tt(ar, lamre, dtt, ALU.mult); tt(ai, lamim, dtt, ALU.mult)
            mag = s32(); act(mag, ar, AF.Exp, scale=1.0 / 32)
            sn = s32(); act(sn, ai, AF.Sin, scale=1.0 / 32)
            cs = s32(); act(cs, ai, AF.Sin, scale=1.0 / 32, bias=halfpi)
            zr = s32(); zi = s32(); tt(zr, mag, cs, ALU.mult); tt(zi, mag, sn, ALU.mult)
            z = (zr, zi)
            for _ in range(5):
                z = cmul(z, z)
            lb = z
            one = s32(); zero = s32(); memset(one, 1.0); memset(zero, 0.0)
            pw = [(one, zero), lb]
            for e_ in range(2, 9):
                pw.append(cmul(pw[-1], lb))
            m2 = s32(); t_ = s32(); tt(m2, lb[0], lb[0], ALU.mult); tt(t_, lb[1], lb[1], ALU.mult)
            tt(m2, m2, t_, ALU.add); rinv = s32(); recip(rinv, m2)
            ilr = s32(); ili = s32(); tt(ilr, lb[0], rinv, ALU.mult); tt(ili, lb[1], rinv, ALU.mult)
            ts(ili, ili, -1.0, None, ALU.mult)
            ilb = (ilr, ili)
            ipw = [(one, zero), ilb]
            for e_ in range(2, 8):
                ipw.append(cmul(ipw[-1], ilb))
            nr = s32(); ts(nr, lb[0], -1.0, None, ALU.add)
            den = s32(); t2_ = s32(); tt(den, lamre, lamre, ALU.mult); tt(t2_, lamim, lamim, ALU.mult)
            tt(den, den, t2_, ALU.add); rden = s32(); recip(rden, den)
            t5 = s32(); t6 = s32(); kr = s32(); ki = s32()
            tt(t5, nr, lamre, ALU.mult); tt(t6, lb[1], lamim, ALU.mult); tt(kr, t5, t6, ALU.add)
            tt(t5, lb[1], lamre, ALU.mult); tt(t6, nr, lamim, ALU.mult); tt(ki, t5, t6, ALU.subtract)
            tt(kr, kr, rden, ALU.mult); tt(ki, ki, rden, ALU.mult)
            selPr = newT([128, 32, 8]); selPi = newT([128, 32, 8]); selQr = newT([128, 32, 8]); selQi = newT([128, 32, 8])
            for j in range(8):
                for (dst, src, comp) in ((selPr, pw, 0), (selPi, pw, 1), (selQr, ipw, 0), (selQi, ipw, 1)):
                    cp(dst[0:64, :, j], src[7 - j][comp][0:64, :])
                    cp(dst[64:128, :, j], src[j][comp][64:128, :])
            def bc8(x):
                return x.v(lambda a: a.unsqueeze(2).to_broadcast([128, 32, 8]))
            wr = newT([128, 32, 8]); wi = newT([128, 32, 8]); ta = newT([128, 32, 8]); tb_ = newT([128, 32, 8])
            tt(ta, selPr, bc8(kr), ALU.mult); tt(tb_, selPi, bc8(ki), ALU.mult); tt(wr, ta, tb_, ALU.subtract)
            tt(ta, selPr, bc8(ki), ALU.mult); tt(tb_, selPi, bc8(kr), ALU.mult); tt(wi, ta, tb_, ALU.add)
            def bj(x):
                return x.v(lambda a: a.unsqueeze(3).to_broadcast([128, 32, 8, 16]))
            def bh(x):
                return x.v(lambda a: a.unsqueeze(2).to_broadcast([128, 32, 8, 16]))
            big1 = newT([128, 32, 8, 16]); big2 = newT([128, 32, 8, 16])
            Pr = newT([128, 32, 8, 16], BF16, "Pr"); Pi = newT([128, 32, 8, 16], BF16, "Pi")
            tt(big1, bj(wr), bh(Bre), ALU.mult); tt(big2, bj(wi), bh(Bim), ALU.mult); tt(Pr, big1, big2, ALU.subtract)
            tt(big1, bj(wr), bh(Bim), ALU.mult); tt(big2, bj(wi), bh(Bre), ALU.mult); tt(Pi, big1, big2, ALU.add)
            tt(big1, bj(selQr), bh(Cre), ALU.mult); tt(big2, bj(selQi), bh(Cim), ALU.mult); tt(Qr, big1, big2, ALU.subtract)
            tt(big1, bj(selQr), bh(Cim), ALU.mult); tt(big2, bj(selQi), bh(Cre), ALU.mult); tt(big1, big1, big2, ALU.add)
            ts(QiN, big1, -1.0, None, ALU.mult)
            def g128(x, g):
                return x.v(lambda a: a[:, g, :, :].rearrange("p j h -> p (j h)"))
            mtmp = newT([128, 4, 128])
            qa = newT([128, 32, 8, 16], BF16); qb = newT([128, 32, 8, 16], BF16)
            for d_ in range(2):
                cp(qa, Qr); cp(qb, QiN, eng="act")
                z = slice(64, 128) if d_ == 0 else slice(0, 64)
                memset(qa[z], 0.0); memset(qb[z], 0.0)
                msk = (mle if d_ == 0 else mge).v(lambda a: a.unsqueeze(1).to_broadcast([128, 4, 128]))
                for g0 in range(0, 32, 4):
                    bk = nbank()
                    for gg in range(4):
                        g = g0 + gg
                        mmg(bk[:, gg * 128:(gg + 1) * 128], [(g128(Pr, g), g128(qa, g)), (g128(Pi, g), g128(qb, g))])
                    bv = bk.v(lambda a: a.rearrange("p (g c) -> p g c", g=4))
                    tt(mtmp, bv, msk, ALU.mult)
                    for gg in range(4):
                        g = g0 + gg
                        if d_ == 0:
                            stt(Mt[:, g, :], identF, Dfold[:, g:g + 1], mtmp[:, gg, :], ALU.mult, ALU.add)
                        else:
                            tt(Mt[:, g, :], Mt[:, g, :], mtmp[:, gg, :], ALU.add)
            for g0 in range(0, 32, 4):
                bk = bf(nbank())
                for gg in range(4):
                    for ri, src in enumerate((Pr, Pi)):
                        col = (gg * 2 + ri) * 128
                        tpose(bk[:, col:col + 128], g128(src, g0 + gg), identB)
                cp(PT[:, g0:g0 + 4, :, :], bk.v(lambda a: a.rearrange("p (g r c) -> p g r c", g=4, r=2)), eng=evac_eng())
            cp(A1[:, 0, :], pw[8][0]); cp(A1[:, 1, :], pw[8][0])
            ts(A2[:, 0, :], pw[8][1], -1.0, None, ALU.mult); cp(A2[:, 1, :], pw[8][1])
            scope_end()

            return Qr, QiN, Mt, PT, A1, A2

        print("NOPS at end of consts", Sched.nops)
        if STAGE < 1:
            S.dead = True
        class _Gen:
            pass
        gen = _Gen()

        def alloc_gen(kv=True):
            gen.xt = [newT([128, D]) for i in range(2)]
            gen.xs = [newT([128, D], BF16) for i in range(2)]
            gen.junk = newT([128, D], BF16)
            gen.ssq = [newT([128, 1]) for i in range(2)]
            gen.rstd = [newT([128, 1]) for i in range(2)]
            if kv == "one":
                _kv2 = newT([128, 1024])
                gen.kv_sb = [_kv2, _kv2]
            elif kv:
                gen.kv_sb = [newT([128, 1024]) for i in range(2)]
            else:
                _kv1 = newT([128, 512])
                gen.kv_sb = [_kv1, _kv1]
        _nt = [0]

        def norm_tile(x_in, hT_out, scT, shT):
            _nt[0] += 1
            i = _nt[0] % 2
            act(gen.junk, x_in, AF.Square, accum=gen.ssq[i])
            act(gen.rstd[i], gen.ssq[i], AF.Sqrt, scale=1.0 / D, bias=epsT[:, 0:1])
            recip(gen.rstd[i], gen.rstd[i])
            ts(gen.xs[i], x_in, gen.rstd[i][:, 0:1], None, ALU.mult)
            bk = bf(nbank())
            for kc in range(KC):
                tpose(bk[:, kc * 128:(kc + 1) * 128], gen.xs[i][:, kc * 128:(kc + 1) * 128], identB)
            for kc in range(KC):
                act(hT_out[:, kc, :], bk[:, kc * 128:(kc + 1) * 128], AF.Identity,
                    scale=scT[:, kc:kc + 1], bias=shT[:, kc:kc + 1])

        lq = load(lq_bc, [128, 2, 64]); lk = load(lk_bc, [128, 2, 64])
        lprod = newT([128, 2, 64]); tt(lprod, lq, lk, ALU.mult)
        lsum = newT([128, 2])
        S.op("dve", lambda e: e.tensor_reduce(out=lsum.ap, in_=lprod.ap, axis=AX.X, op=ALU.add),
             reads=[lprod.buf], writes=[lsum.buf])
        lexp = newT([128, 2]); act(lexp, lsum, AF.Exp)
        lamneg = newT([128, 1])
        tt(lamneg, lexp[:, 1:2], lexp[:, 0:1], ALU.subtract)
        ts(lamneg, lamneg, -0.2, None, ALU.add)
        subw = load(subw_bc, [128, 128]); ts(subw, subw, 0.8, None, ALU.mult)

        catS = newT([128, KC, NS_TOK], BF16, "catS")
        pos = load(pos_rc, [128, 32, 2]); fidx = load(fidx_bc, [128, 16])
        freq = newT([128, 16]); act(freq, fidx, AF.Exp, scale=-float(np.log(10000.0)) / 16)
        scope_begin()
        Qr, QiN, Mt, PT, A1, A2 = build_s5_consts()
        scope_begin()
        Uf = newT([128, 32, 128], BF16, "Uf")
        yf = newT([128, 32, 128], BF16, "yf")
        qT = newT([128, 4, NP_TOK], BF16, "qT"); kT = newT([128, 4, NP_TOK], BF16, "kT")
        Vaug = newT([128, 8, 4, 129], BF16, "Vaug"); memset(Vaug, 1.0)
        scope_begin()
        winT = load_win()
        hT = newT([128, KC, NP_TOK], BF16, "hT")
        scope_begin()
        alloc_gen(kv="one")
        _qk1 = newT([128, 1024], BF16, name="qkbf")
        qk_bf = [_qk1, _qk1]
        for t in range(8):
            tok0 = t * 128
            i = t % 2
            S.dma("sp", gen.xt[i].ap, xp[tok0:tok0 + 128, :], writes=[gen.xt[i].buf], stream="x")
            norm_tile(gen.xt[i], hT[:, :, tok0:tok0 + 128], sc1T[:, :, 0], modTT[:, 0, :, 0])
            bq, bkk, bv_ = nbank(), nbank(), nbank()
            for bnk, c0 in ((bq, 512), (bkk, 1024), (bv_, 1536)):
                mmg(bnk, [(hT[:, kc, tok0:tok0 + 128], winT[:, kc, c0:c0 + 512]) for kc in range(KC)])
            cp(gen.kv_sb[i][:, 0:512], bkk, eng="act"); cp(gen.kv_sb[i][:, 512:1024], bv_, eng="dve")
            S.dma("sp", o_ck[tok0:tok0 + 128, :], gen.kv_sb[i].ap[:, 0:512], reads=[gen.kv_sb[i].buf], stream="o")
            S.dma("sp", o_cv[tok0:tok0 + 128, :], gen.kv_sb[i].ap[:, 512:1024], reads=[gen.kv_sb[i].buf], stream="o")
            cp(qk_bf[i][:, 0:512], bq, eng="act"); cp(qk_bf[i][:, 512:1024], gen.kv_sb[i][:, 0:512], eng="dve")
            cp(Vaug[:, t, :, 0:128], gen.kv_sb[i][:, 512:1024].v(lambda a: a.rearrange("p (h e) -> p h e", h=4)), eng="dve")
            bt = bf(nbank())
            for k8 in range(8):
                tpose(bt[:, k8 * 128:(k8 + 1) * 128], qk_bf[i][:, k8 * 128:(k8 + 1) * 128], identB)
            cp(qT[:, :, tok0:tok0 + 128], bt[:, 0:512].v(lambda a: a.rearrange("p (h c) -> p h c", h=4)), eng="act")
            cp(kT[:, :, tok0:tok0 + 128], bt[:, 512:1024].v(lambda a: a.rearrange("p (h c) -> p h c", h=4)), eng="act")

        scope_end()
        hT_cj = hT.v(lambda a: a.rearrange("p k (c j) -> p k c j", j=8))
        u_cm = newT([128, 32, 8, 16], BF16, "u_cm")
        for j in range(8):
            bk = nbank()
            mmg(bk, [(hT_cj[:, kc, :, j], winT[:, kc, 0:512]) for kc in range(KC)])
            cp(u_cm[:, :, j, :], bk.v(lambda a: a.rearrange("p (g h) -> p g h", h=16)), eng=evac_eng())
        for g0 in range(0, 32, 8):
            bk = bf(nbank())
            for gg in range(8):
                g = g0 + gg
                tpose(bk[:, gg * 128:(gg + 1) * 128], g128(u_cm, g), identB)
            cp(Uf[:, g0:g0 + 8, :], bk.v(lambda a: a.rearrange("p (g c) -> p g c", g=8)), eng=evac_eng())
        scope_end()
        scope_begin()
        Sp = newT([128, 128, 2, 32], BF16, "Sp")
        if STAGE < 2:
            S.dead = True
        for g0 in range(0, 32, 2):
            bk = nbank()
            for gg in range(2):
                for ri in range(2):
                    col = (gg * 2 + ri) * 128
                    mmg(bk[:, col:col + 128], [(PT[:, g0 + gg, ri, :], Uf[:, g0 + gg, :])])
            cp(Sp[:, :, :, g0:g0 + 2], bk.v(lambda a: a.rearrange("p (g r c) -> p c r g", g=2, r=2)), eng=evac_eng())
        if STAGE < 3:
            S.dead = True
        Gs = newT([128, 4, 2, 32], F32, "Gs"); memset(Gs, 0.0)
        Tt_ = newT([128, 4, 2, 32], F32, "Tt"); X1 = newT([128, 4, 2, 32]); X2 = newT([128, 4, 2, 32])
        Gst = newT([128, 2, 32, 128], BF16, "Gst")
        Sp_sl = Sp.v(lambda a: a.rearrange("p (s l) r g -> p s l r g", l=32))
        Gst_sl = Gst.v(lambda a: a.rearrange("p r g (s l) -> p s r g l", l=32))
        A1b = A1.v(lambda a: a.unsqueeze(1).to_broadcast([128, 4, 2, 32]))
        for step in range(32):
            for half, l in ((slice(0, 64), step), (slice(64, 128), 31 - step)):
                cp(Gst_sl[half, :, :, :, l], Gs[half])
                tt(Tt_[half], Gs[half], Sp_sl[half, :, l, :, :], ALU.add)
                tt(X1[half], Tt_[half], A1b[half], ALU.mult)
                for ri in range(2):
                    tt(X2[half, :, ri, :], Tt_[half, :, 1 - ri, :],
                       A2.v(lambda a: a[:, ri, :].unsqueeze(1).to_broadcast([128, 4, 32]))[half], ALU.mult)
                tt(Gs[half], X1[half], X2[half], ALU.add)
        if STAGE < 4:
            S.dead = True
        hfin = newT([128, 2, 128], F32, "hfin")
        Tt_flat = Tt_.v(lambda a: a.rearrange("p s r g -> p (s r g)"))
        bk = nbank()
        for k2 in range(2):
            tpose(bk[:, k2 * 128:(k2 + 1) * 128], Tt_flat[:, k2 * 128:(k2 + 1) * 128], identF)
        cp(hfin, bk[:, 0:256].v(lambda a: a.rearrange("p (k c) -> p k c", k=2)))
        S.dma("sp", o_st.rearrange("(k p) c -> p k c", p=128), hfin.ap, reads=[hfin.buf], stream="o")
        for g0 in range(0, 32, 4):
            bk = nbank()
            for gg in range(4):
                g = g0 + gg
                mmg(bk[:, gg * 128:(gg + 1) * 128],
                    [(Mt[:, g, :], Uf[:, g, :]), (g128(Qr, g), Gst[:, 0, g, :]), (g128(QiN, g), Gst[:, 1, g, :])])
            cp(yf[:, g0:g0 + 4, :], bk.v(lambda a: a.rearrange("p (g c) -> p g c", g=4)), eng=evac_eng())
        scope_end()
        catT = newT([128, KC, NP_TOK], BF16, "catT")
        scope_begin()
        y_cm = newT([128, 8, 512], F32, "y_cm")
        for g0 in range(0, 32, 8):
            bk = bf(nbank())
            for gg in range(8):
                tpose(bk[:, gg * 128:(gg + 1) * 128], yf[:, g0 + gg, :], identB)
            cp(y_cm[:, :, 16 * g0:16 * g0 + 128].v(lambda a: a.rearrange("p i (g h) -> p g i h", g=8)),
               bk.v(lambda a: a.rearrange("p (g i h) -> p g i h", g=8, i=8)), eng=evac_eng())
        if STAGE < 5:
            S.dead = True
        g_cm = newT([128, 8, 512], BF16, "g_cm")
        gt1 = newT([128, 4, 512]); gt2 = newT([128, 4, 512])
        for hf in range(2):
            ysl = y_cm[:, hf * 4:(hf + 1) * 4, :]
            act(gt1, ysl, AF.Square)
            ts(gt1, gt1, 0.044715, 1.0, ALU.mult, ALU.add)
            tt(gt1, gt1, ysl, ALU.mult)
            act(gt2, gt1, AF.Sigmoid, scale=1.5957691216057308)
            tt(g_cm[:, hf * 4:(hf + 1) * 4, :], ysl, gt2, ALU.mult)
        gT = newT([128, 4, NP_TOK], BF16, "gT")
        gT_v = gT.v(lambda a: a.rearrange("p k (c j) -> p k c j", j=8))
        for i0 in range(0, 8, 2):
            bk = bf(nbank())
            for ii in range(2):
                for k4 in range(4):
                    col = (ii * 4 + k4) * 128
                    tpose(bk[:, col:col + 128], g_cm[:, i0 + ii, k4 * 128:(k4 + 1) * 128], identB)
            for ii in range(2):
                cp(gT_v[:, :, :, i0 + ii],
                   bk[:, ii * 512:(ii + 1) * 512].v(lambda a: a.rearrange("p (k c) -> p k c", k=4)), eng="act")
        sg = [newT([128, 512], name=f"sg{i}") for i in range(2)]
        for m4 in range(4):
            for tb2 in range(2):
                bk = nbank()
                tsl = slice(tb2 * 512, (tb2 + 1) * 512)
                mmg(bk, [(wglu_sb[:, k4, m4 * 128:(m4 + 1) * 128], gT[:, k4, tsl]) for k4 in range(4)])
                act(sg[tb2], bk, AF.Sigmoid)
                tt(catT[:, m4, tsl], gT[:, m4, tsl], sg[tb2], ALU.mult)
        scope_end()

        scope_begin()
        kTm = [newT([128, 4, NP_TOK], BF16, f"kTm{m}") for m in range(2)]
        for m in range(2):
            cp(kTm[m], kT, eng=("act" if m else "dve"))
            z = slice(64, 128) if m == 0 else slice(0, 64)
            memset(kTm[m][z], 0.0)
        PTs = [newT([128, 512], BF16, name=f"PTs{m}") for m in range(2)]
        o_tok = [newT([128, 4, 128], name=f"otok{q}") for q in range(2)]
        rr = newT([128, 2]); sq = newT([128, 4, 128]); ss4 = newT([128, 4]); on = newT([128, 4, 128], BF16)
        for s_ in range(4):
            for hd in range(4):
                for m in range(2):
                    bk = nbank()
                    for kt in range(2):
                        k0 = s_ * 256 + kt * 128
                        mmg(bk[:, kt * 256:(kt + 1) * 256],
                            [(kTm[m][:, hd, k0:k0 + 128], qT[:, hd, s_ * 256:(s_ + 1) * 256])])
                    act(PTs[m], bk, AF.Exp, scale=0.125)
                for qt in range(2):
                    bk = nbank()
                    for m in range(2):
                        mmg(bk[:, m * 129:(m + 1) * 129],
                            [(PTs[m][:, kt * 256 + qt * 128:kt * 256 + qt * 128 + 128], Vaug[:, 2 * s_ + kt, hd, :])
                             for kt in range(2)])
                    recip(rr[:, 0:1], bk[:, 128:129]); recip(rr[:, 1:2], bk[:, 257:258])
                    tt(rr[:, 1:2], rr[:, 1:2], lamneg, ALU.mult)
                    ts(o_tok[qt][:, hd, :], bk[:, 0:128], rr[:, 0:1], None, ALU.mult)
                    stt(o_tok[qt][:, hd, :], bk[:, 129:257], rr[:, 1:2], o_tok[qt][:, hd, :], ALU.mult, ALU.add)
            for qt in range(2):
                tok0 = s_ * 256 + qt * 128
                tt(sq, o_tok[qt], o_tok[qt], ALU.mult)
                S.op("dve", lambda e: e.tensor_reduce(out=ss4.ap, in_=sq.ap, axis=AX.X, op=ALU.add),
                     reads=[sq.buf], writes=[ss4.buf])
                act(ss4, ss4, AF.Sqrt, scale=1.0 / 128, bias=epsT[:, 0:1])
                recip(ss4, ss4)
                tt(sq, o_tok[qt], ss4.v(lambda a: a.unsqueeze(2).to_broadcast([128, 4, 128])), ALU.mult)
                tt(on, sq, subw.v(lambda a: a.unsqueeze(1).to_broadcast([128, 4, 128])), ALU.mult)
                bk = bf(nbank())
                for hd in range(4):
                    tpose(bk[:, hd * 128:(hd + 1) * 128], on[:, hd, :], identB)
                cp(catT[:, 4:8, tok0:tok0 + 128], bk[:, 0:512].v(lambda a: a.rearrange("p (h c) -> p h c", h=4)), eng="act")
        scope_end()

        scope_begin()
        alloc_gen()
        wout_sb = newT([128, KC, D], BF16, "wout_sb")
        for kc in range(KC):
            S.dma("pool", wout_sb.ap[:, kc, :], wout_v[:, kc, :], writes=[wout_sb.buf], stream="w")
        x1t = [newT([128, D], name=f"x1t{i}") for i in range(2)]
        wtmp = newT([128, 512])
        for t in range(8):
            tok0 = t * 128
            i = t % 2
            S.dma("sp", gen.xt[i].ap, xp[tok0:tok0 + 128, :], writes=[gen.xt[i].buf], stream="x")
            for cb in range(2):
                csl = slice(cb * 512, (cb + 1) * 512)
                bk = nbank()
                mmg(bk, [(catT[:, kc, tok0:tok0 + 128], wout_sb[:, kc, csl]) for kc in range(KC)])
                tt(wtmp, bk, gateT[:, 0, 0, csl], ALU.mult)
                tt(x1t[i][:, csl], wtmp, gen.xt[i][:, csl], ALU.add)
            S.dma("sp", x1_d[tok0:tok0 + 128, :], x1t[i].ap, reads=[x1t[i].buf], stream="o")
        S.dead = False
        scope_end()
        scope_end()
        if STAGE < 6:
            S.dead = True
        H0, H1 = slice(0, 64), slice(64, 128)

        yf2 = newT([128, 2, 32, 128], BF16, "yf2")
        scope_begin()
        UfA = newT([128, 32, 512], BF16, "UfA")
        scope_begin()
        alloc_gen()
        winu = newT([128, KC, 512], BF16, "winu")
        for kc in range(KC):
            S.dma("pool", winu.ap[:, kc, :], w_in_v[:, kc, 0:512], writes=[winu.buf], stream="w")
        hTb = newT([128, KC, 1024], BF16, "hTb"); u_cm = newT([128, 32, 8, 16], BF16, "u_cm_s")
        hTb_cj = hTb.v(lambda a: a.rearrange("p k (c j) -> p k c j", j=8))
        for blk in range(4):
            for t in range(8):
                i = t % 2
                r0 = blk * 1024 + t * 128
                S.dma("sp", gen.xt[i].ap, xs_all[r0:r0 + 128, :], writes=[gen.xt[i].buf], stream="x")
                norm_tile(gen.xt[i], hTb[:, :, t * 128:(t + 1) * 128], sc1T[:, :, 1], modTT[:, 0, :, 1])
            for j in range(8):
                bk = nbank()
                mmg(bk, [(hTb_cj[:, kc, :, j], winu[:, kc, :]) for kc in range(KC)])
                cp(u_cm[:, :, j, :], bk.v(lambda a: a.rearrange("p (g h) -> p g h", h=16)), eng=evac_eng())
            for g0 in range(0, 32, 8):
                bk = bf(nbank())
                for gg in range(8):
                    tpose(bk[:, gg * 128:(gg + 1) * 128], g128(u_cm, g0 + gg), identB)
                cp(UfA[:, g0:g0 + 8, blk * 128:(blk + 1) * 128],
                   bk.v(lambda a: a.rearrange("p (g c) -> p g c", g=8)), eng="act")
        scope_end()
        GstO = newT([128, 2, 32, 256], BF16, "GstO")
        SpA = newT([128, 64, 2, 32], BF16, "SpA"); SpB = newT([128, 64, 2, 32], BF16, "SpB")
        h0 = load(h0_l, [128, 2, 32])
        Gs = newT([128, 2, 32]); Tt2 = newT([128, 2, 32]); X1s = newT([128, 2, 32]); X2s = newT([128, 2, 32])

        def compute_Sp(dst, c0):
            for g0 in range(0, 32, 4):
                bk = nbank()
                for gg in range(4):
                    for ri in range(2):
                        col = (gg * 2 + ri) * 64
                        mmg(bk[:, col:col + 64], [(PT[:, g0 + gg, ri, :], UfA[:, g0 + gg, c0:c0 + 64])])
                cp(dst[:, :, :, g0:g0 + 4], bk.v(lambda a: a.rearrange("p (g r c) -> p c r g", g=4, r=2)), eng=evac_eng())

        X1p = newT([128, 2, 32]); Tt2p = newT([128, 2, 32]); Gsp = newT([128, 2, 32])
        X2dT = newT([128, 2, 32]); X2qT = newT([128, 2, 32])
        X2d = [TT(X2dT.ap[:, ri, :], Buf()) for ri in range(2)]
        X2q = [TT(X2qT.ap[:, ri, :], Buf()) for ri in range(2)]

        def a8mul(dst, src, half, eng="dve"):
            xa, xb, xw = (X1s, X2d, X2dT) if eng == "dve" else (X1p, X2q, X2qT)
            tt(xa[half], src[half], A1[half], ALU.mult, eng=eng)
            for ri in range(2):
                tt(xb[ri][half], src[half, 1 - ri, :], A2[half, ri, :], ALU.mult, eng=eng)
            S.op(eng, lambda e: e.tensor_tensor(out=dst[half].ap, in0=xa[half].ap, in1=xw[half].ap, op=ALU.add),
                 reads=[xa.buf, xb[0].buf, xb[1].buf], writes=[dst.buf])

        a8mul(Gsp, h0, H0, eng="pool"); a8mul(Gs, h0, H1)
        for k in range(256):
            if k % 64 == 0:
                compute_Sp(SpA, k); compute_Sp(SpB, 448 - k)
            cp(GstO[H0, :, :, k], Gsp[H0], eng="act")
            tt(Tt2p[H0], Gsp[H0], SpA[H0, k % 64, :, :], ALU.add, eng="pool")
            a8mul(Gsp, Tt2p, H0, eng="pool")
            tt(Tt2[H1], Gs[H1], SpB[H1, 63 - k % 64, :, :], ALU.add)
            a8mul(Gs, Tt2, H1)
        for k in range(256, 512):
            cb_ = 511 - k
            if k % 64 == 0:
                compute_Sp(SpB, 448 - k)
            cp(GstO[H1, :, :, cb_], Gs[H1], eng="act")
            tt(Tt2[H1], Gs[H1], SpB[H1, 63 - k % 64, :, :], ALU.add)
            a8mul(Gs, Tt2, H1)
        for blk in range(2):
            csl = slice(blk * 128, (blk + 1) * 128)
            for g0 in range(0, 32, 4):
                bk = nbank()
                for gg in range(4):
                    g = g0 + gg
                    mmg(bk[:, gg * 128:(gg + 1) * 128],
                        [(Mt[:, g, :], UfA[:, g, csl]), (g128(Qr, g), GstO[:, 0, g, csl]), (g128(QiN, g), GstO[:, 1, g, csl])])
                cp(yf2[:, blk, g0:g0 + 4, :], bk.v(lambda a: a.rearrange("p (g c) -> p g c", g=4)), eng=evac_eng())
        scope_end()
        scope_begin()
        y_cm = newT([128, 8, 512], F32, "y_cm_s"); g_cm = newT([128, 8, 512], BF16, "g_cm_s")
        gt1 = newT([128, 4, 512]); gt2 = newT([128, 4, 512])
        gT = newT([128, 4, 1024], BF16, "gT_s")
        gT_v = gT.v(lambda a: a.rearrange("p k (c j) -> p k c j", j=8))
        sg = [newT([128, 512]) for i in range(2)]
        for blk in range(2):
            for g0 in range(0, 32, 8):
                bk = bf(nbank())
                for gg in range(8):
                    tpose(bk[:, gg * 128:(gg + 1) * 128], yf2[:, blk, g0 + gg, :], identB)
                cp(y_cm[:, :, 16 * g0:16 * g0 + 128].v(lambda a: a.rearrange("p i (g h) -> p g i h", g=8)),
                   bk.v(lambda a: a.rearrange("p (g i h) -> p g i h", g=8, i=8)), eng="act")
            for hf in range(2):
                ysl = y_cm[:, hf * 4:(hf + 1) * 4, :]
                act(gt1, ysl, AF.Square)
                ts(gt1, gt1, 0.044715, 1.0, ALU.mult, ALU.add)
                tt(gt1, gt1, ysl, ALU.mult)
                act(gt2, gt1, AF.Sigmoid, scale=1.5957691216057308)
                tt(g_cm[:, hf * 4:(hf + 1) * 4, :], ysl, gt2, ALU.mult)
            for i0 in range(0, 8, 2):
                bk = bf(nbank())
                for ii in range(2):
                    for k4 in range(4):
                        col = (ii * 4 + k4) * 128
                        tpose(bk[:, col:col + 128], g_cm[:, i0 + ii, k4 * 128:(k4 + 1) * 128], identB)
                for ii in range(2):
                    cp(gT_v[:, :, :, i0 + ii],
                       bk[:, ii * 512:(ii + 1) * 512].v(lambda a: a.rearrange("p (k c) -> p k c", k=4)), eng="act")
            for m4 in range(4):
                for tb2 in range(2):
                    bk = nbank()
                    tsl = slice(tb2 * 512, (tb2 + 1) * 512)
                    osl = slice(blk * 1024 + tb2 * 512, blk * 1024 + (tb2 + 1) * 512)
                    mmg(bk, [(wglu_sb[:, k4, m4 * 128:(m4 + 1) * 128], gT[:, k4, tsl]) for k4 in range(4)])
                    act(sg[tb2], bk, AF.Sigmoid)
                    tt(catS[:, m4, osl], gT[:, m4, tsl], sg[tb2], ALU.mult)
        scope_end()
        scope_end()

        if STAGE < 7:
            S.dead = True
        scope_begin()
        kTa = newT([128, 4, 4608], BF16, "kTa"); Vs = newT([128, 36, 4, 129], BF16, "Vs"); memset(Vs, 1.0)
        qTs = newT([128, 4, NS_TOK], BF16, "qTs")
        scope_begin()
        alloc_gen(kv=False)
        winq = newT([128, KC, 1536], BF16, "winq")
        for kc in range(KC):
            S.dma("pool", winq.ap[:, kc, :], w_in_v[:, kc, 512:2048], writes=[winq.buf], stream="w")
        hTt2 = [newT([128, KC, 128], BF16, "hTt") for _ in range(2)]
        qkf2 = [newT([128, 2, 512], F32, "qkf") for _ in range(2)]
        qkb2 = [newT([128, 2, 512], BF16, "qkb") for _ in range(2)]
        ang4 = newT([128, 2, 2, 16]); kf4 = newT([128, 2, 2, 16])
        SC2 = [newT([128, 2, 2, 16]) for _ in range(2)]
        r1 = newT([128, 16, 2, 16]); r2 = newT([128, 16, 2, 16])
        MAGIC = 12582912.0
        TWO_PI = float(2 * np.pi)
        vbank = {}

        def stA(tile):
            own = tile < 16
            hTt, qkf, qkb = hTt2[tile % 2], qkf2[tile % 2], qkb2[tile % 2]
            if tile < 32:
                i = tile % 2
                S.dma("sp", gen.xt[i].ap, xs_all[tile * 128:(tile + 1) * 128, :], writes=[gen.xt[i].buf], stream="x")
                SC = SC2[tile % 2]
                tt(ang4[:, 0, :, :], pos[:, tile, :].v(lambda a: a.unsqueeze(2).to_broadcast([128, 2, 16])),
                   freq.v(lambda a: a.unsqueeze(1).to_broadcast([128, 2, 16])), ALU.mult)
                ts(ang4[:, 1, :, :], ang4[:, 0, :, :], float(np.pi / 2), None, ALU.add)
                ts(kf4, ang4, 1.0 / TWO_PI, MAGIC, ALU.mult, ALU.add)
                ts(kf4, kf4, -MAGIC, None, ALU.add)
                stt(kf4, kf4, -TWO_PI, ang4, ALU.mult, ALU.add)
                ts(kf4, kf4, -3.1415925, 3.1415925, ALU.max, ALU.min)
                act(SC, kf4, AF.Sin)
                norm_tile(gen.xt[i], hTt, sc1T[:, :, 1], modTT[:, 0, :, 1])
                bkk, bv_ = nbank(), nbank()
                mmg(bkk, [(hTt[:, kc, :], winq[:, kc, 512:1024]) for kc in range(KC)])
                mmg(bv_, [(hTt[:, kc, :], winq[:, kc, 1024:1536]) for kc in range(KC)])
                cp(qkf[:, 1, :], bkk, eng="act")
                vbank[tile] = bv_
                if own:
                    bq = nbank()
                    mmg(bq, [(hTt[:, kc, :], winq[:, kc, 0:512]) for kc in range(KC)])
                    cp(qkf[:, 0, :], bq, eng="act")
            else:
                ct = tile - 32
                S.dma("sp", qkf.ap[:, 1, :], ck_in[ct * 128:(ct + 1) * 128, :], writes=[qkf.buf], stream="x")
                S.dma("sp", gen.xt[tile % 2].ap[:, 0:512], cv_in[ct * 128:(ct + 1) * 128, :], writes=[gen.xt[tile % 2].buf], stream="x")

        def stB(tile):
            own = tile < 16
            hTt, qkf, qkb = hTt2[tile % 2], qkf2[tile % 2], qkb2[tile % 2]
            if tile < 32:
                SC = SC2[tile % 2]
                cp(Vs[:, tile, :, 0:128], vbank.pop(tile).v(lambda a: a.rearrange("p (h e) -> p h e", h=4)), eng="dve")
                a0 = 0 if own else 1
                na = 2 - a0
                xv = qkf[:, a0:2, :].v(lambda a: a.rearrange("p a (b r x f) -> p (a b) r x f", r=2, x=2, f=16))
                ov = qkb[:, a0:2, :].v(lambda a: a.rearrange("p a (b r x f) -> p (a b) r x f", r=2, x=2, f=16))
                A_ = na * 8
                COS = SC[:, 1, :, :].v(lambda a: a.unsqueeze(1).to_broadcast([128, A_, 2, 16]))
                SIN = SC[:, 0, :, :].v(lambda a: a.unsqueeze(1).to_broadcast([128, A_, 2, 16]))
                x1 = xv[:, :, :, 0, :]; x2 = xv[:, :, :, 1, :]
                tt(r1[:, 0:A_], x1, COS, ALU.mult); tt(r2[:, 0:A_], x2, SIN, ALU.mult)
                tt(ov[:, :, :, 0, :], r1[:, 0:A_], r2[:, 0:A_], ALU.subtract)
                tt(r1[:, 0:A_], x2, COS, ALU.mult); tt(r2[:, 0:A_], x1, SIN, ALU.mult)
                tt(ov[:, :, :, 1, :], r1[:, 0:A_], r2[:, 0:A_], ALU.add)
            else:
                cp(qkb[:, 1, :], qkf[:, 1, :])
                cp(Vs[:, tile, :, 0:128], gen.xt[tile % 2][:, 0:512].v(lambda a: a.rearrange("p (h e) -> p h e", h=4)))
            bt = bf(nbank())
            for k8 in range(4 if not own else 8):
                src = qkb[:, 1, (k8 % 4) * 128:(k8 % 4 + 1) * 128] if k8 < 4 else qkb[:, 0, (k8 - 4) * 128:(k8 - 3) * 128]
                tpose(bt[:, k8 * 128:(k8 + 1) * 128], src, identB)
            cp(kTa[:, :, tile * 128:(tile + 1) * 128], bt[:, 0:512].v(lambda a: a.rearrange("p (h c) -> p h c", h=4)), eng="act")
            if own:
                cp(qTs[:, :, tile * 128:(tile + 1) * 128], bt[:, 512:1024].v(lambda a: a.rearrange("p (h c) -> p h c", h=4)), eng="act")

        stA(0)
        for tile in range(36):
            if tile + 1 < 36:
                stA(tile + 1)
            stB(tile)
        scope_end()
        qTm = [newT([128, 512], BF16, name=f"qTm{m}") for m in range(2)]
        PTs = [newT([128, 512], BF16, name=f"PTss{i}") for i in range(4)]
        Osb = [newT([128, 4, 129], name=f"Osb{m}") for m in range(2)]
        o_tok = newT([128, 4, 4, 128], F32, "o_tok_s")
        rr = newT([128, 2]); sq = newT([128, 4, 128]); ss4 = newT([128, 4]); on = newT([128, 4, 128], BF16)
        obank = [TT(banks[i][:, :], bbuf[i]) for i in range(4)]
        _sbk = [0]
        def sbank():
            _sbk[0] = (_sbk[0] + 1) % 4
            i = 4 + _sbk[0]
            return TT(banks[i][:, :], bbuf[i])
        _pt = [0]
        for qb in range(4):
            for hd in range(4):
                for m in range(2):
                    cp(qTm[m], qTs[:, hd, qb * 512:(qb + 1) * 512], eng=("act" if m else "dve"))
                    z = H1 if m == 0 else H0
                    memset(qTm[m][z], 0.0)
                    sbks = {}
                    Pbuf = {}
                    for it in range(36 + 3):
                        if it < 36:
                            kt = it
                            sbks[kt] = sbank()
                            mmg(sbks[kt], [(kTa[:, hd, kt * 128:(kt + 1) * 128], qTm[m])])
                        if 0 <= it - 2 < 36:
                            kt = it - 2
                            _pt[0] = (_pt[0] + 1) % 4
                            Pbuf[kt] = PTs[_pt[0]]
                            act(Pbuf[kt], sbks[kt], AF.Exp, scale=0.125)
                        if 0 <= it - 3 < 36:
                            kt = it - 3
                            P_ = Pbuf[kt]
                            for qt in range(4):
                                S.op("pe", lambda e, qt=qt, P_=P_, kt=kt: e.matmul(
                                    obank[qt].ap[:, 0:129], lhsT=P_.ap[:, qt * 128:(qt + 1) * 128],
                                    rhs=Vs.ap[:, kt, hd, :], start=(kt == 0), stop=(kt == 35)),
                                    reads=[P_.buf, Vs.buf], writes=[obank[qt].buf])
                    for qt in range(4):
                        cp(Osb[m][:, qt, :], obank[qt][:, 0:129], eng=("act" if qt % 2 else "dve"))
                for qt in range(4):
                    recip(rr[:, 0:1], Osb[0][:, qt, 128:129]); recip(rr[:, 1:2], Osb[1][:, qt, 128:129])
                    tt(rr[:, 1:2], rr[:, 1:2], lamneg, ALU.mult)
                    ts(o_tok[:, qt, hd, :], Osb[0][:, qt, 0:128], rr[:, 0:1], None, ALU.mult)
                    stt(o_tok[:, qt, hd, :], Osb[1][:, qt, 0:128], rr[:, 1:2], o_tok[:, qt, hd, :], ALU.mult, ALU.add)
            for qt in range(4):
                tok0 = qb * 512 + qt * 128
                ot = o_tok[:, qt, :, :]
                tt(sq, ot, ot, ALU.mult)
                S.op("dve", lambda e: e.tensor_reduce(out=ss4.ap, in_=sq.ap, axis=AX.X, op=ALU.add),
                     reads=[sq.buf], writes=[ss4.buf])
                act(ss4, ss4, AF.Sqrt, scale=1.0 / 128, bias=epsT[:, 0:1])
                recip(ss4, ss4)
                tt(sq, ot, ss4.v(lambda a: a.unsqueeze(2).to_broadcast([128, 4, 128])), ALU.mult)
                tt(on, sq, subw.v(lambda a: a.unsqueeze(1).to_broadcast([128, 4, 128])), ALU.mult)
                bk = TT(banks[4 + qt][:, :].bitcast(BF16), bbuf[4 + qt])
                for hd in range(4):
                    tpose(bk[:, hd * 128:(hd + 1) * 128], on[:, hd, :], identB)
                cp(catS[:, 4:8, tok0:tok0 + 128], bk[:, 0:512].v(lambda a: a.rearrange("p (h c) -> p h c", h=4)), eng="act")
        scope_end()

        scope_begin()
        alloc_gen()
        wout_sb = newT([128, KC, D], BF16, "wout_sb2")
        for kc in range(KC):
            S.dma("pool", wout_sb.ap[:, kc, :], wout_v[:, kc, :], writes=[wout_sb.buf], stream="w")
        x1t = [newT([128, D]) for i in range(2)]
        wtmp = newT([128, 512])
        for t in range(16):
            tok0 = t * 128
            i = t % 2
            S.dma("sp", gen.xt[i].ap, xs_all[tok0:tok0 + 128, :], writes=[gen.xt[i].buf], stream="x")
            for cb in range(2):
                csl = slice(cb * 512, (cb + 1) * 512)
                bk = nbank()
                mmg(bk, [(catS[:, kc, tok0:tok0 + 128], wout_sb[:, kc, csl]) for kc in range(KC)])
                tt(wtmp, bk, gateT[:, 1, 0, csl], ALU.mult)
                tt(x1t[i][:, csl], wtmp, gen.xt[i][:, csl], ALU.add)
            S.dma("sp", x1_d[NP_TOK + tok0:NP_TOK + tok0 + 128, :], x1t[i].ap, reads=[x1t[i].buf], stream="o")
        S.dead = False
        scope_end()
        scope_end()

        NT = NT_MOE
        scope_begin()
        h2T = newT([128, KC, NT * 128], BF16, "h2T")
        acc = newT([128, NT, D], F32, "acc")
        gates = newT([128, NT, 65], F32, "gates"); memset(gates, 1.0)
        _xt1 = newT([128, D])
        gen.xt = [_xt1, _xt1]
        _xs1 = newT([128, D], BF16)
        gen.xs = [_xs1, _xs1]
        gen.junk = newT([128, D], BF16)
        gen.ssq = [newT([128, 1]) for i in range(2)]
        gen.rstd = [newT([128, 1]) for i in range(2)]
        wr_sb = newT([128, KC, 64], BF16, "wr_sb")
        S.dma("pool", wr_sb.ap, w_router_d.rearrange("(kc p) n -> p kc n", p=128), writes=[wr_sb.buf], stream="w")
        rbias = load(rbias_bc, [128, 64])
        sco = newT([128, 64]); bia = newT([128, 64]); eq = newT([128, 64]); mk1 = newT([128, 64])
        gm1 = newT([128, 8]); gm2 = newT([128, 8]); gsc = newT([128, 8]); top8 = newT([128, 8])
        gmask = newT([128, 8]); pen = newT([128, 8]); sel = newT([128, 64]); den = newT([128, 1])
        def g88(x):
            return x.v(lambda a: a.rearrange("p (g e) -> p g e", e=8))
        def b88(x):
            return x.v(lambda a: a.unsqueeze(2).to_broadcast([128, 8, 8]))
        def prologue(t):
            cond = 0 if t < 8 else 1
            i = t % 2
            S.dma("sp", gen.xt[i].ap, x1_d[t * 128:(t + 1) * 128, :], writes=[gen.xt[i].buf], stream="x")
            norm_tile(gen.xt[i], h2T[:, :, t * 128:(t + 1) * 128], sc2T[:, :, cond], modTT[:, 2, :, cond])
            bk = nbank()
            mmg(bk[:, 0:64], [(h2T[:, kc, t * 128:(t + 1) * 128], wr_sb[:, kc, :]) for kc in range(KC)])
            act(sco, bk[:, 0:64], AF.Sigmoid)
            tt(bia, sco, rbias, ALU.add)
            S.op("dve", lambda e: e.tensor_reduce(out=gm1.ap, in_=g88(bia).ap, axis=AX.X, op=ALU.max),
                 reads=[bia.buf], writes=[gm1.buf])
            tt(g88(eq), g88(bia), b88(gm1), ALU.is_equal)
            stt(mk1, eq, -1e9, bia, ALU.mult, ALU.add)
            S.op("dve", lambda e: e.tensor_reduce(out=gm2.ap, in_=g88(mk1).ap, axis=AX.X, op=ALU.max),
                 reads=[mk1.buf], writes=[gm2.buf])
            tt(gsc, gm1, gm2, ALU.add)
            S.op("dve", lambda e: e.max(out=top8.ap, in_=gsc.ap), reads=[gsc.buf], writes=[top8.buf])
            ts(gmask, gsc, top8[:, 3:4], None, ALU.is_ge)
            ts(pen, gmask, -1.0, 1e9, ALU.add, ALU.mult)
            tt(g88(mk1), g88(bia), b88(gmask), ALU.mult)
            tt(g88(mk1), g88(mk1), b88(pen), ALU.add)
            S.op("dve", lambda e: e.max(out=top8.ap, in_=mk1.ap), reads=[mk1.buf], writes=[top8.buf])
            ts(sel, mk1, top8[:, 7:8], None, ALU.is_ge)
            tt(sel, sel, sco, ALU.mult)
            S.op("dve", lambda e: e.tensor_reduce(out=den.ap, in_=sel.ap, axis=AX.X, op=ALU.add),
                 reads=[sel.buf], writes=[den.buf])
            recip(den, den)
            ts(gates[:, t, 0:64], sel, den[:, 0:1], 2.5, ALU.mult, ALU.mult)
        scope_begin()
        wgu = [newT([128, KC, 512], BF16, f"wgu{i}") for i in range(2)]
        wd = [newT([128, 2, D], BF16, f"wd{i}") for i in range(2)]
        sgl = [newT([128, 512], BF16, name=f"sgl{i}") for i in range(2)]
        actT = [newT([128, 512], BF16, name=f"actT{i}") for i in range(2)]
        NE = NE_MOE
        NB_ = NT // 2
        items = [(e_, blk) for e_ in range(NE) for blk in range(NB_)]

        def load_expert(e_):
            wb = e_ % 2
            S.dma("pool", wgu[wb].ap[:, :, 0:256], weg[e_].rearrange("(kc p) f -> p kc f", p=128),
                  writes=[wgu[wb].buf], stream="w")
            S.dma("pool", wgu[wb].ap[:, :, 256:512], weu[e_].rearrange("(kc p) f -> p kc f", p=128),
                  writes=[wgu[wb].buf], stream="w")
            S.dma("pool", wd[wb].ap, wed[e_].rearrange("(c p) f -> p c f", p=128),
                  writes=[wd[wb].buf], stream="w")

        ugb = {}

        def UG(idx):
            e_, blk = items[idx]
            wb = e_ % 2
            tsl = slice(blk * 256, (blk + 1) * 256)
            bA, bB = nbank(), nbank()
            for ffc in range(2):
                mmg(bA[:, ffc * 256:(ffc + 1) * 256],
                    [(wgu[wb][:, kc, ffc * 128:(ffc + 1) * 128], h2T[:, kc, tsl]) for kc in range(KC)])
            for ffc in range(2):
                mmg(bB[:, ffc * 256:(ffc + 1) * 256],
                    [(wgu[wb][:, kc, 256 + ffc * 128:256 + (ffc + 1) * 128], h2T[:, kc, tsl]) for kc in range(KC)])
            ugb[idx] = (bA, bB)

        def MID(idx):
            bA, bB = ugb.pop(idx)
            i = idx % 2
            act(sgl[i], bA, AF.Silu)
            tt(actT[i], sgl[i], bB, ALU.mult)

        def DOWN(idx):
            e_, blk = items[idx]
            wb = e_ % 2
            i = idx % 2
            for t2 in range(2):
                tile_ = blk * 2 + t2
                for cb in range(2):
                    csl = slice(cb * 512, (cb + 1) * 512)
                    bC = nbank()
                    mmg(bC, [(actT[i][:, ffc * 256 + t2 * 128:ffc * 256 + t2 * 128 + 128], wd[wb][:, ffc, csl])
                             for ffc in range(2)])
                    if e_ == 0:
                        ts(acc[:, tile_, csl], bC, gates[:, tile_, e_:e_ + 1], None, ALU.mult)
                    else:
                        stt(acc[:, tile_, csl], bC, gates[:, tile_, e_:e_ + 1], acc[:, tile_, csl], ALU.mult, ALU.add)

        load_expert(0)
        if NE > 1:
            load_expert(1)
        prologue(0); prologue(1)
        UG(0)
        for idx in range(len(items)):
            e_, blk = items[idx]
            MID(idx)
            if idx + 1 < len(items):
                if items[idx + 1][0] == 0:
                    prologue(2 * items[idx + 1][1]); prologue(2 * items[idx + 1][1] + 1)
                UG(idx + 1)
            DOWN(idx)
            if blk == NB_ - 1 and e_ + 2 < NE:
                load_expert(e_ + 2)
        scope_end()
        fwb = load(fw_bc, [128, D])
        _yt1 = newT([128, D], name="ytile")
        ytile = [_yt1, _yt1]
        for t in range(NT):
            cond = 0 if t < 8 else 1
            i = t % 2
            S.dma("sp", gen.xt[i].ap, x1_d[t * 128:(t + 1) * 128, :], writes=[gen.xt[i].buf], stream="x")
            tt(ytile[i], acc[:, t, :], gateT[:, cond, 1, :], ALU.mult)
            tt(gen.xt[i], gen.xt[i], ytile[i], ALU.add)
            act(gen.junk, gen.xt[i], AF.Square, accum=gen.ssq[i])
            act(gen.rstd[i], gen.ssq[i], AF.Sqrt, scale=1.0 / D, bias=epsT[:, 0:1])
            recip(gen.rstd[i], gen.rstd[i])
            stt(ytile[i], gen.xt[i], gen.rstd[i][:, 0:1], fwb, ALU.mult, ALU.mult)
            S.dma("sp", o_y[t * 128:(t + 1) * 128, :], ytile[i].ap, reads=[ytile[i].buf], stream="o")
        scope_end()
        S.finish()
    return nc


_NC_CACHE = {}
_DBG = {}


def kernel(x_prompt, x_sample, c, cache_k, cache_v, state_ssm_re, state_ssm_im,
           c_ctx, w_ada, b_ada, norm1_w, w_in, ssm_lambda_re, ssm_lambda_im, ssm_log_dt,
           ssm_b_re, ssm_b_im, ssm_c_re, ssm_c_im, ssm_d, ssm_w_glu,
           diff_lambda_q, diff_lambda_k, diff_subln_w, w_out, norm2_w,
           w_router, router_bias, w_exp_gate, w_exp_up, w_exp_down,
           w_sh_gate, w_sh_up, w_sh_down, final_norm_w):
    f32 = np.float32
    A = lambda a: np.ascontiguousarray(np.asarray(a, dtype=f32))
    x_prompt = A(x_prompt); x_sample = A(x_sample); c = A(c); c_ctx = A(c_ctx)

    if "nc" not in _NC_CACHE:
        _NC_CACHE["nc"] = build_program()
    nc = _NC_CACHE["nc"]

    def fm(v):
        return np.ascontiguousarray(np.asarray(v, f32).reshape(KC, 128).T)

    jj = np.arange(128) // 16
    shared = {
        "w_ada": A(w_ada)[0],
        "b_ada_bc": np.ascontiguousarray(np.broadcast_to(A(b_ada)[0][None, :], (128, 6 * D))),
        "n1w": fm(A(norm1_w)[0]),
        "w_in": A(w_in)[0],
        "ident": np.eye(128, dtype=f32),
        "w_out": A(w_out)[0], "w_glu": A(ssm_w_glu)[0],
        "dfold_l": np.ascontiguousarray(np.tile(A(ssm_d)[0].reshape(32, 16).T, (8, 1))),
        "mle_l": (jj[:, None] <= jj[None, :]).astype(f32), "mge_l": (jj[:, None] >= jj[None, :]).astype(f32),
        "n2w": fm(A(norm2_w)[0]), "w_router": A(w_router)[0],
        "rbias_bc": np.ascontiguousarray(np.broadcast_to(A(router_bias)[0][None], (128, 64))),
        "fw_bc": np.ascontiguousarray(np.broadcast_to(A(final_norm_w)[None], (128, D))),
        "weg": np.concatenate([A(w_exp_gate)[0], A(w_sh_gate)], axis=0),
        "weu": np.concatenate([A(w_exp_up)[0], A(w_sh_up)], axis=0),
        "wed": np.concatenate([A(w_exp_down)[0], A(w_sh_down)], axis=0),
        "lq_bc": np.ascontiguousarray(np.broadcast_to(A(diff_lambda_q)[0][None], (128, 2, 64))),
        "lk_bc": np.ascontiguousarray(np.broadcast_to(A(diff_lambda_k)[0][None], (128, 2, 64))),
        "subw_bc": np.ascontiguousarray(np.broadcast_to(A(diff_subln_w)[0][None], (128, 128))),
    }
    def s5l(rev):
        ds = [1, 0] if rev else [0, 1]
        C = np.ascontiguousarray
        o = {}
        o["lam_re_l"] = C(A(ssm_lambda_re)[0][ds].transpose(0, 2, 1).reshape(128, 32))
        o["lam_im_l"] = C(A(ssm_lambda_im)[0][ds].transpose(0, 2, 1).reshape(128, 32))
        o["logdt_l"] = C(np.repeat(A(ssm_log_dt)[0][ds][:, None, :], 64, axis=1).reshape(128, 32))
        o["bre_l"] = C(A(ssm_b_re)[0][ds].transpose(0, 2, 1, 3).reshape(128, 32, 16))
        o["bim_l"] = C(A(ssm_b_im)[0][ds].transpose(0, 2, 1, 3).reshape(128, 32, 16))
        o["cre_l"] = C(A(ssm_c_re)[0][ds].transpose(0, 3, 1, 2).reshape(128, 32, 16))
        o["cim_l"] = C(A(ssm_c_im)[0][ds].transpose(0, 3, 1, 2).reshape(128, 32, 16))
        return o
    s5maps = [s5l(False), s5l(True)]
    in_maps = []
    for i in range(NCORES):
        b, rev = i // 2, (i % 2 == 1)
        xpi = x_prompt[4 * i:4 * i + 4]
        if rev:
            xpi = xpi[:, ::-1]
        cond2 = np.stack([c_ctx, c[b]], axis=0)
        condT = np.ascontiguousarray(cond2.reshape(2, KC, 128).transpose(2, 1, 0))
        m = dict(shared)
        m["xp"] = np.ascontiguousarray(xpi.reshape(NP_TOK, D))
        m["condT"] = condT
        m.update(s5maps[i % 2])
        xsb = x_sample[b]
        idx = np.arange(4096)
        if rev:
            xsb = xsb[::-1]
            idx = idx[::-1]
        m["xs_all"] = np.ascontiguousarray(xsb)
        rc = np.stack([(idx // 64).astype(f32), (idx % 64).astype(f32)], axis=-1)
        m["pos_rc"] = np.ascontiguousarray(rc.reshape(32, 128, 2).transpose(1, 0, 2))
        m["fidx_bc"] = np.ascontiguousarray(np.broadcast_to(np.arange(16, dtype=f32)[None], (128, 16)))
        m["ck_in"] = np.ascontiguousarray(A(cache_k)[b, 0].reshape(512, 512))
        m["cv_in"] = np.ascontiguousarray(A(cache_v)[b, 0].reshape(512, 512))
        ds_ = [1, 0] if rev else [0, 1]
        hr = A(state_ssm_re)[b, 0][ds_].transpose(0, 2, 1).reshape(128, 32)
        hi = A(state_ssm_im)[b, 0][ds_].transpose(0, 2, 1).reshape(128, 32)
        m["h0_l"] = np.ascontiguousarray(np.stack([hr, hi], axis=1))
        in_maps.append(m)

    res = run_bass_kernel_spmd(nc, in_maps, core_ids=list(range(NCORES)))
    R = res.results

    y_prompt = np.zeros((32, 256, D), f32)
    y_sample = np.zeros((4, 4096, D), f32)
    new_ck = np.zeros((32, 1, 256, 4, 128), f32)
    new_cv = np.zeros((32, 1, 256, 4, 128), f32)
    new_re = np.zeros((32, 1, 2, 32, 64), f32)
    new_im = np.zeros((32, 1, 2, 32, 64), f32)
    for i in range(NCORES):
        rev = (i % 2 == 1)
        ck = np.asarray(R[i]["o_ck"]).reshape(4, 256, 4, 128)
        cv = np.asarray(R[i]["o_cv"]).reshape(4, 256, 4, 128)
        if rev:
            ck, cv = ck[:, ::-1], cv[:, ::-1]
        new_ck[4 * i:4 * i + 4, 0] = ck
        new_cv[4 * i:4 * i + 4, 0] = cv
        stt_ = np.asarray(R[i]["o_st"]).reshape(4, 2, 32, 2, 64)
        if rev:
            stt_ = stt_[:, :, :, ::-1]
        new_re[4 * i:4 * i + 4, 0] = stt_[:, 0].transpose(0, 2, 1, 3)
        new_im[4 * i:4 * i + 4, 0] = stt_[:, 1].transpose(0, 2, 1, 3)
        yy = np.asarray(R[i]["o_y"])
        yp = yy[:NP_TOK].reshape(4, 256, D)
        if rev:
            yp = yp[:, ::-1]
        y_prompt[4 * i:4 * i + 4] = yp
        ys = yy[NP_TOK:]
        if rev:
            y_sample[i // 2, 2048:] = ys[::-1]
        else:
            y_sample[i // 2, :2048] = ys
    return (y_prompt, y_sample, new_ck, new_cv, new_re, new_im)
```

```python
import contextlib
import os
STAGE = int(os.environ.get('KSTAGE', '9'))
import numpy as np
import ml_dtypes
import concourse.bass as bass
import concourse.mybir as mybir
from concourse.bass_utils import run_bass_kernel_spmd

F32 = mybir.dt.float32
BF16 = mybir.dt.bfloat16
ALU = mybir.AluOpType
AF = mybir.ActivationFunctionType
AX = mybir.AxisListType

NCORES = 8
D = 1024
KC = 8
NP_TOK = 1024
NS_TOK = 2048
NT_MOE = int(os.environ.get("NT_MOE", "24"))
NE_MOE = int(os.environ.get("NE_MOE", "65"))
EPS = 1e-6


class Tk:
    __slots__ = ("sem", "val", "eng")

    def __init__(self, sem, val, eng):
        self.sem, self.val, self.eng = sem, val, eng


class Buf:
    __slots__ = ("name", "w", "r")

    def __init__(self, name=""):
        self.name, self.w, self.r = name, None, {}


class Sched:
    def __init__(self, nc, stack):
        self.nc = nc
        self.engs = {"pe": nc.tensor, "act": nc.scalar, "dve": nc.vector,
                     "pool": nc.gpsimd, "sp": nc.sync}
        self.sem = {}
        self.cnt = {}
        for k in ("pe", "act", "dve", "pool"):
            self.sem[k] = stack.enter_context(nc.semaphore("c_" + k))
            self.cnt[k] = 0
        self.seen = {k: {} for k in self.engs}
        self.dsem = {}
        self.dcnt = {}
        self.bsem = {}
        self.free = {"sw": [], "hw": []}
        self.kind = {}
        self.stack = stack

    def _wait(self, e, tk):
        if tk is None:
            return
        if tk.eng == "pe" and e == "pe":
            return
        key = tk.sem.num
        if self.seen[e].get(key, 0) >= tk.val:
            return
        self.engs[e].wait_ge(tk.sem, tk.val)
        self.seen[e][key] = tk.val

    def _deps(self, e, reads, writes):
        need = {}

        def add(tk):
            if tk is None or (tk.eng == "pe" and e == "pe"):
                return
            k = tk.sem.num
            if k not in need or need[k].val < tk.val:
                need[k] = tk
        for b in reads:
            add(b.w)
        for b in writes:
            add(b.w)
            for t in b.r.values():
                add(t)
        for tk in need.values():
            self._wait(e, tk)

    def _mark(self, tk, reads, writes):
        for b in reads:
            b.r[tk.sem.num] = tk
        for b in writes:
            b.w = tk
            b.r = {}

    dead = False
    nops = 0
    KCUT = int(os.environ.get('KCUT', '100000000'))

    def op(self, e, fn, reads=(), writes=()):
        Sched.nops += 1
        if self.dead or Sched.nops > Sched.KCUT:
            return
        self._deps(e, reads, writes)
        ins = fn(self.engs[e])
        self.cnt[e] += 1
        ins.then_inc(self.sem[e], 1)
        self._mark(Tk(self.sem[e], self.cnt[e], e), reads, writes)

    def dma(self, q, out, in_, reads=(), writes=(), stream="ld", **kw):
        Sched.nops += 1
        if self.dead or Sched.nops > Sched.KCUT:
            return
        kind = "sw" if q == "pool" else "hw"
        key = (id(writes[0]) if writes else id(reads[0]), kind)
        if key not in self.bsem:
            if self.free[kind]:
                num = self.free[kind].pop()
            else:
                h = self.stack.enter_context(self.nc.semaphore("d%d" % len(self.dsem)))
                num = h.num
                self.dsem[num] = h
                self.dcnt[num] = 0
                self.kind[num] = kind
            self.bsem[key] = num
        num = self.bsem[key]
        self._deps(q, reads, writes)
        ins = self.engs[q].dma_start(out=out, in_=in_, **kw)
        self.dcnt[num] += 16
        ins.then_inc(self.dsem[num], 16)
        self._mark(Tk(self.dsem[num], self.dcnt[num], "dma"), reads, writes)

    def barrier(self):
        es = ("pe", "act", "dve", "pool", "sp")
        for e in es:
            for k in ("pe", "act", "dve", "pool"):
                if k != e and self.cnt[k]:
                    self._wait(e, Tk(self.sem[k], self.cnt[k], k))
            for s_, sem in self.dsem.items():
                if self.dcnt[s_]:
                    self._wait(e, Tk(sem, self.dcnt[s_], "dma"))
        self.free = {"sw": [n for n in self.dsem if self.kind[n] == "sw"],
                     "hw": [n for n in self.dsem if self.kind[n] == "hw"]}
        self.bsem = {}

    def finish(self):
        for s, sem in self.dsem.items():
            if self.dcnt[s]:
                self.engs["sp"].wait_ge(sem, self.dcnt[s])


def build_program():
    nc = bass.Bass("TRN2", target_bir_lowering=False)
    dt = nc.dram_tensor

    def din(name, shape, dtype=F32):
        return dt(name, list(shape), dtype, kind="ExternalInput").ap()

    def dout(name, shape, dtype=F32):
        return dt(name, list(shape), dtype, kind="ExternalOutput").ap()

    xp = din("xp", [NP_TOK, D])
    condT = din("condT", [128, KC, 2])
    w_ada = din("w_ada", [D, 6 * D])
    b_ada_bc = din("b_ada_bc", [128, 6 * D])
    n1w = din("n1w", [128, KC])
    w_in = din("w_in", [D, 2048])
    ident_in = din("ident", [128, 128])
    w_out_d = din("w_out", [D, D])
    w_glu_d = din("w_glu", [512, 512])
    lam_re_l = din("lam_re_l", [128, 32]); lam_im_l = din("lam_im_l", [128, 32]); logdt_l = din("logdt_l", [128, 32])
    bre_l = din("bre_l", [128, 32, 16]); bim_l = din("bim_l", [128, 32, 16])
    cre_l = din("cre_l", [128, 32, 16]); cim_l = din("cim_l", [128, 32, 16])
    dfold_l = din("dfold_l", [128, 32]); mle_l = din("mle_l", [128, 128]); mge_l = din("mge_l", [128, 128])
    xs_all = din("xs_all", [4096, D]); pos_rc = din("pos_rc", [128, 32, 2]); fidx_bc = din("fidx_bc", [128, 16])
    ck_in = din("ck_in", [512, 512]); cv_in = din("cv_in", [512, 512]); h0_l = din("h0_l", [128, 2, 32])
    n2w = din("n2w", [128, KC]); w_router_d = din("w_router", [D, 64]); rbias_bc = din("rbias_bc", [128, 64])
    fw_bc = din("fw_bc", [128, D])
    weg = din("weg", [65, D, 256]); weu = din("weu", [65, D, 256]); wed = din("wed", [65, 256, D])
    lq_bc = din("lq_bc", [128, 2, 64]); lk_bc = din("lk_bc", [128, 2, 64]); subw_bc = din("subw_bc", [128, 128])

    o_ck = dout("o_ck", [NP_TOK, 512])
    o_cv = dout("o_cv", [NP_TOK, 512])
    o_st = dout("o_st", [256, 128])
    o_y = dout("o_y", [NP_TOK + NS_TOK, D])
    x1_d = dt("x1_scratch", [NP_TOK + NS_TOK, D], F32, kind="Internal").ap()

    with contextlib.ExitStack() as st:
        S = Sched(nc, st)

        cur = [st]

        def sb(name, shape, dtype=F32):
            return cur[0].enter_context(nc.sbuf_tensor(name, list(shape), dtype))

        _stk = []

        def scope_begin():
            _stk.append(cur[0])
            cur[0] = contextlib.ExitStack()

        def scope_end():
            S.barrier()
            cur[0].close()
            cur[0] = _stk.pop()

        def psum(name, shape, dtype=F32):
            return st.enter_context(nc.psum_tensor(name, list(shape), dtype))

        banks = [psum(f"bank{i}", [128, 512], F32) for i in range(8)]
        bbuf = [Buf(f"bank{i}") for i in range(8)]

        ident_f = sb("ident_f", [128, 128]); b_ident_f = Buf()
        ident_b = sb("ident_b", [128, 128], BF16); b_ident_b = Buf()
        S.dma("sp", ident_f[:], ident_in, writes=[b_ident_f], stream="c")
        S.op("dve", lambda e: e.tensor_copy(out=ident_b[:], in_=ident_f[:]),
             reads=[b_ident_f], writes=[b_ident_b])

        eps_t = sb("eps_t", [128, 1]); b_eps = Buf()
        S.op("dve", lambda e: e.memset(eps_t[:], EPS), writes=[b_eps])
        n1w_sb = sb("n1w_sb", [128, KC]); b_n1w = Buf()
        S.dma("sp", n1w_sb[:], n1w, writes=[b_n1w], stream="c")

        w_in_v = w_in.rearrange("(kc p) n -> p kc n", p=128)

        gate_bc = sb("gate_bc", [128, 2, 2, D]); b_gate = Buf()
        modT = sb("modT", [128, 4, KC, 2]); b_modT = Buf()
        sc1 = sb("sc1", [128, KC, 2]); b_sc1 = Buf()
        scope_begin()
        cT = sb("cT", [128, KC, 2]); b_cT = Buf()
        S.dma("sp", cT[:], condT, writes=[b_cT], stream="c")
        sil = sb("sil", [128, KC, 2]); b_sil = Buf()
        S.op("act", lambda e: e.activation(out=sil[:], in_=cT[:], func=AF.Silu),
             reads=[b_cT], writes=[b_sil])
        silrep = sb("silrep", [128, 2, KC, 128], BF16); b_silrep = Buf()
        for r in range(2):
            S.op("dve", lambda e, r=r: e.tensor_copy(
                out=silrep[:, r, :, :],
                in_=sil[:, :, r:r + 1].to_broadcast([128, KC, 128])),
                reads=[b_sil], writes=[b_silrep])

        wada_sb = [sb(f"wada{i}", [128, KC, 512], BF16) for i in range(2)]
        b_wada = [Buf(), Buf()]
        bada_sb = [sb(f"bada{i}", [128, 512]) for i in range(2)]
        b_bada = [Buf(), Buf()]
        w_ada_v = w_ada.rearrange("(kc p) n -> p kc n", p=128)
        modrow = sb("modrow", [128, 512]); b_modrow = Buf()
        vec_of_chunk = {0: 0, 1: 1, 3: 2, 4: 3}
        gate_of_chunk = {2: 0, 5: 1}
        for cb in range(12):
            chunk, hf = cb // 2, cb % 2
            wb = cb % 2
            S.dma("pool", wada_sb[wb][:], w_ada_v[:, :, cb * 512:(cb + 1) * 512],
                  writes=[b_wada[wb]], stream="w")
            S.dma("sp", bada_sb[wb][:], b_ada_bc[:, cb * 512:(cb + 1) * 512],
                  writes=[b_bada[wb]], stream="c")
            for r in range(2):
                pb = (cb * 2 + r) % 2
                def mm(e, r=r, wb=wb, pb=pb):
                    for kc in range(KC):
                        ins = e.matmul(banks[pb][:, :], lhsT=silrep[:, r, kc, :],
                                       rhs=wada_sb[wb][:, kc, :],
                                       start=(kc == 0), stop=(kc == KC - 1))
                    return ins
                S.op("pe", mm, reads=[b_silrep, b_wada[wb]], writes=[bbuf[pb]])
                if chunk in gate_of_chunk:
                    g = gate_of_chunk[chunk]
                    S.op("dve", lambda e, r=r, g=g, hf=hf, pb=pb, wb=wb: e.tensor_tensor(
                        out=gate_bc[:, r, g, hf * 512:(hf + 1) * 512],
                        in0=banks[pb][:, :], in1=bada_sb[wb][:], op=ALU.add),
                        reads=[bbuf[pb], b_bada[wb]], writes=[b_gate])
                else:
                    v = vec_of_chunk[chunk]
                    S.op("dve", lambda e, pb=pb, wb=wb: e.tensor_tensor(
                        out=modrow[:], in0=banks[pb][:, :], in1=bada_sb[wb][:], op=ALU.add),
                        reads=[bbuf[pb], b_bada[wb]], writes=[b_modrow])
                    tb = 2 + (cb * 2 + r) % 2
                    def tp(e, tb=tb):
                        for j in range(4):
                            ins = e.transpose(out=banks[tb][:, j * 128:(j + 1) * 128],
                                              in_=modrow[:, j * 128:(j + 1) * 128],
                                              identity=ident_f[:])
                        return ins
                    S.op("pe", tp, reads=[b_modrow, b_ident_f], writes=[bbuf[tb]])
                    S.op("dve", lambda e, tb=tb, v=v, hf=hf, r=r: e.tensor_copy(
                        out=modT[:, v, hf * 4:(hf + 1) * 4, r],
                        in_=banks[tb][:, :].rearrange("p (j m) -> p j m", m=128)[:, :, 0]),
                        reads=[bbuf[tb]], writes=[b_modT])
        scope_end()
        n2w_sb = sb("n2w_sb", [128, KC]); b_n2w = Buf()
        S.dma("sp", n2w_sb[:], n2w, writes=[b_n2w], stream="c")
        sc2 = sb("sc2", [128, KC, 2]); b_sc2 = Buf()
        S.op("dve", lambda e: e.tensor_scalar(out=sc2[:], in0=modT[:, 3, :, :], scalar1=1.0,
                                              scalar2=None, op0=ALU.add),
             reads=[b_modT], writes=[b_sc2])
        S.op("dve", lambda e: e.tensor_tensor(out=sc2[:], in0=sc2[:],
                                              in1=n2w_sb[:, :].unsqueeze(2).to_broadcast([128, KC, 2]),
                                              op=ALU.mult),
             reads=[b_sc2, b_n2w], writes=[b_sc2])
        S.op("dve", lambda e: e.tensor_scalar(out=sc1[:], in0=modT[:, 1, :, :], scalar1=1.0,
                                              scalar2=None, op0=ALU.add),
             reads=[b_modT], writes=[b_sc1])
        S.op("dve", lambda e: e.tensor_tensor(out=sc1[:], in0=sc1[:],
                                              in1=n1w_sb[:, :].unsqueeze(2).to_broadcast([128, KC, 2]),
                                              op=ALU.mult),
             reads=[b_sc1, b_n1w], writes=[b_sc1])

        class TT:
            def __init__(s, ap, buf):
                s.ap, s.buf = ap, buf
            def __getitem__(s, k):
                return TT(s.ap[k], s.buf)
            def v(s, fn):
                return TT(fn(s.ap), s.buf)

        _cnt = [0]
        def newT(shape, dtype=F32, name=None):
            _cnt[0] += 1
            t = sb(f"{name or 't'}_{_cnt[0]}", shape, dtype)
            return TT(t[tuple(slice(None) for _ in shape)], Buf())

        def _rb(*xs):
            return [x.buf for x in xs if isinstance(x, TT)]
        def _a(x):
            return x.ap if isinstance(x, TT) else x
        def tt(o, a, b, op, eng="dve"):
            S.op(eng, lambda e: e.tensor_tensor(out=o.ap, in0=a.ap, in1=b.ap, op=op),
                 reads=_rb(a, b), writes=[o.buf])
        def ts(o, a, s1, s2, op0, op1=None, eng="dve"):
            if op1 is None:
                S.op(eng, lambda e: e.tensor_scalar(out=o.ap, in0=a.ap, scalar1=_a(s1), scalar2=None, op0=op0),
                     reads=_rb(a, s1), writes=[o.buf])
            else:
                S.op(eng, lambda e: e.tensor_scalar(out=o.ap, in0=a.ap, scalar1=_a(s1), scalar2=_a(s2),
                                                    op0=op0, op1=op1),
                     reads=_rb(a, s1, s2), writes=[o.buf])
        def stt(o, a, sc, b, op0, op1, eng="dve"):
            S.op(eng, lambda e: e.scalar_tensor_tensor(out=o.ap, in0=a.ap, scalar=_a(sc), in1=b.ap, op0=op0, op1=op1),
                 reads=_rb(a, sc, b), writes=[o.buf])
        def cp(o, a, eng="dve"):
            if eng == "act":
                S.op("act", lambda e: e.copy(out=o.ap, in_=a.ap), reads=[a.buf], writes=[o.buf])
            else:
                S.op(eng, lambda e: e.tensor_copy(out=o.ap, in_=a.ap), reads=[a.buf], writes=[o.buf])
        def act(o, a, func, scale=1.0, bias=None, accum=None):
            kw = {}
            if bias is not None:
                kw["bias"] = _a(bias)
            if accum is not None:
                kw["accum_out"] = accum.ap
            S.op("act", lambda e: e.activation(out=o.ap, in_=a.ap, func=func, scale=_a(scale), **kw),
                 reads=_rb(a, scale, bias), writes=[o.buf] + ([accum.buf] if accum is not None else []))
        def memset(o, val, eng="dve"):
            S.op(eng, lambda e: e.memset(o.ap, val), writes=[o.buf])
        def recip(o, a):
            S.op("dve", lambda e: e.reciprocal(out=o.ap, in_=a.ap), reads=[a.buf], writes=[o.buf])
        def mmg(o, pairs):
            def f(e):
                n = len(pairs)
                for i, (l, r) in enumerate(pairs):
                    ins = e.matmul(o.ap, lhsT=l.ap, rhs=r.ap, start=(i == 0), stop=(i == n - 1))
                return ins
            rd = []
            for l, r in pairs:
                rd += [l.buf, r.buf]
            S.op("pe", f, reads=rd, writes=[o.buf])
        def tpose(o, a, idt):
            S.op("pe", lambda e: e.transpose(out=o.ap, in_=a.ap, identity=idt.ap),
                 reads=[a.buf, idt.buf], writes=[o.buf])
        def load(dram_ap, shape, dtype=F32, q="sp", stream="c"):
            t = newT(shape, dtype)
            S.dma(q, t.ap, dram_ap, writes=[t.buf], stream=stream)
            return t
        _bk = [0]
        def nbank():
            _bk[0] = (_bk[0] + 1) % 8
            i = _bk[0]
            return TT(banks[i][:, :], bbuf[i])
        def bf(bank):
            return TT(bank.ap.bitcast(BF16), bank.buf)
        _ae = [0]
        def evac_eng():
            _ae[0] ^= 1
            return "act" if _ae[0] else "dve"

        identF = TT(ident_f[:], b_ident_f); identB = TT(ident_b[:], b_ident_b)
        epsT = TT(eps_t[:], b_eps)
        sc2T = TT(sc2[:], b_sc2)
        sc1T = TT(sc1[:], b_sc1); modTT = TT(modT[:], b_modT); gateT = TT(gate_bc[:], b_gate)
        def load_win():
            w = newT([128, KC, 2048], BF16, "w_in_sb")
            for kc in range(KC):
                S.dma("pool", w.ap[:, kc, :], w_in_v[:, kc, :], writes=[w.buf], stream="w")
            return w

        scope_begin()
        wout_v = w_out_d.rearrange("(kc p) n -> p kc n", p=128)
        wglu_sb = newT([128, 4, 512], BF16, "wglu_sb")
        S.dma("pool", wglu_sb.ap, w_glu_d.rearrange("(kc p) n -> p kc n", p=128), writes=[wglu_sb.buf], stream="w")

        def g128(x, g):
            return x.v(lambda a: a[:, g, :, :].rearrange("p j h -> p (j h)"))

        def build_s5_consts():
            Qr = newT([128, 32, 8, 16], BF16, "Qr"); QiN = newT([128, 32, 8, 16], BF16, "QiN")
            Mt = newT([128, 32, 128], BF16, "Mt")
            PT = newT([128, 32, 2, 128], BF16, "PT")
            A1 = newT([128, 2, 32]); A2 = newT([128, 2, 32])
            scope_begin()
            lamre = load(lam_re_l, [128, 32]); lamim = load(lam_im_l, [128, 32]); logdt = load(logdt_l, [128, 32])
            Bre = load(bre_l, [128, 32, 16]); Bim = load(bim_l, [128, 32, 16])
            Cre = load(cre_l, [128, 32, 16]); Cim = load(cim_l, [128, 32, 16])
            Dfold = load(dfold_l, [128, 32]); mle = load(mle_l, [128, 128]); mge = load(mge_l, [128, 128])
            halfpi = newT([128, 1]); memset(halfpi, float(np.pi / 2))

            def s32():
                return newT([128, 32])
            def cmul(a, b):
                (ar_, ai_), (br_, bi_) = a, b
                t1, t2, t3, t4, cr, ci = s32(), s32(), s32(), s32(), s32(), s32()
                tt(t1, ar_, br_, ALU.mult); tt(t2, ai_, bi_, ALU.mult); tt(cr, t1, t2, ALU.subtract)
                tt(t3, ar_, bi_, ALU.mult); tt(t4, ai_, br_, ALU.mult); tt(ci, t3, t4, ALU.add)
                return (cr, ci)

            dtt = s32(); act(dtt, logdt, AF.Exp)
            ar = s32(); ai = s32(); tt(ar, lamre, dtt, ALU.mult); tt(ai, lamim, dtt, ALU.mult)
            mag = s32(); act(mag, ar, AF.Exp, scale=1.0 / 32)
            sn = s32(); act(sn, ai, AF.Sin, scale=1.0 / 32)
            cs = s32(); act(cs, ai, AF.Sin, scale=1.0 / 32, bias=halfpi)
            zr = s32(); zi = s32(); tt(zr, mag, cs, ALU.mult); tt(zi, mag, sn, ALU.mult)
            z = (zr, zi)
            for _ in range(5):
                z = cmul(z, z)
            lb = z
            one = s32(); zero = s32(); memset(one, 1.0); memset(zero, 0.0)
            pw = [(one, zero), lb]
            for e_ in range(2, 9):
                pw.append(cmul(pw[-1], lb))
            m2 = s32(); t_ = s32(); tt(m2, lb[0], lb[0], ALU.mult); tt(t_, lb[1], lb[1], ALU.mult)
            tt(m2, m2, t_, ALU.add); rinv = s32(); recip(rinv, m2)
            ilr = s32(); ili = s32(); tt(ilr, lb[0], rinv, ALU.mult); tt(ili, lb[1], rinv, ALU.mult)
            ts(ili, ili, -1.0, None, ALU.mult)
            ilb = (ilr, ili)
            ipw = [(one, zero), ilb]
            for e_ in range(2, 8):
                ipw.append(cmul(ipw[-1], ilb))
            nr = s32(); ts(nr, lb[0], -1.0, None, ALU.add)
            den = s32(); t2_ = s32(); tt(den, lamre, lamre, ALU.mult); tt(t2_, lamim, lamim, ALU.mult)
            tt(den, den, t2_, ALU.add); rden = s32(); recip(rden, den)
            t5 = s32(); t6 = s32(); kr = s32(); ki = s32()
            tt(t5, nr, lamre, ALU.mult); tt(t6, lb[1], lamim, ALU.mult); tt(kr, t5, t6, ALU.add)
            tt(t5, lb[1], lamre, ALU.mult); tt(t6, nr, lamim, ALU.mult); tt(ki, t5, t6, ALU.subtract)
            tt(kr, kr, rden, ALU.mult); tt(ki, ki, rden, ALU.mult)
            selPr = newT([128, 32, 8]); selPi = newT([128, 32, 8]); selQr = newT([128, 32, 8]); selQi = newT([128, 32, 8])
            for j in range(8):
                for (dst, src, comp) in ((selPr, pw, 0), (selPi, pw, 1), (selQr, ipw, 0), (selQi, ipw, 1)):
                    cp(dst[0:64, :, j], src[7 - j][comp][0:64, :])
                    cp(dst[64:128, :, j], src[j][comp][64:128, :])
            def bc8(x):
                return x.v(lambda a: a.unsqueeze(2).to_broadcast([128, 32, 8]))
            wr = newT([128, 32, 8]); wi = newT([128, 32, 8]); ta = newT([128, 32, 8]); tb_ = newT([128, 32, 8])
            tt(ta, selPr, bc8(kr), ALU.mult); tt(tb_, selPi, bc8(ki), ALU.mult); tt(wr, ta, tb_, ALU.subtract)
            tt(ta, selPr, bc8(ki), ALU.mult); tt(tb_, selPi, bc8(kr), ALU.mult); tt(wi, ta, tb_, ALU.add)
            def bj(x):
                return x.v(lambda a: a.unsqueeze(3).to_broadcast([128, 32, 8, 16]))
            def bh(x):
                return x.v(lambda a: a.unsqueeze(2).to_broadcast([128, 32, 8, 16]))
            big1 = newT([128, 32, 8, 16]); big2 = newT([128, 32, 8, 16])
            Pr = newT([128, 32, 8, 16], BF16, "Pr"); Pi = newT([128, 32, 8, 16], BF16, "Pi")
            tt(big1, bj(wr), bh(Bre), ALU.mult); tt(big2, bj(wi), bh(Bim), ALU.mult); tt(Pr, big1, big2, ALU.subtract)
            tt(big1, bj(wr), bh(Bim), ALU.mult); tt(big2, bj(wi), bh(Bre), ALU.mult); tt(Pi, big1, big2, ALU.add)
            tt(big1, bj(selQr), bh(Cre), ALU.mult); tt(big2, bj(selQi), bh(Cim), ALU.mult); tt(Qr, big1, big2, ALU.subtract)
            tt(big1, bj(selQr), bh(Cim), ALU.mult); tt(big2, bj(selQi), bh(Cre), ALU.mult); tt(big1, big1, big2, ALU.add)
            ts(QiN, big1, -1.0, None, ALU.mult)
            def g128(x, g):
                return x.v(lambda a: a[:, g, :, :].rearrange("p j h -> p (j h)"))
            mtmp = newT([128, 4, 128])
            qa = newT([128, 32, 8, 16], BF16); qb = newT([128, 32, 8, 16], BF16)
            for d_ in range(2):
                cp(qa, Qr); cp(qb, QiN, eng="act")
                z = slice(64, 128) if d_ == 0 else slice(0, 64)
                memset(qa[z], 0.0); memset(qb[z], 0.0)
                msk = (mle if d_ == 0 else mge).v(lambda a: a.unsqueeze(1).to_broadcast([128, 4, 128]))
                for g0 in range(0, 32, 4):
                    bk = nbank()
                    for gg in range(4):
                        g = g0 + gg
                        mmg(bk[:, gg * 128:(gg + 1) * 128], [(g128(Pr, g), g128(qa, g)), (g128(Pi, g), g128(qb, g))])
                    bv = bk.v(lambda a: a.rearrange("p (g c) -> p g c", g=4))
                    tt(mtmp, bv, msk, ALU.mult)
                    for gg in range(4):
                        g = g0 + gg
                        if d_ == 0:
                            stt(Mt[:, g, :], identF, Dfold[:, g:g + 1], mtmp[:, gg, :], ALU.mult, ALU.add)
                        else:
                            tt(Mt[:, g, :], Mt[:, g, :], mtmp[:, gg, :], ALU.add)
            for g0 in range(0, 32, 4):
                bk = bf(nbank())
                for gg in range(4):
                    for ri, src in enumerate((Pr, Pi)):
                        col = (gg * 2 + ri) * 128
                        tpose(bk[:, col:col + 128], g128(src, g0 + gg), identB)
                cp(PT[:, g0:g0 + 4, :, :], bk.v(lambda a: a.rearrange("p (g r c) -> p g r c", g=4, r=2)), eng=evac_eng())
            cp(A1[:, 0, :], pw[8][0]); cp(A1[:, 1, :], pw[8][0])
            ts(A2[:, 0, :], pw[8][1], -1.0, None, ALU.mult); cp(A2[:, 1, :], pw[8][1])
            scope_end()

            return Qr, QiN, Mt, PT, A1, A2

        print("NOPS at end of consts", Sched.nops)
        if STAGE < 1:
            S.dead = True
        class _Gen:
            pass
        gen = _Gen()

        def alloc_gen(kv=True):
            gen.xt = [newT([128, D]) for i in range(2)]
            gen.xs = [newT([128, D], BF16) for i in range(2)]
            gen.junk = newT([128, D], BF16)
            gen.ssq = [newT([128, 1]) for i in range(2)]
            gen.rstd = [newT([128, 1]) for i in range(2)]
            if kv == "one":
                _kv2 = newT([128, 1024])
                gen.kv_sb = [_kv2, _kv2]
            elif kv:
                gen.kv_sb = [newT([128, 1024]) for i in range(2)]
            else:
                _kv1 = newT([128, 512])
                gen.kv_sb = [_kv1, _kv1]
        _nt = [0]

        def norm_tile(x_in, hT_out, scT, shT):
            _nt[0] += 1
            i = _nt[0] % 2
            act(gen.junk, x_in, AF.Square, accum=gen.ssq[i])
            act(gen.rstd[i], gen.ssq[i], AF.Sqrt, scale=1.0 / D, bias=epsT[:, 0:1])
            recip(gen.rstd[i], gen.rstd[i])
            ts(gen.xs[i], x_in, gen.rstd[i][:, 0:1], None, ALU.mult)
            bk = bf(nbank())
            for kc in range(KC):
                tpose(bk[:, kc * 128:(kc + 1) * 128], gen.xs[i][:, kc * 128:(kc + 1) * 128], identB)
            for kc in range(KC):
                act(hT_out[:, kc, :], bk[:, kc * 128:(kc + 1) * 128], AF.Identity,
                    scale=scT[:, kc:kc + 1], bias=shT[:, kc:kc + 1])

        lq = load(lq_bc, [128, 2, 64]); lk = load(lk_bc, [128, 2, 64])
        lprod = newT([128, 2, 64]); tt(lprod, lq, lk, ALU.mult)
        lsum = newT([128, 2])
        S.op("dve", lambda e: e.tensor_reduce(out=lsum.ap, in_=lprod.ap, axis=AX.X, op=ALU.add),
             reads=[lprod.buf], writes=[lsum.buf])
        lexp = newT([128, 2]); act(lexp, lsum, AF.Exp)
        lamneg = newT([128, 1])
        tt(lamneg, lexp[:, 1:2], lexp[:, 0:1], ALU.subtract)
        ts(lamneg, lamneg, -0.2, None, ALU.add)
        subw = load(subw_bc, [128, 128]); ts(subw, subw, 0.8, None, ALU.mult)

        catS = newT([128, KC, NS_TOK], BF16, "catS")
        pos = load(pos_rc, [128, 32, 2]); fidx = load(fidx_bc, [128, 16])
        freq = newT([128, 16]); act(freq, fidx, AF.Exp, scale=-float(np.log(10000.0)) / 16)
        scope_begin()
        Qr, QiN, Mt, PT, A1, A2 = build_s5_consts()
        scope_begin()
        Uf = newT([128, 32, 128], BF16, "Uf")
        yf = newT([128, 32, 128], BF16, "yf")
        qT = newT([128, 4, NP_TOK], BF16, "qT"); kT = newT([128, 4, NP_TOK], BF16, "kT")
        Vaug = newT([128, 8, 4, 129], BF16, "Vaug"); memset(Vaug, 1.0)
        scope_begin()
        winT = load_win()
        hT = newT([128, KC, NP_TOK], BF16, "hT")
        scope_begin()
        alloc_gen(kv="one")
        _qk1 = newT([128, 1024], BF16, name="qkbf")
        qk_bf = [_qk1, _qk1]
        for t in range(8):
            tok0 = t * 128
            i = t % 2
            S.dma("sp", gen.xt[i].ap, xp[tok0:tok0 + 128, :], writes=[gen.xt[i].buf], stream="x")
            norm_tile(gen.xt[i], hT[:, :, tok0:tok0 + 128], sc1T[:, :, 0], modTT[:, 0, :, 0])
            bq, bkk, bv_ = nbank(), nbank(), nbank()
            for bnk, c0 in ((bq, 512), (bkk, 1024), (bv_, 1536)):
                mmg(bnk, [(hT[:, kc, tok0:tok0 + 128], winT[:, kc, c0:c0 + 512]) for kc in range(KC)])
            cp(gen.kv_sb[i][:, 0:512], bkk, eng="act"); cp(gen.kv_sb[i][:, 512:1024], bv_, eng="dve")
            S.dma("sp", o_ck[tok0:tok0 + 128, :], gen.kv_sb[i].ap[:, 0:512], reads=[gen.kv_sb[i].buf], stream="o")
            S.dma("sp", o_cv[tok0:tok0 + 128, :], gen.kv_sb[i].ap[:, 512:1024], reads=[gen.kv_sb[i].buf], stream="o")
            cp(qk_bf[i][:, 0:512], bq, eng="act"); cp(qk_bf[i][:, 512:1024], gen.kv_sb[i][:, 0:512], eng="dve")
            cp(Vaug[:, t, :, 0:128], gen.kv_sb[i][:, 512:1024].v(lambda a: a.rearrange("p (h e) -> p h e", h=4)), eng="dve")
            bt = bf(nbank())
            for k8 in range(8):
                tpose(bt[:, k8 * 128:(k8 + 1) * 128], qk_bf[i][:, k8 * 128:(k8 + 1) * 128], identB)
            cp(qT[:, :, tok0:tok0 + 128], bt[:, 0:512].v(lambda a: a.rearrange("p (h c) -> p h c", h=4)), eng="act")
            cp(kT[:, :, tok0:tok0 + 128], bt[:, 512:1024].v(lambda a: a.rearrange("p (h c) -> p h c", h=4)), eng="act")

        scope_end()
        hT_cj = hT.v(lambda a: a.rearrange("p k (c j) -> p k c j", j=8))
        u_cm = newT([128, 32, 8, 16], BF16, "u_cm")
        for j in range(8):
            bk = nbank()
            mmg(bk, [(hT_cj[:, kc, :, j], winT[:, kc, 0:512]) for kc in range(KC)])
            cp(u_cm[:, :, j, :], bk.v(lambda a: a.rearrange("p (g h) -> p g h", h=16)), eng=evac_eng())
        for g0 in range(0, 32, 8):
            bk = bf(nbank())
            for gg in range(8):
                g = g0 + gg
                tpose(bk[:, gg * 128:(gg + 1) * 128], g128(u_cm, g), identB)
            cp(Uf[:, g0:g0 + 8, :], bk.v(lambda a: a.rearrange("p (g c) -> p g c", g=8)), eng=evac_eng())
        scope_end()
        scope_begin()
        Sp = newT([128, 128, 2, 32], BF16, "Sp")
        if STAGE < 2:
            S.dead = True
        for g0 in range(0, 32, 2):
            bk = nbank()
            for gg in range(2):
                for ri in range(2):
                    col = (gg * 2 + ri) * 128
                    mmg(bk[:, col:col + 128], [(PT[:, g0 + gg, ri, :], Uf[:, g0 + gg, :])])
            cp(Sp[:, :, :, g0:g0 + 2], bk.v(lambda a: a.rearrange("p (g r c) -> p c r g", g=2, r=2)), eng=evac_eng())
        if STAGE < 3:
            S.dead = True
        Gs = newT([128, 4, 2, 32], F32, "Gs"); memset(Gs, 0.0)
        Tt_ = newT([128, 4, 2, 32], F32, "Tt"); X1 = newT([128, 4, 2, 32]); X2 = newT([128, 4, 2, 32])
        Gst = newT([128, 2, 32, 128], BF16, "Gst")
        Sp_sl = Sp.v(lambda a: a.rearrange("p (s l) r g -> p s l r g", l=32))
        Gst_sl = Gst.v(lambda a: a.rearrange("p r g (s l) -> p s r g l", l=32))
        A1b = A1.v(lambda a: a.unsqueeze(1).to_broadcast([128, 4, 2, 32]))
        for step in range(32):
            for half, l in ((slice(0, 64), step), (slice(64, 128), 31 - step)):
                cp(Gst_sl[half, :, :, :, l], Gs[half], eng="act")
                tt(Tt_[half], Gs[half], Sp_sl[half, :, l, :, :], ALU.add)
                tt(X1[half], Tt_[half], A1b[half], ALU.mult)
                for ri in range(2):
                    tt(X2[half, :, ri, :], Tt_[half, :, 1 - ri, :],
                       A2.v(lambda a: a[:, ri, :].unsqueeze(1).to_broadcast([128, 4, 32]))[half], ALU.mult)
                tt(Gs[half], X1[half], X2[half], ALU.add)
        if STAGE < 4:
            S.dead = True
        hfin = newT([128, 2, 128], F32, "hfin")
        Tt_flat = Tt_.v(lambda a: a.rearrange("p s r g -> p (s r g)"))
        bk = nbank()
        for k2 in range(2):
            tpose(bk[:, k2 * 128:(k2 + 1) * 128], Tt_flat[:, k2 * 128:(k2 + 1) * 128], identF)
        cp(hfin, bk[:, 0:256].v(lambda a: a.rearrange("p (k c) -> p k c", k=2)))
        S.dma("sp", o_st.rearrange("(k p) c -> p k c", p=128), hfin.ap, reads=[hfin.buf], stream="o")
        for g0 in range(0, 32, 4):
            bk = nbank()
            for gg in range(4):
                g = g0 + gg
                mmg(bk[:, gg * 128:(gg + 1) * 128],
                    [(Mt[:, g, :], Uf[:, g, :]), (g128(Qr, g), Gst[:, 0, g, :]), (g128(QiN, g), Gst[:, 1, g, :])])
            cp(yf[:, g0:g0 + 4, :], bk.v(lambda a: a.rearrange("p (g c) -> p g c", g=4)), eng=evac_eng())
        scope_end()
        catT = newT([128, KC, NP_TOK], BF16, "catT")
        scope_begin()
        y_cm = newT([128, 8, 512], F32, "y_cm")
        for g0 in range(0, 32, 8):
            bk = bf(nbank())
            for gg in range(8):
                tpose(bk[:, gg * 128:(gg + 1) * 128], yf[:, g0 + gg, :], identB)
            cp(y_cm[:, :, 16 * g0:16 * g0 + 128].v(lambda a: a.rearrange("p i (g h) -> p g i h", g=8)),
               bk.v(lambda a: a.rearrange("p (g i h) -> p g i h", g=8, i=8)), eng=evac_eng())
        if STAGE < 5:
            S.dead = True
        g_cm = newT([128, 8, 512], BF16, "g_cm")
        gt1 = newT([128, 4, 512]); gt2 = newT([128, 4, 512])
        for hf in range(2):
            ysl = y_cm[:, hf * 4:(hf + 1) * 4, :]
            act(gt1, ysl, AF.Square)
            ts(gt1, gt1, 0.044715, 1.0, ALU.mult, ALU.add)
            tt(gt1, gt1, ysl, ALU.mult)
            act(gt2, gt1, AF.Sigmoid, scale=1.5957691216057308)
            tt(g_cm[:, hf * 4:(hf + 1) * 4, :], ysl, gt2, ALU.mult)
        gT = newT([128, 4, NP_TOK], BF16, "gT")
        gT_v = gT.v(lambda a: a.rearrange("p k (c j) -> p k c j", j=8))
        for i0 in range(0, 8, 2):
            bk = bf(nbank())
            for ii in range(2):
                for k4 in range(4):
                    col = (ii * 4 + k4) * 128
                    tpose(bk[:, col:col + 128], g_cm[:, i0 + ii, k4 * 128:(k4 + 1) * 128], identB)
            for ii in range(2):
                cp(gT_v[:, :, :, i0 + ii],
                   bk[:, ii * 512:(ii + 1) * 512].v(lambda a: a.rearrange("p (k c) -> p k c", k=4)), eng="act")
        sg = [newT([128, 512], name=f"sg{i}") for i in range(2)]
        for m4 in range(4):
            for tb2 in range(2):
                bk = nbank()
                tsl = slice(tb2 * 512, (tb2 + 1) * 512)
                mmg(bk, [(wglu_sb[:, k4, m4 * 128:(m4 + 1) * 128], gT[:, k4, tsl]) for k4 in range(4)])
                act(sg[tb2], bk, AF.Sigmoid)
                tt(catT[:, m4, tsl], gT[:, m4, tsl], sg[tb2], ALU.mult)
        scope_end()

        scope_begin()
        kTm = [newT([128, 4, NP_TOK], BF16, f"kTm{m}") for m in range(2)]
        for m in range(2):
            cp(kTm[m], kT, eng=("act" if m else "dve"))
            z = slice(64, 128) if m == 0 else slice(0, 64)
            memset(kTm[m][z], 0.0)
        PTs = [newT([128, 512], BF16, name=f"PTs{m}") for m in range(2)]
        o_tok = [newT([128, 4, 128], name=f"otok{q}") for q in range(2)]
        rr = newT([128, 2]); sq = newT([128, 4, 128]); ss4 = newT([128, 4]); on = newT([128, 4, 128], BF16)
        for s_ in range(4):
            for hd in range(4):
                for m in range(2):
                    bk = nbank()
                    for kt in range(2):
                        k0 = s_ * 256 + kt * 128
                        mmg(bk[:, kt * 256:(kt + 1) * 256],
                            [(kTm[m][:, hd, k0:k0 + 128], qT[:, hd, s_ * 256:(s_ + 1) * 256])])
                    act(PTs[m], bk, AF.Exp, scale=0.125)
                for qt in range(2):
                    bk = nbank()
                    for m in range(2):
                        mmg(bk[:, m * 129:(m + 1) * 129],
                            [(PTs[m][:, kt * 256 + qt * 128:kt * 256 + qt * 128 + 128], Vaug[:, 2 * s_ + kt, hd, :])
                             for kt in range(2)])
                    recip(rr[:, 0:1], bk[:, 128:129]); recip(rr[:, 1:2], bk[:, 257:258])
                    tt(rr[:, 1:2], rr[:, 1:2], lamneg, ALU.mult)
                    ts(o_tok[qt][:, hd, :], bk[:, 0:128], rr[:, 0:1], None, ALU.mult)
                    stt(o_tok[qt][:, hd, :], bk[:, 129:257], rr[:, 1:2], o_tok[qt][:, hd, :], ALU.mult, ALU.add)
            for qt in range(2):
                tok0 = s_ * 256 + qt * 128
                tt(sq, o_tok[qt], o_tok[qt], ALU.mult)
                S.op("dve", lambda e: e.tensor_reduce(out=ss4.ap, in_=sq.ap, axis=AX.X, op=ALU.add),
                     reads=[sq.buf], writes=[ss4.buf])
                act(ss4, ss4, AF.Sqrt, scale=1.0 / 128, bias=epsT[:, 0:1])
                recip(ss4, ss4)
                tt(sq, o_tok[qt], ss4.v(lambda a: a.unsqueeze(2).to_broadcast([128, 4, 128])), ALU.mult)
                tt(on, sq, subw.v(lambda a: a.unsqueeze(1).to_broadcast([128, 4, 128])), ALU.mult)
                bk = bf(nbank())
                for hd in range(4):
                    tpose(bk[:, hd * 128:(hd + 1) * 128], on[:, hd, :], identB)
                cp(catT[:, 4:8, tok0:tok0 + 128], bk[:, 0:512].v(lambda a: a.rearrange("p (h c) -> p h c", h=4)), eng="act")
        scope_end()

        scope_begin()
        alloc_gen()
        wout_sb = newT([128, KC, D], BF16, "wout_sb")
        for kc in range(KC):
            S.dma("pool", wout_sb.ap[:, kc, :], wout_v[:, kc, :], writes=[wout_sb.buf], stream="w")
        x1t = [newT([128, D], name=f"x1t{i}") for i in range(2)]
        wtmp = newT([128, 512])
        for t in range(8):
            tok0 = t * 128
            i = t % 2
            S.dma("sp", gen.xt[i].ap, xp[tok0:tok0 + 128, :], writes=[gen.xt[i].buf], stream="x")
            for cb in range(2):
                csl = slice(cb * 512, (cb + 1) * 512)
                bk = nbank()
                mmg(bk, [(catT[:, kc, tok0:tok0 + 128], wout_sb[:, kc, csl]) for kc in range(KC)])
                tt(wtmp, bk, gateT[:, 0, 0, csl], ALU.mult)
                tt(x1t[i][:, csl], wtmp, gen.xt[i][:, csl], ALU.add)
            S.dma("sp", x1_d[tok0:tok0 + 128, :], x1t[i].ap, reads=[x1t[i].buf], stream="o")
        S.dead = False
        scope_end()
        scope_end()
        if STAGE < 6:
            S.dead = True
        H0, H1 = slice(0, 64), slice(64, 128)

        yf2 = newT([128, 2, 32, 128], BF16, "yf2")
        scope_begin()
        UfA = newT([128, 32, 512], BF16, "UfA")
        scope_begin()
        alloc_gen()
        winu = newT([128, KC, 512], BF16, "winu")
        for kc in range(KC):
            S.dma("pool", winu.ap[:, kc, :], w_in_v[:, kc, 0:512], writes=[winu.buf], stream="w")
        hTb = newT([128, KC, 1024], BF16, "hTb"); u_cm = newT([128, 32, 8, 16], BF16, "u_cm_s")
        hTb_cj = hTb.v(lambda a: a.rearrange("p k (c j) -> p k c j", j=8))
        for blk in range(4):
            for t in range(8):
                i = t % 2
                r0 = blk * 1024 + t * 128
                S.dma("sp", gen.xt[i].ap, xs_all[r0:r0 + 128, :], writes=[gen.xt[i].buf], stream="x")
                norm_tile(gen.xt[i], hTb[:, :, t * 128:(t + 1) * 128], sc1T[:, :, 1], modTT[:, 0, :, 1])
            for j in range(8):
                bk = nbank()
                mmg(bk, [(hTb_cj[:, kc, :, j], winu[:, kc, :]) for kc in range(KC)])
                cp(u_cm[:, :, j, :], bk.v(lambda a: a.rearrange("p (g h) -> p g h", h=16)), eng=evac_eng())
            for g0 in range(0, 32, 8):
                bk = bf(nbank())
                for gg in range(8):
                    tpose(bk[:, gg * 128:(gg + 1) * 128], g128(u_cm, g0 + gg), identB)
                cp(UfA[:, g0:g0 + 8, blk * 128:(blk + 1) * 128],
                   bk.v(lambda a: a.rearrange("p (g c) -> p g c", g=8)), eng="act")
        scope_end()
        GstO = newT([128, 2, 32, 256], BF16, "GstO")
        SpA = newT([128, 64, 2, 32], BF16, "SpA"); SpB = newT([128, 64, 2, 32], BF16, "SpB")
        h0 = load(h0_l, [128, 2, 32])
        Gs = newT([128, 2, 32]); Tt2 = newT([128, 2, 32]); X1s = newT([128, 2, 32]); X2s = newT([128, 2, 32])

        def compute_Sp(dst, c0):
            for g0 in range(0, 32, 4):
                bk = nbank()
                for gg in range(4):
                    for ri in range(2):
                        col = (gg * 2 + ri) * 64
                        mmg(bk[:, col:col + 64], [(PT[:, g0 + gg, ri, :], UfA[:, g0 + gg, c0:c0 + 64])])
                cp(dst[:, :, :, g0:g0 + 4], bk.v(lambda a: a.rearrange("p (g r c) -> p c r g", g=4, r=2)), eng=evac_eng())

        X1p = newT([128, 2, 32]); Tt2p = newT([128, 2, 32]); Gsp = newT([128, 2, 32])
        X2dT = newT([128, 2, 32]); X2qT = newT([128, 2, 32])
        X2d = [TT(X2dT.ap[:, ri, :], Buf()) for ri in range(2)]
        X2q = [TT(X2qT.ap[:, ri, :], Buf()) for ri in range(2)]

        def a8mul(dst, src, half, eng="dve"):
            xa, xb, xw = (X1s, X2d, X2dT) if eng == "dve" else (X1p, X2q, X2qT)
            tt(xa[half], src[half], A1[half], ALU.mult, eng=eng)
            for ri in range(2):
                tt(xb[ri][half], src[half, 1 - ri, :], A2[half, ri, :], ALU.mult, eng=eng)
            S.op(eng, lambda e: e.tensor_tensor(out=dst[half].ap, in0=xa[half].ap, in1=xw[half].ap, op=ALU.add),
                 reads=[xa.buf, xb[0].buf, xb[1].buf], writes=[dst.buf])

        a8mul(Gsp, h0, H0, eng="pool"); a8mul(Gs, h0, H1)
        for k in range(256):
            if k % 64 == 0:
                compute_Sp(SpA, k); compute_Sp(SpB, 448 - k)
            cp(GstO[H0, :, :, k], Gsp[H0], eng="act")
            tt(Tt2p[H0], Gsp[H0], SpA[H0, k % 64, :, :], ALU.add, eng="pool")
            a8mul(Gsp, Tt2p, H0, eng="pool")
            tt(Tt2[H1], Gs[H1], SpB[H1, 63 - k % 64, :, :], ALU.add)
            a8mul(Gs, Tt2, H1)
        for k in range(256, 512):
            cb_ = 511 - k
            if k % 64 == 0:
                compute_Sp(SpB, 448 - k)
            cp(GstO[H1, :, :, cb_], Gs[H1], eng="act")
            tt(Tt2[H1], Gs[H1], SpB[H1, 63 - k % 64, :, :], ALU.add)
            a8mul(Gs, Tt2, H1)
        for blk in range(2):
            csl = slice(blk * 128, (blk + 1) * 128)
            for g0 in range(0, 32, 4):
                bk = nbank()
                for gg in range(4):
                    g = g0 + gg
                    mmg(bk[:, gg * 128:(gg + 1) * 128],
                        [(Mt[:, g, :], UfA[:, g, csl]), (g128(Qr, g), GstO[:, 0, g, csl]), (g128(QiN, g), GstO[:, 1, g, csl])])
                cp(yf2[:, blk, g0:g0 + 4, :], bk.v(lambda a: a.rearrange("p (g c) -> p g c", g=4)), eng=evac_eng())
        scope_end()
        scope_begin()
        y_cm = newT([128, 8, 512], F32, "y_cm_s"); g_cm = newT([128, 8, 512], BF16, "g_cm_s")
        gt1 = newT([128, 4, 512]); gt2 = newT([128, 4, 512])
        gT = newT([128, 4, 1024], BF16, "gT_s")
        gT_v = gT.v(lambda a: a.rearrange("p k (c j) -> p k c j", j=8))
        sg = [newT([128, 512]) for i in range(2)]
        for blk in range(2):
            for g0 in range(0, 32, 8):
                bk = bf(nbank())
                for gg in range(8):
                    tpose(bk[:, gg * 128:(gg + 1) * 128], yf2[:, blk, g0 + gg, :], identB)
                cp(y_cm[:, :, 16 * g0:16 * g0 + 128].v(lambda a: a.rearrange("p i (g h) -> p g i h", g=8)),
                   bk.v(lambda a: a.rearrange("p (g i h) -> p g i h", g=8, i=8)), eng="act")
            for hf in range(2):
                ysl = y_cm[:, hf * 4:(hf + 1) * 4, :]
                act(gt1, ysl, AF.Square)
                ts(gt1, gt1, 0.044715, 1.0, ALU.mult, ALU.add)
                tt(gt1, gt1, ysl, ALU.mult)
                act(gt2, gt1, AF.Sigmoid, scale=1.5957691216057308)
                tt(g_cm[:, hf * 4:(hf + 1) * 4, :], ysl, gt2, ALU.mult)
            for i0 in range(0, 8, 2):
                bk = bf(nbank())
                for ii in range(2):
                    for k4 in range(4):
                        col = (ii * 4 + k4) * 128
                        tpose(bk[:, col:col + 128], g_cm[:, i0 + ii, k4 * 128:(k4 + 1) * 128], identB)
                for ii in range(2):
                    cp(gT_v[:, :, :, i0 + ii],
                       bk[:, ii * 512:(ii + 1) * 512].v(lambda a: a.rearrange("p (k c) -> p k c", k=4)), eng="act")
            for m4 in range(4):
                for tb2 in range(2):
                    bk = nbank()
                    tsl = slice(tb2 * 512, (tb2 + 1) * 512)
                    osl = slice(blk * 1024 + tb2 * 512, blk * 1024 + (tb2 + 1) * 512)
                    mmg(bk, [(wglu_sb[:, k4, m4 * 128:(m4 + 1) * 128], gT[:, k4, tsl]) for k4 in range(4)])
                    act(sg[tb2], bk, AF.Sigmoid)
                    tt(catS[:, m4, osl], gT[:, m4, tsl], sg[tb2], ALU.mult)
        scope_end()
        scope_end()

        if STAGE < 7:
            S.dead = True
        scope_begin()
        kTa = newT([128, 4, 4608], BF16, "kTa"); Vs = newT([128, 36, 4, 129], BF16, "Vs"); memset(Vs, 1.0)
        qTs = newT([128, 4, NS_TOK], BF16, "qTs")
        scope_begin()
        alloc_gen(kv=False)
        winq = newT([128, KC, 1536], BF16, "winq")
        for kc in range(KC):
            S.dma("pool", winq.ap[:, kc, :], w_in_v[:, kc, 512:2048], writes=[winq.buf], stream="w")
        hTt2 = [newT([128, KC, 128], BF16, "hTt") for _ in range(2)]
        qkf2 = [newT([128, 2, 512], F32, "qkf") for _ in range(2)]
        qkb2 = [newT([128, 2, 512], BF16, "qkb") for _ in range(2)]
        ang4 = newT([128, 2, 2, 16]); kf4 = newT([128, 2, 2, 16])
        SC2 = [newT([128, 2, 2, 16]) for _ in range(2)]
        r1 = newT([128, 16, 2, 16]); r2 = newT([128, 16, 2, 16])
        MAGIC = 12582912.0
        TWO_PI = float(2 * np.pi)
        vbank = {}

        def stA(tile):
            own = tile < 16
            hTt, qkf, qkb = hTt2[tile % 2], qkf2[tile % 2], qkb2[tile % 2]
            if tile < 32:
                i = tile % 2
                S.dma("sp", gen.xt[i].ap, xs_all[tile * 128:(tile + 1) * 128, :], writes=[gen.xt[i].buf], stream="x")
                SC = SC2[tile % 2]
                tt(ang4[:, 0, :, :], pos[:, tile, :].v(lambda a: a.unsqueeze(2).to_broadcast([128, 2, 16])),
                   freq.v(lambda a: a.unsqueeze(1).to_broadcast([128, 2, 16])), ALU.mult)
                ts(ang4[:, 1, :, :], ang4[:, 0, :, :], float(np.pi / 2), None, ALU.add)
                ts(kf4, ang4, 1.0 / TWO_PI, MAGIC, ALU.mult, ALU.add)
                ts(kf4, kf4, -MAGIC, None, ALU.add)
                stt(kf4, kf4, -TWO_PI, ang4, ALU.mult, ALU.add)
                ts(kf4, kf4, -3.1415925, 3.1415925, ALU.max, ALU.min)
                act(SC, kf4, AF.Sin)
                norm_tile(gen.xt[i], hTt, sc1T[:, :, 1], modTT[:, 0, :, 1])
                bkk, bv_ = nbank(), nbank()
                mmg(bkk, [(hTt[:, kc, :], winq[:, kc, 512:1024]) for kc in range(KC)])
                mmg(bv_, [(hTt[:, kc, :], winq[:, kc, 1024:1536]) for kc in range(KC)])
                cp(qkf[:, 1, :], bkk, eng="act")
                vbank[tile] = bv_
                if own:
                    bq = nbank()
                    mmg(bq, [(hTt[:, kc, :], winq[:, kc, 0:512]) for kc in range(KC)])
                    cp(qkf[:, 0, :], bq, eng="act")
            else:
                ct = tile - 32
                S.dma("sp", qkf.ap[:, 1, :], ck_in[ct * 128:(ct + 1) * 128, :], writes=[qkf.buf], stream="x")
                S.dma("sp", gen.xt[tile % 2].ap[:, 0:512], cv_in[ct * 128:(ct + 1) * 128, :], writes=[gen.xt[tile % 2].buf], stream="x")

        def stB(tile):
            own = tile < 16
            hTt, qkf, qkb = hTt2[tile % 2], qkf2[tile % 2], qkb2[tile % 2]
            if tile < 32:
                SC = SC2[tile % 2]
                cp(Vs[:, tile, :, 0:128], vbank.pop(tile).v(lambda a: a.rearrange("p (h e) -> p h e", h=4)), eng="dve")
                a0 = 0 if own else 1
                na = 2 - a0
                xv = qkf[:, a0:2, :].v(lambda a: a.rearrange("p a (b r x f) -> p (a b) r x f", r=2, x=2, f=16))
                ov = qkb[:, a0:2, :].v(lambda a: a.rearrange("p a (b r x f) -> p (a b) r x f", r=2, x=2, f=16))
                A_ = na * 8
                COS = SC[:, 1, :, :].v(lambda a: a.unsqueeze(1).to_broadcast([128, A_, 2, 16]))
                SIN = SC[:, 0, :, :].v(lambda a: a.unsqueeze(1).to_broadcast([128, A_, 2, 16]))
                x1 = xv[:, :, :, 0, :]; x2 = xv[:, :, :, 1, :]
                tt(r1[:, 0:A_], x1, COS, ALU.mult); tt(r2[:, 0:A_], x2, SIN, ALU.mult)
                tt(ov[:, :, :, 0, :], r1[:, 0:A_], r2[:, 0:A_], ALU.subtract)
                tt(r1[:, 0:A_], x2, COS, ALU.mult); tt(r2[:, 0:A_], x1, SIN, ALU.mult)
                tt(ov[:, :, :, 1, :], r1[:, 0:A_], r2[:, 0:A_], ALU.add)
            else:
                cp(qkb[:, 1, :], qkf[:, 1, :])
                cp(Vs[:, tile, :, 0:128], gen.xt[tile % 2][:, 0:512].v(lambda a: a.rearrange("p (h e) -> p h e", h=4)))
            bt = bf(nbank())
            for k8 in range(4 if not own else 8):
                src = qkb[:, 1, (k8 % 4) * 128:(k8 % 4 + 1) * 128] if k8 < 4 else qkb[:, 0, (k8 - 4) * 128:(k8 - 3) * 128]
                tpose(bt[:, k8 * 128:(k8 + 1) * 128], src, identB)
            cp(kTa[:, :, tile * 128:(tile + 1) * 128], bt[:, 0:512].v(lambda a: a.rearrange("p (h c) -> p h c", h=4)), eng="act")
            if own:
                cp(qTs[:, :, tile * 128:(tile + 1) * 128], bt[:, 512:1024].v(lambda a: a.rearrange("p (h c) -> p h c", h=4)), eng="act")

        stA(0)
        for tile in range(36):
            if tile + 1 < 36:
                stA(tile + 1)
            stB(tile)
        scope_end()
        qTm = [newT([128, 512], BF16, name=f"qTm{m}") for m in range(2)]
        PTs = [newT([128, 512], BF16, name=f"PTss{i}") for i in range(4)]
        Osb = [newT([128, 4, 129], name=f"Osb{m}") for m in range(2)]
        o_tok = newT([128, 4, 4, 128], F32, "o_tok_s")
        rr = newT([128, 2]); sq = newT([128, 4, 128]); ss4 = newT([128, 4]); on = newT([128, 4, 128], BF16)
        obank = [TT(banks[i][:, :], bbuf[i]) for i in range(4)]
        _sbk = [0]
        def sbank():
            _sbk[0] = (_sbk[0] + 1) % 4
            i = 4 + _sbk[0]
            return TT(banks[i][:, :], bbuf[i])
        _pt = [0]
        for qb in range(4):
            for hd in range(4):
                for m in range(2):
                    cp(qTm[m], qTs[:, hd, qb * 512:(qb + 1) * 512], eng=("act" if m else "dve"))
                    z = H1 if m == 0 else H0
                    memset(qTm[m][z], 0.0)
                    sbks = {}
                    Pbuf = {}
                    for it in range(36 + 3):
                        if it < 36:
                            kt = it
                            sbks[kt] = sbank()
                            mmg(sbks[kt], [(kTa[:, hd, kt * 128:(kt + 1) * 128], qTm[m])])
                        if 0 <= it - 2 < 36:
                            kt = it - 2
                            _pt[0] = (_pt[0] + 1) % 4
                            Pbuf[kt] = PTs[_pt[0]]
                            act(Pbuf[kt], sbks[kt], AF.Exp, scale=0.125)
                        if 0 <= it - 3 < 36:
                            kt = it - 3
                            P_ = Pbuf[kt]
                            for qt in range(4):
                                S.op("pe", lambda e, qt=qt, P_=P_, kt=kt: e.matmul(
                                    obank[qt].ap[:, 0:129], lhsT=P_.ap[:, qt * 128:(qt + 1) * 128],
                                    rhs=Vs.ap[:, kt, hd, :], start=(kt == 0), stop=(kt == 35)),
                                    reads=[P_.buf, Vs.buf], writes=[obank[qt].buf])
                    for qt in range(4):
                        cp(Osb[m][:, qt, :], obank[qt][:, 0:129], eng=("act" if qt % 2 else "dve"))
                for qt in range(4):
                    recip(rr[:, 0:1], Osb[0][:, qt, 128:129]); recip(rr[:, 1:2], Osb[1][:, qt, 128:129])
                    tt(rr[:, 1:2], rr[:, 1:2], lamneg, ALU.mult)
                    ts(o_tok[:, qt, hd, :], Osb[0][:, qt, 0:128], rr[:, 0:1], None, ALU.mult)
                    stt(o_tok[:, qt, hd, :], Osb[1][:, qt, 0:128], rr[:, 1:2], o_tok[:, qt, hd, :], ALU.mult, ALU.add)
            for qt in range(4):
                tok0 = qb * 512 + qt * 128
                ot = o_tok[:, qt, :, :]
                tt(sq, ot, ot, ALU.mult)
                S.op("dve", lambda e: e.tensor_reduce(out=ss4.ap, in_=sq.ap, axis=AX.X, op=ALU.add),
                     reads=[sq.buf], writes=[ss4.buf])
                act(ss4, ss4, AF.Sqrt, scale=1.0 / 128, bias=epsT[:, 0:1])
                recip(ss4, ss4)
                tt(sq, ot, ss4.v(lambda a: a.unsqueeze(2).to_broadcast([128, 4, 128])), ALU.mult)
                tt(on, sq, subw.v(lambda a: a.unsqueeze(1).to_broadcast([128, 4, 128])), ALU.mult)
                bk = TT(banks[4 + qt][:, :].bitcast(BF16), bbuf[4 + qt])
                for hd in range(4):
                    tpose(bk[:, hd * 128:(hd + 1) * 128], on[:, hd, :], identB)
                cp(catS[:, 4:8, tok0:tok0 + 128], bk[:, 0:512].v(lambda a: a.rearrange("p (h c) -> p h c", h=4)), eng="act")
        scope_end()

        scope_begin()
        alloc_gen()
        wout_sb = newT([128, KC, D], BF16, "wout_sb2")
        for kc in range(KC):
            S.dma("pool", wout_sb.ap[:, kc, :], wout_v[:, kc, :], writes=[wout_sb.buf], stream="w")
        x1t = [newT([128, D]) for i in range(2)]
        wtmp = newT([128, 512])
        for t in range(16):
            tok0 = t * 128
            i = t % 2
            S.dma("sp", gen.xt[i].ap, xs_all[tok0:tok0 + 128, :], writes=[gen.xt[i].buf], stream="x")
            for cb in range(2):
                csl = slice(cb * 512, (cb + 1) * 512)
                bk = nbank()
                mmg(bk, [(catS[:, kc, tok0:tok0 + 128], wout_sb[:, kc, csl]) for kc in range(KC)])
                tt(wtmp, bk, gateT[:, 1, 0, csl], ALU.mult)
                tt(x1t[i][:, csl], wtmp, gen.xt[i][:, csl], ALU.add)
            S.dma("sp", x1_d[NP_TOK + tok0:NP_TOK + tok0 + 128, :], x1t[i].ap, reads=[x1t[i].buf], stream="o")
        S.dead = False
        scope_end()
        scope_end()

        NT = NT_MOE
        scope_begin()
        h2T = newT([128, KC, NT * 128], BF16, "h2T")
        acc = newT([128, NT, D], F32, "acc")
        gates = newT([128, NT, 65], F32, "gates"); memset(gates, 1.0)
        _xt1 = newT([128, D])
        gen.xt = [_xt1, _xt1]
        _xs1 = newT([128, D], BF16)
        gen.xs = [_xs1, _xs1]
        gen.junk = newT([128, D], BF16)
        gen.ssq = [newT([128, 1]) for i in range(2)]
        gen.rstd = [newT([128, 1]) for i in range(2)]
        wr_sb = newT([128, KC, 64], BF16, "wr_sb")
        S.dma("pool", wr_sb.ap, w_router_d.rearrange("(kc p) n -> p kc n", p=128), writes=[wr_sb.buf], stream="w")
        rbias = load(rbias_bc, [128, 64])
        sco = newT([128, 64]); bia = newT([128, 64]); eq = newT([128, 64]); mk1 = newT([128, 64])
        gm1 = newT([128, 8]); gm2 = newT([128, 8]); gsc = newT([128, 8]); top8 = newT([128, 8])
        gmask = newT([128, 8]); pen = newT([128, 8]); sel = newT([128, 64]); den = newT([128, 1])
        def g88(x):
            return x.v(lambda a: a.rearrange("p (g e) -> p g e", e=8))
        def b88(x):
            return x.v(lambda a: a.unsqueeze(2).to_broadcast([128, 8, 8]))
        def prologue(t):
            cond = 0 if t < 8 else 1
            i = t % 2
            S.dma("sp", gen.xt[i].ap, x1_d[t * 128:(t + 1) * 128, :], writes=[gen.xt[i].buf], stream="x")
            norm_tile(gen.xt[i], h2T[:, :, t * 128:(t + 1) * 128], sc2T[:, :, cond], modTT[:, 2, :, cond])
            bk = nbank()
            mmg(bk[:, 0:64], [(h2T[:, kc, t * 128:(t + 1) * 128], wr_sb[:, kc, :]) for kc in range(KC)])
            act(sco, bk[:, 0:64], AF.Sigmoid)
            tt(bia, sco, rbias, ALU.add)
            S.op("dve", lambda e: e.tensor_reduce(out=gm1.ap, in_=g88(bia).ap, axis=AX.X, op=ALU.max),
                 reads=[bia.buf], writes=[gm1.buf])
            tt(g88(eq), g88(bia), b88(gm1), ALU.is_equal)
            stt(mk1, eq, -1e9, bia, ALU.mult, ALU.add)
            S.op("dve", lambda e: e.tensor_reduce(out=gm2.ap, in_=g88(mk1).ap, axis=AX.X, op=ALU.max),
                 reads=[mk1.buf], writes=[gm2.buf])
            tt(gsc, gm1, gm2, ALU.add)
            S.op("dve", lambda e: e.max(out=top8.ap, in_=gsc.ap), reads=[gsc.buf], writes=[top8.buf])
            ts(gmask, gsc, top8[:, 3:4], None, ALU.is_ge)
            ts(pen, gmask, -1.0, 1e9, ALU.add, ALU.mult)
            tt(g88(mk1), g88(bia), b88(gmask), ALU.mult)
            tt(g88(mk1), g88(mk1), b88(pen), ALU.add)
            S.op("dve", lambda e: e.max(out=top8.ap, in_=mk1.ap), reads=[mk1.buf], writes=[top8.buf])
            ts(sel, mk1, top8[:, 7:8], None, ALU.is_ge)
            tt(sel, sel, sco, ALU.mult)
            S.op("dve", lambda e: e.tensor_reduce(out=den.ap, in_=sel.ap, axis=AX.X, op=ALU.add),
                 reads=[sel.buf], writes=[den.buf])
            recip(den, den)
            ts(gates[:, t, 0:64], sel, den[:, 0:1], 2.5, ALU.mult, ALU.mult)
        scope_begin()
        wgu = [newT([128, KC, 512], BF16, f"wgu{i}") for i in range(2)]
        wd = [newT([128, 2, D], BF16, f"wd{i}") for i in range(2)]
        sgl = [newT([128, 512], BF16, name=f"sgl{i}") for i in range(2)]
        actT = [newT([128, 512], BF16, name=f"actT{i}") for i in range(2)]
        NE = NE_MOE
        NB_ = NT // 2
        items = [(e_, blk) for e_ in range(NE) for blk in range(NB_)]

        def load_expert(e_):
            wb = e_ % 2
            S.dma("pool", wgu[wb].ap[:, :, 0:256], weg[e_].rearrange("(kc p) f -> p kc f", p=128),
                  writes=[wgu[wb].buf], stream="w")
            S.dma("pool", wgu[wb].ap[:, :, 256:512], weu[e_].rearrange("(kc p) f -> p kc f", p=128),
                  writes=[wgu[wb].buf], stream="w")
            S.dma("pool", wd[wb].ap, wed[e_].rearrange("(c p) f -> p c f", p=128),
                  writes=[wd[wb].buf], stream="w")

        ugb = {}

        def UG(idx):
            e_, blk = items[idx]
            wb = e_ % 2
            tsl = slice(blk * 256, (blk + 1) * 256)
            bA, bB = nbank(), nbank()
            for ffc in range(2):
                mmg(bA[:, ffc * 256:(ffc + 1) * 256],
                    [(wgu[wb][:, kc, ffc * 128:(ffc + 1) * 128], h2T[:, kc, tsl]) for kc in range(KC)])
            for ffc in range(2):
                mmg(bB[:, ffc * 256:(ffc + 1) * 256],
                    [(wgu[wb][:, kc, 256 + ffc * 128:256 + (ffc + 1) * 128], h2T[:, kc, tsl]) for kc in range(KC)])
            ugb[idx] = (bA, bB)

        def MID(idx):
            bA, bB = ugb.pop(idx)
            i = idx % 2
            act(sgl[i], bA, AF.Silu)
            tt(actT[i], sgl[i], bB, ALU.mult)

        def DOWN(idx):
            e_, blk = items[idx]
            wb = e_ % 2
            i = idx % 2
            for t2 in range(2):
                tile_ = blk * 2 + t2
                for cb in range(2):
                    csl = slice(cb * 512, (cb + 1) * 512)
                    bC = nbank()
                    mmg(bC, [(actT[i][:, ffc * 256 + t2 * 128:ffc * 256 + t2 * 128 + 128], wd[wb][:, ffc, csl])
                             for ffc in range(2)])
                    if e_ == 0:
                        ts(acc[:, tile_, csl], bC, gates[:, tile_, e_:e_ + 1], None, ALU.mult)
                    else:
                        stt(acc[:, tile_, csl], bC, gates[:, tile_, e_:e_ + 1], acc[:, tile_, csl], ALU.mult, ALU.add)

        load_expert(0)
        if NE > 1:
            load_expert(1)
        prologue(0); prologue(1)
        UG(0)
        for idx in range(len(items)):
            e_, blk = items[idx]
            MID(idx)
            if idx + 1 < len(items):
                if items[idx + 1][0] == 0:
                    prologue(2 * items[idx + 1][1]); prologue(2 * items[idx + 1][1] + 1)
                UG(idx + 1)
            DOWN(idx)
            if blk == NB_ - 1 and e_ + 2 < NE:
                load_expert(e_ + 2)
        scope_end()
        fwb = load(fw_bc, [128, D])
        _yt1 = newT([128, D], name="ytile")
        ytile = [_yt1, _yt1]
        for t in range(NT):
            cond = 0 if t < 8 else 1
            i = t % 2
            S.dma("sp", gen.xt[i].ap, x1_d[t * 128:(t + 1) * 128, :], writes=[gen.xt[i].buf], stream="x")
            tt(ytile[i], acc[:, t, :], gateT[:, cond, 1, :], ALU.mult)
            tt(gen.xt[i], gen.xt[i], ytile[i], ALU.add)
            act(gen.junk, gen.xt[i], AF.Square, accum=gen.ssq[i])
            act(gen.rstd[i], gen.ssq[i], AF.Sqrt, scale=1.0 / D, bias=epsT[:, 0:1])
            recip(gen.rstd[i], gen.rstd[i])
            stt(ytile[i], gen.xt[i], gen.rstd[i][:, 0:1], fwb, ALU.mult, ALU.mult)
            S.dma("sp", o_y[t * 128:(t + 1) * 128, :], ytile[i].ap, reads=[ytile[i].buf], stream="o")
        scope_end()
        S.finish()
    return nc


_NC_CACHE = {}
_DBG = {}


def kernel(x_prompt, x_sample, c, cache_k, cache_v, state_ssm_re, state_ssm_im,
           c_ctx, w_ada, b_ada, norm1_w, w_in, ssm_lambda_re, ssm_lambda_im, ssm_log_dt,
           ssm_b_re, ssm_b_im, ssm_c_re, ssm_c_im, ssm_d, ssm_w_glu,
           diff_lambda_q, diff_lambda_k, diff_subln_w, w_out, norm2_w,
           w_router, router_bias, w_exp_gate, w_exp_up, w_exp_down,
           w_sh_gate, w_sh_up, w_sh_down, final_norm_w):
    f32 = np.float32
    A = lambda a: np.ascontiguousarray(np.asarray(a, dtype=f32))
    x_prompt = A(x_prompt); x_sample = A(x_sample); c = A(c); c_ctx = A(c_ctx)

    if "nc" not in _NC_CACHE:
        _NC_CACHE["nc"] = build_program()
    nc = _NC_CACHE["nc"]

    def fm(v):
        return np.ascontiguousarray(np.asarray(v, f32).reshape(KC, 128).T)

    jj = np.arange(128) // 16
    shared = {
        "w_ada": A(w_ada)[0],
        "b_ada_bc": np.ascontiguousarray(np.broadcast_to(A(b_ada)[0][None, :], (128, 6 * D))),
        "n1w": fm(A(norm1_w)[0]),
        "w_in": A(w_in)[0],
        "ident": np.eye(128, dtype=f32),
        "w_out": A(w_out)[0], "w_glu": A(ssm_w_glu)[0],
        "dfold_l": np.ascontiguousarray(np.tile(A(ssm_d)[0].reshape(32, 16).T, (8, 1))),
        "mle_l": (jj[:, None] <= jj[None, :]).astype(f32), "mge_l": (jj[:, None] >= jj[None, :]).astype(f32),
        "n2w": fm(A(norm2_w)[0]), "w_router": A(w_router)[0],
        "rbias_bc": np.ascontiguousarray(np.broadcast_to(A(router_bias)[0][None], (128, 64))),
        "fw_bc": np.ascontiguousarray(np.broadcast_to(A(final_norm_w)[None], (128, D))),
        "weg": np.concatenate([A(w_exp_gate)[0], A(w_sh_gate)], axis=0),
        "weu": np.concatenate([A(w_exp_up)[0], A(w_sh_up)], axis=0),
        "wed": np.concatenate([A(w_exp_down)[0], A(w_sh_down)], axis=0),
        "lq_bc": np.ascontiguousarray(np.broadcast_to(A(diff_lambda_q)[0][None], (128, 2, 64))),
        "lk_bc": np.ascontiguousarray(np.broadcast_to(A(diff_lambda_k)[0][None], (128, 2, 64))),
        "subw_bc": np.ascontiguousarray(np.broadcast_to(A(diff_subln_w)[0][None], (128, 128))),
    }
    def s5l(rev):
        ds = [1, 0] if rev else [0, 1]
        C = np.ascontiguousarray
        o = {}
        o["lam_re_l"] = C(A(ssm_lambda_re)[0][ds].transpose(0, 2, 1).reshape(128, 32))
        o["lam_im_l"] = C(A(ssm_lambda_im)[0][ds].transpose(0, 2, 1).reshape(128, 32))
        o["logdt_l"] = C(np.repeat(A(ssm_log_dt)[0][ds][:, None, :], 64, axis=1).reshape(128, 32))
        o["bre_l"] = C(A(ssm_b_re)[0][ds].transpose(0, 2, 1, 3).reshape(128, 32, 16))
        o["bim_l"] = C(A(ssm_b_im)[0][ds].transpose(0, 2, 1, 3).reshape(128, 32, 16))
        o["cre_l"] = C(A(ssm_c_re)[0][ds].transpose(0, 3, 1, 2).reshape(128, 32, 16))
        o["cim_l"] = C(A(ssm_c_im)[0][ds].transpose(0, 3, 1, 2).reshape(128, 32, 16))
        return o
    s5maps = [s5l(False), s5l(True)]
    in_maps = []
    for i in range(NCORES):
        b, rev = i // 2, (i % 2 == 1)
        xpi = x_prompt[4 * i:4 * i + 4]
        if rev:
            xpi = xpi[:, ::-1]
        cond2 = np.stack([c_ctx, c[b]], axis=0)
        condT = np.ascontiguousarray(cond2.reshape(2, KC, 128).transpose(2, 1, 0))
        m = dict(shared)
        m["xp"] = np.ascontiguousarray(xpi.reshape(NP_TOK, D))
        m["condT"] = condT
        m.update(s5maps[i % 2])
        xsb = x_sample[b]
        idx = np.arange(4096)
        if rev:
            xsb = xsb[::-1]
            idx = idx[::-1]
        m["xs_all"] = np.ascontiguousarray(xsb)
        rc = np.stack([(idx // 64).astype(f32), (idx % 64).astype(f32)], axis=-1)
        m["pos_rc"] = np.ascontiguousarray(rc.reshape(32, 128, 2).transpose(1, 0, 2))
        m["fidx_bc"] = np.ascontiguousarray(np.broadcast_to(np.arange(16, dtype=f32)[None], (128, 16)))
        m["ck_in"] = np.ascontiguousarray(A(cache_k)[b, 0].reshape(512, 512))
        m["cv_in"] = np.ascontiguousarray(A(cache_v)[b, 0].reshape(512, 512))
        ds_ = [1, 0] if rev else [0, 1]
        hr = A(state_ssm_re)[b, 0][ds_].transpose(0, 2, 1).reshape(128, 32)
        hi = A(state_ssm_im)[b, 0][ds_].transpose(0, 2, 1).reshape(128, 32)
        m["h0_l"] = np.ascontiguousarray(np.stack([hr, hi], axis=1))
        in_maps.append(m)

    res = run_bass_kernel_spmd(nc, in_maps, core_ids=list(range(NCORES)))
    R = res.results

    y_prompt = np.zeros((32, 256, D), f32)
    y_sample = np.zeros((4, 4096, D), f32)
    new_ck = np.zeros((32, 1, 256, 4, 128), f32)
    new_cv = np.zeros((32, 1, 256, 4, 128), f32)
    new_re = np.zeros((32, 1, 2, 32, 64), f32)
    new_im = np.zeros((32, 1, 2, 32, 64), f32)
    for i in range(NCORES):
        rev = (i % 2 == 1)
        ck = np.asarray(R[i]["o_ck"]).reshape(4, 256, 4, 128)
        cv = np.asarray(R[i]["o_cv"]).reshape(4, 256, 4, 128)
        if rev:
            ck, cv = ck[:, ::-1], cv[:, ::-1]
        new_ck[4 * i:4 * i + 4, 0] = ck
        new_cv[4 * i:4 * i + 4, 0] = cv
        stt_ = np.asarray(R[i]["o_st"]).reshape(4, 2, 32, 2, 64)
        if rev:
            stt_ = stt_[:, :, :, ::-1]
        new_re[4 * i:4 * i + 4, 0] = stt_[:, 0].transpose(0, 2, 1, 3)
        new_im[4 * i:4 * i + 4, 0] = stt_[:, 1].transpose(0, 2, 1, 3)
        yy = np.asarray(R[i]["o_y"])
        yp = yy[:NP_TOK].reshape(4, 256, D)
        if rev:
            yp = yp[:, ::-1]
        y_prompt[4 * i:4 * i + 4] = yp
        ys = yy[NP_TOK:]
        if rev:
            y_sample[i // 2, 2048:] = ys[::-1]
        else:
            y_sample[i // 2, :2048] = ys
    return (y_prompt, y_sample, new_ck, new_cv, new_re, new_im)
```

```python
import contextlib
import os
STAGE = int(os.environ.get('KSTAGE', '9'))
import numpy as np
import ml_dtypes
import concourse.bass as bass
import concourse.mybir as mybir
from concourse.bass_utils import run_bass_kernel_spmd

F32 = mybir.dt.float32
BF16 = mybir.dt.bfloat16
ALU = mybir.AluOpType
AF = mybir.ActivationFunctionType
AX = mybir.AxisListType

NCORES = 8
D = 1024
KC = 8
NP_TOK = 1024
NS_TOK = 2048
NT_MOE = int(os.environ.get("NT_MOE", "24"))
NE_MOE = int(os.environ.get("NE_MOE", "65"))
EPS = 1e-6


class Tk:
    __slots__ = ("sem", "val", "eng")

    def __init__(self, sem, val, eng):
        self.sem, self.val, self.eng = sem, val, eng


class Buf:
    __slots__ = ("name", "w", "r")

    def __init__(self, name=""):
        self.name, self.w, self.r = name, None, {}


class Sched:
    def __init__(self, nc, stack):
        self.nc = nc
        self.engs = {"pe": nc.tensor, "act": nc.scalar, "dve": nc.vector,
                     "pool": nc.gpsimd, "sp": nc.sync}
        self.sem = {}
        self.cnt = {}
        for k in ("pe", "act", "dve", "pool"):
            self.sem[k] = stack.enter_context(nc.semaphore("c_" + k))
            self.cnt[k] = 0
        self.seen = {k: {} for k in self.engs}
        self.dsem = {}
        self.dcnt = {}
        self.bsem = {}
        self.free = {"sw": [], "hw": []}
        self.kind = {}
        self.stack = stack

    def _wait(self, e, tk):
        if tk is None:
            return
        if tk.eng == "pe" and e == "pe":
            return
        key = tk.sem.num
        if self.seen[e].get(key, 0) >= tk.val:
            return
        self.engs[e].wait_ge(tk.sem, tk.val)
        self.seen[e][key] = tk.val

    def _deps(self, e, reads, writes):
        need = {}

        def add(tk):
            if tk is None or (tk.eng == "pe" and e == "pe"):
                return
            k = tk.sem.num
            if k not in need or need[k].val < tk.val:
                need[k] = tk
        for b in reads:
            add(b.w)
        for b in writes:
            add(b.w)
            for t in b.r.values():
                add(t)
        for tk in need.values():
            self._wait(e, tk)

    def _mark(self, tk, reads, writes):
        for b in reads:
            b.r[tk.sem.num] = tk
        for b in writes:
            b.w = tk
            b.r = {}

    dead = False
    nops = 0
    KCUT = int(os.environ.get('KCUT', '100000000'))

    def op(self, e, fn, reads=(), writes=()):
        Sched.nops += 1
        if self.dead or Sched.nops > Sched.KCUT:
            return
        self._deps(e, reads, writes)
        ins = fn(self.engs[e])
        self.cnt[e] += 1
        ins.then_inc(self.sem[e], 1)
        self._mark(Tk(self.sem[e], self.cnt[e], e), reads, writes)

    def dma(self, q, out, in_, reads=(), writes=(), stream="ld", **kw):
        Sched.nops += 1
        if self.dead or Sched.nops > Sched.KCUT:
            return
        kind = "sw" if q == "pool" else "hw"
        key = (id(writes[0]) if writes else id(reads[0]), kind)
        if key not in self.bsem:
            if self.free[kind]:
                num = self.free[kind].pop()
            else:
                h = self.stack.enter_context(self.nc.semaphore("d%d" % len(self.dsem)))
                num = h.num
                self.dsem[num] = h
                self.dcnt[num] = 0
                self.kind[num] = kind
            self.bsem[key] = num
        num = self.bsem[key]
        self._deps(q, reads, writes)
        ins = self.engs[q].dma_start(out=out, in_=in_, **kw)
        self.dcnt[num] += 16
        ins.then_inc(self.dsem[num], 16)
        self._mark(Tk(self.dsem[num], self.dcnt[num], "dma"), reads, writes)

    def barrier(self):
        es = ("pe", "act", "dve", "pool", "sp")
        for e in es:
            for k in ("pe", "act", "dve", "pool"):
                if k != e and self.cnt[k]:
                    self._wait(e, Tk(self.sem[k], self.cnt[k], k))
            for s_, sem in self.dsem.items():
                if self.dcnt[s_]:
                    self._wait(e, Tk(sem, self.dcnt[s_], "dma"))
        self.free = {"sw": [n for n in self.dsem if self.kind[n] == "sw"],
                     "hw": [n for n in self.dsem if self.kind[n] == "hw"]}
        self.bsem = {}

    def finish(self):
        for s, sem in self.dsem.items():
            if self.dcnt[s]:
                self.engs["sp"].wait_ge(sem, self.dcnt[s])


def build_program():
    nc = bass.Bass("TRN2", target_bir_lowering=False)
    dt = nc.dram_tensor

    def din(name, shape, dtype=F32):
        return dt(name, list(shape), dtype, kind="ExternalInput").ap()

    def dout(name, shape, dtype=F32):
        return dt(name, list(shape), dtype, kind="ExternalOutput").ap()

    xp = din("xp", [NP_TOK, D])
    condT = din("condT", [128, KC, 2])
    w_ada = din("w_ada", [D, 6 * D])
    b_ada_bc = din("b_ada_bc", [128, 6 * D])
    n1w = din("n1w", [128, KC])
    w_in = din("w_in", [D, 2048])
    ident_in = din("ident", [128, 128])
    w_out_d = din("w_out", [D, D])
    w_glu_d = din("w_glu", [512, 512])
    lam_re_l = din("lam_re_l", [128, 32]); lam_im_l = din("lam_im_l", [128, 32]); logdt_l = din("logdt_l", [128, 32])
    bre_l = din("bre_l", [128, 32, 16]); bim_l = din("bim_l", [128, 32, 16])
    cre_l = din("cre_l", [128, 32, 16]); cim_l = din("cim_l", [128, 32, 16])
    dfold_l = din("dfold_l", [128, 32]); mle_l = din("mle_l", [128, 128]); mge_l = din("mge_l", [128, 128])
    xs_all = din("xs_all", [4096, D]); pos_rc = din("pos_rc", [128, 32, 2]); fidx_bc = din("fidx_bc", [128, 16])
    ck_in = din("ck_in", [512, 512]); cv_in = din("cv_in", [512, 512]); h0_l = din("h0_l", [128, 2, 32])
    n2w = din("n2w", [128, KC]); w_router_d = din("w_router", [D, 64]); rbias_bc = din("rbias_bc", [128, 64])
    fw_bc = din("fw_bc", [128, D])
    weg = din("weg", [65, D, 256]); weu = din("weu", [65, D, 256]); wed = din("wed", [65, 256, D])
    lq_bc = din("lq_bc", [128, 2, 64]); lk_bc = din("lk_bc", [128, 2, 64]); subw_bc = din("subw_bc", [128, 128])

    o_ck = dout("o_ck", [NP_TOK, 512])
    o_cv = dout("o_cv", [NP_TOK, 512])
    o_st = dout("o_st", [256, 128])
    o_y = dout("o_y", [NP_TOK + NS_TOK, D])
    x1_d = dt("x1_scratch", [NP_TOK + NS_TOK, D], F32, kind="Internal").ap()

    with contextlib.ExitStack() as st:
        S = Sched(nc, st)

        cur = [st]

        def sb(name, shape, dtype=F32):
            return cur[0].enter_context(nc.sbuf_tensor(name, list(shape), dtype))

        _stk = []

        def scope_begin():
            _stk.append(cur[0])
            cur[0] = contextlib.ExitStack()

        def scope_end():
            S.barrier()
            cur[0].close()
            cur[0] = _stk.pop()

        def psum(name, shape, dtype=F32):
            return st.enter_context(nc.psum_tensor(name, list(shape), dtype))

        banks = [psum(f"bank{i}", [128, 512], F32) for i in range(8)]
        bbuf = [Buf(f"bank{i}") for i in range(8)]

        ident_f = sb("ident_f", [128, 128]); b_ident_f = Buf()
        ident_b = sb("ident_b", [128, 128], BF16); b_ident_b = Buf()
        S.dma("sp", ident_f[:], ident_in, writes=[b_ident_f], stream="c")
        S.op("dve", lambda e: e.tensor_copy(out=ident_b[:], in_=ident_f[:]),
             reads=[b_ident_f], writes=[b_ident_b])

        eps_t = sb("eps_t", [128, 1]); b_eps = Buf()
        S.op("dve", lambda e: e.memset(eps_t[:], EPS), writes=[b_eps])
        n1w_sb = sb("n1w_sb", [128, KC]); b_n1w = Buf()
        S.dma("sp", n1w_sb[:], n1w, writes=[b_n1w], stream="c")

        w_in_v = w_in.rearrange("(kc p) n -> p kc n", p=128)

        gate_bc = sb("gate_bc", [128, 2, 2, D]); b_gate = Buf()
        modT = sb("modT", [128, 4, KC, 2]); b_modT = Buf()
        sc1 = sb("sc1", [128, KC, 2]); b_sc1 = Buf()
        scope_begin()
        cT = sb("cT", [128, KC, 2]); b_cT = Buf()
        S.dma("sp", cT[:], condT, writes=[b_cT], stream="c")
        sil = sb("sil", [128, KC, 2]); b_sil = Buf()
        S.op("act", lambda e: e.activation(out=sil[:], in_=cT[:], func=AF.Silu),
             reads=[b_cT], writes=[b_sil])
        silrep = sb("silrep", [128, 2, KC, 128], BF16); b_silrep = Buf()
        for r in range(2):
            S.op("dve", lambda e, r=r: e.tensor_copy(
                out=silrep[:, r, :, :],
                in_=sil[:, :, r:r + 1].to_broadcast([128, KC, 128])),
                reads=[b_sil], writes=[b_silrep])

        wada_sb = [sb(f"wada{i}", [128, KC, 512], BF16) for i in range(2)]
        b_wada = [Buf(), Buf()]
        bada_sb = [sb(f"bada{i}", [128, 512]) for i in range(2)]
        b_bada = [Buf(), Buf()]
        w_ada_v = w_ada.rearrange("(kc p) n -> p kc n", p=128)
        modrow = sb("modrow", [128, 512]); b_modrow = Buf()
        vec_of_chunk = {0: 0, 1: 1, 3: 2, 4: 3}
        gate_of_chunk = {2: 0, 5: 1}
        for cb in range(12):
            chunk, hf = cb // 2, cb % 2
            wb = cb % 2
            S.dma("pool", wada_sb[wb][:], w_ada_v[:, :, cb * 512:(cb + 1) * 512],
                  writes=[b_wada[wb]], stream="w")
            S.dma("sp", bada_sb[wb][:], b_ada_bc[:, cb * 512:(cb + 1) * 512],
                  writes=[b_bada[wb]], stream="c")
            for r in range(2):
                pb = (cb * 2 + r) % 2
                def mm(e, r=r, wb=wb, pb=pb):
                    for kc in range(KC):
                        ins = e.matmul(banks[pb][:, :], lhsT=silrep[:, r, kc, :],
                                       rhs=wada_sb[wb][:, kc, :],
                                       start=(kc == 0), stop=(kc == KC - 1))
                    return ins
                S.op("pe", mm, reads=[b_silrep, b_wada[wb]], writes=[bbuf[pb]])
                if chunk in gate_of_chunk:
                    g = gate_of_chunk[chunk]
                    S.op("dve", lambda e, r=r, g=g, hf=hf, pb=pb, wb=wb: e.tensor_tensor(
                        out=gate_bc[:, r, g, hf * 512:(hf + 1) * 512],
                        in0=banks[pb][:, :], in1=bada_sb[wb][:], op=ALU.add),
                        reads=[bbuf[pb], b_bada[wb]], writes=[b_gate])
                else:
                    v = vec_of_chunk[chunk]
                    S.op("dve", lambda e, pb=pb, wb=wb: e.tensor_tensor(
                        out=modrow[:], in0=banks[pb][:, :], in1=bada_sb[wb][:], op=ALU.add),
                        reads=[bbuf[pb], b_bada[wb]], writes=[b_modrow])
                    tb = 2 + (cb * 2 + r) % 2
                    def tp(e, tb=tb):
                        for j in range(4):
                            ins = e.transpose(out=banks[tb][:, j * 128:(j + 1) * 128],
                                              in_=modrow[:, j * 128:(j + 1) * 128],
                                              identity=ident_f[:])
                        return ins
                    S.op("pe", tp, reads=[b_modrow, b_ident_f], writes=[bbuf[tb]])
                    S.op("dve", lambda e, tb=tb, v=v, hf=hf, r=r: e.tensor_copy(
                        out=modT[:, v, hf * 4:(hf + 1) * 4, r],
                        in_=banks[tb][:, :].rearrange("p (j m) -> p j m", m=128)[:, :, 0]),
                        reads=[bbuf[tb]], writes=[b_modT])
        scope_end()
        n2w_sb = sb("n2w_sb", [128, KC]); b_n2w = Buf()
        S.dma("sp", n2w_sb[:], n2w, writes=[b_n2w], stream="c")
        sc2 = sb("sc2", [128, KC, 2]); b_sc2 = Buf()
        S.op("dve", lambda e: e.tensor_scalar(out=sc2[:], in0=modT[:, 3, :, :], scalar1=1.0,
                                              scalar2=None, op0=ALU.add),
             reads=[b_modT], writes=[b_sc2])
        S.op("dve", lambda e: e.tensor_tensor(out=sc2[:], in0=sc2[:],
                                              in1=n2w_sb[:, :].unsqueeze(2).to_broadcast([128, KC, 2]),
                                              op=ALU.mult),
             reads=[b_sc2, b_n2w], writes=[b_sc2])
        S.op("dve", lambda e: e.tensor_scalar(out=sc1[:], in0=modT[:, 1, :, :], scalar1=1.0,
                                              scalar2=None, op0=ALU.add),
             reads=[b_modT], writes=[b_sc1])
        S.op("dve", lambda e: e.tensor_tensor(out=sc1[:], in0=sc1[:],
                                              in1=n1w_sb[:, :].unsqueeze(2).to_broadcast([128, KC, 2]),
                                              op=ALU.mult),
             reads=[b_sc1, b_n1w], writes=[b_sc1])

        class TT:
            def __init__(s, ap, buf):
                s.ap, s.buf = ap, buf
            def __getitem__(s, k):
                return TT(s.ap[k], s.buf)
            def v(s, fn):
                return TT(fn(s.ap), s.buf)

        _cnt = [0]
        def newT(shape, dtype=F32, name=None):
            _cnt[0] += 1
            t = sb(f"{name or 't'}_{_cnt[0]}", shape, dtype)
            return TT(t[tuple(slice(None) for _ in shape)], Buf())

        def _rb(*xs):
            return [x.buf for x in xs if isinstance(x, TT)]
        def _a(x):
            return x.ap if isinstance(x, TT) else x
        def tt(o, a, b, op, eng="dve"):
            S.op(eng, lambda e: e.tensor_tensor(out=o.ap, in0=a.ap, in1=b.ap, op=op),
                 reads=_rb(a, b), writes=[o.buf])
        def ts(o, a, s1, s2, op0, op1=None, eng="dve"):
            if op1 is None:
                S.op(eng, lambda e: e.tensor_scalar(out=o.ap, in0=a.ap, scalar1=_a(s1), scalar2=None, op0=op0),
                     reads=_rb(a, s1), writes=[o.buf])
            else:
                S.op(eng, lambda e: e.tensor_scalar(out=o.ap, in0=a.ap, scalar1=_a(s1), scalar2=_a(s2),
                                                    op0=op0, op1=op1),
                     reads=_rb(a, s1, s2), writes=[o.buf])
        def stt(o, a, sc, b, op0, op1, eng="dve"):
            S.op(eng, lambda e: e.scalar_tensor_tensor(out=o.ap, in0=a.ap, scalar=_a(sc), in1=b.ap, op0=op0, op1=op1),
                 reads=_rb(a, sc, b), writes=[o.buf])
        def cp(o, a, eng="dve"):
            if eng == "act":
                S.op("act", lambda e: e.copy(out=o.ap, in_=a.ap), reads=[a.buf], writes=[o.buf])
            else:
                S.op(eng, lambda e: e.tensor_copy(out=o.ap, in_=a.ap), reads=[a.buf], writes=[o.buf])
        def act(o, a, func, scale=1.0, bias=None, accum=None):
            kw = {}
            if bias is not None:
                kw["bias"] = _a(bias)
            if accum is not None:
                kw["accum_out"] = accum.ap
            S.op("act", lambda e: e.activation(out=o.ap, in_=a.ap, func=func, scale=_a(scale), **kw),
                 reads=_rb(a, scale, bias), writes=[o.buf] + ([accum.buf] if accum is not None else []))
        def memset(o, val, eng="dve"):
            S.op(eng, lambda e: e.memset(o.ap, val), writes=[o.buf])
        def recip(o, a):
            S.op("dve", lambda e: e.reciprocal(out=o.ap, in_=a.ap), reads=[a.buf], writes=[o.buf])
        def mmg(o, pairs):
            def f(e):
                n = len(pairs)
                for i, (l, r) in enumerate(pairs):
                    ins = e.matmul(o.ap, lhsT=l.ap, rhs=r.ap, start=(i == 0), stop=(i == n - 1))
                return ins
            rd = []
            for l, r in pairs:
                rd += [l.buf, r.buf]
            S.op("pe", f, reads=rd, writes=[o.buf])
        def tpose(o, a, idt):
            S.op("pe", lambda e: e.transpose(out=o.ap, in_=a.ap, identity=idt.ap),
                 reads=[a.buf, idt.buf], writes=[o.buf])
        def load(dram_ap, shape, dtype=F32, q="sp", stream="c"):
            t = newT(shape, dtype)
            S.dma(q, t.ap, dram_ap, writes=[t.buf], stream=stream)
            return t
        _bk = [0]
        def nbank():
            _bk[0] = (_bk[0] + 1) % 8
            i = _bk[0]
            return TT(banks[i][:, :], bbuf[i])
        def bf(bank):
            return TT(bank.ap.bitcast(BF16), bank.buf)
        _ae = [0]
        def evac_eng():
            _ae[0] ^= 1
            return "act" if _ae[0] else "dve"

        identF = TT(ident_f[:], b_ident_f); identB = TT(ident_b[:], b_ident_b)
        epsT = TT(eps_t[:], b_eps)
        sc2T = TT(sc2[:], b_sc2)
        sc1T = TT(sc1[:], b_sc1); modTT = TT(modT[:], b_modT); gateT = TT(gate_bc[:], b_gate)
        def load_win():
            w = newT([128, KC, 2048], BF16, "w_in_sb")
            for kc in range(KC):
                S.dma("pool", w.ap[:, kc, :], w_in_v[:, kc, :], writes=[w.buf], stream="w")
            return w

        scope_begin()
        wout_v = w_out_d.rearrange("(kc p) n -> p kc n", p=128)
        wglu_sb = newT([128, 4, 512], BF16, "wglu_sb")
        S.dma("pool", wglu_sb.ap, w_glu_d.rearrange("(kc p) n -> p kc n", p=128), writes=[wglu_sb.buf], stream="w")

        def g128(x, g):
            return x.v(lambda a: a[:, g, :, :].rearrange("p j h -> p (j h)"))

        def build_s5_consts():
            Qr = newT([128, 32, 8, 16], BF16, "Qr"); QiN = newT([128, 32, 8, 16], BF16, "QiN")
            Mt = newT([128, 32, 128], BF16, "Mt")
            PT = newT([128, 32, 2, 128], BF16, "PT")
            A1 = newT([128, 2, 32]); A2 = newT([128, 2, 32])
            scope_begin()
            lamre = load(lam_re_l, [128, 32]); lamim = load(lam_im_l, [128, 32]); logdt = load(logdt_l, [128, 32])
            Bre = load(bre_l, [128, 32, 16]); Bim = load(bim_l, [128, 32, 16])
            Cre = load(cre_l, [128, 32, 16]); Cim = load(cim_l, [128, 32, 16])
            Dfold = load(dfold_l, [128, 32]); mle = load(mle_l, [128, 128]); mge = load(mge_l, [128, 128])
            halfpi = newT([128, 1]); memset(halfpi, float(np.pi / 2))

            def s32():
                return newT([128, 32])
            def cmul(a, b):
                (ar_, ai_), (br_, bi_) = a, b
                t1, t2, t3, t4, cr, ci = s32(), s32(), s32(), s32(), s32(), s32()
                tt(t1, ar_, br_, ALU.mult); tt(t2, ai_, bi_, ALU.mult); tt(cr, t1, t2, ALU.subtract)
                tt(t3, ar_, bi_, ALU.mult); tt(t4, ai_, br_, ALU.mult); tt(ci, t3, t4, ALU.add)
                return (cr, ci)

            dtt = s32(); act(dtt, logdt, AF.Exp)
            ar = s32(); ai = s32(); tt(ar, lamre, dtt, ALU.mult); tt(ai, lamim, dtt, ALU.mult)
            mag = s32(); act(mag, ar, AF.Exp, scale=1.0 / 32)
            sn = s32(); act(sn, ai, AF.Sin, scale=1.0 / 32)
            cs = s32(); act(cs, ai, AF.Sin, scale=1.0 / 32, bias=halfpi)
            zr = s32(); zi = s32(); tt(zr, mag, cs, ALU.mult); tt(zi, mag, sn, ALU.mult)
            z = (zr, zi)
            for _ in range(5):
                z = cmul(z, z)
            lb = z
            one = s32(); zero = s32(); memset(one, 1.0); memset(zero, 0.0)
            pw = [(one, zero), lb]
            for e_ in range(2, 9):
                pw.append(cmul(pw[-1], lb))
            m2 = s32(); t_ = s32(); tt(m2, lb[0], lb[0], ALU.mult); tt(t_, lb[1], lb[1], ALU.mult)
            tt(m2, m2, t_, ALU.add); rinv = s32(); recip(rinv, m2)
            ilr = s32(); ili = s32(); tt(ilr, lb[0], rinv, ALU.mult); tt(ili, lb[1], rinv, ALU.mult)
            ts(ili, ili, -1.0, None, ALU.mult)
            ilb = (ilr, ili)
            ipw = [(one, zero), ilb]
            for e_ in range(2, 8):
                ipw.append(cmul(ipw[-1], ilb))
            nr = s32(); ts(nr, lb[0], -1.0, None, ALU.add)
            den = s32(); t2_ = s32(); tt(den, lamre, lamre, ALU.mult); tt(t2_, lamim, lamim, ALU.mult)
            tt(den, den, t2_, ALU.add); rden = s32(); recip(rden, den)
            t5 = s32(); t6 = s32(); kr = s32(); ki = s32()
            tt(t5, nr, lamre, ALU.mult); tt(t6, lb[1], lamim, ALU.mult); tt(kr, t5, t6, ALU.add)
            tt(t5, lb[1], lamre, ALU.mult); tt(t6, nr, lamim, ALU.mult); tt(ki, t5, t6, ALU.subtract)
            tt(kr, kr, rden, ALU.mult); tt(ki, ki, rden, ALU.mult)
            selPr = newT([128, 32, 8]); selPi = newT([128, 32, 8]); selQr = newT([128, 32, 8]); selQi = newT([128, 32, 8])
            for j in range(8):
                for (dst, src, comp) in ((selPr, pw, 0), (selPi, pw, 1), (selQr, ipw, 0), (selQi, ipw, 1)):
                    cp(dst[0:64, :, j], src[7 - j][comp][0:64, :])
                    cp(dst[64:128, :, j], src[j][comp][64:128, :])
            def bc8(x):
                return x.v(lambda a: a.unsqueeze(2).to_broadcast([128, 32, 8]))
            wr = newT([128, 32, 8]); wi = newT([128, 32, 8]); ta = newT([128, 32, 8]); tb_ = newT([128, 32, 8])
            tt(ta, selPr, bc8(kr), ALU.mult); tt(tb_, selPi, bc8(ki), ALU.mult); tt(wr, ta, tb_, ALU.subtract)
            tt(ta, selPr, bc8(ki), ALU.mult); tt(tb_, selPi, bc8(kr), ALU.mult); tt(wi, ta, tb_, ALU.add)
            def bj(x):
                return x.v(lambda a: a.unsqueeze(3).to_broadcast([128, 32, 8, 16]))
            def bh(x):
                return x.v(lambda a: a.unsqueeze(2).to_broadcast([128, 32, 8, 16]))
            big1 = newT([128, 32, 8, 16]); big2 = newT([128, 32, 8, 16])
            Pr = newT([128, 32, 8, 16], BF16, "Pr"); Pi = newT([128, 32, 8, 16], BF16, "Pi")
            tt(big1, bj(wr), bh(Bre), ALU.mult); tt(big2, bj(wi), bh(Bim), ALU.mult); tt(Pr, big1, big2, ALU.subtract)
            tt(big1, bj(wr), bh(Bim), ALU.mult); tt(big2, bj(wi), bh(Bre), ALU.mult); tt(Pi, big1, big2, ALU.add)
            tt(big1, bj(selQr), bh(Cre), ALU.mult); tt(big2, bj(selQi), bh(Cim), ALU.mult); tt(Qr, big1, big2, ALU.subtract)
            tt(big1, bj(selQr), bh(Cim), ALU.mult); tt(big2, bj(selQi), bh(Cre), ALU.mult); tt(big1, big1, big2, ALU.add)
            ts(QiN, big1, -1.0, None, ALU.mult)
            def g128(x, g):
                return x.v(lambda a: a[:, g, :, :].rearrange("p j h -> p (j h)"))
            mtmp = newT([128, 4, 128])
            qa = newT([128, 32, 8, 16], BF16); qb = newT([128, 32, 8, 16], BF16)
            for d_ in range(2):
                cp(qa, Qr); cp(qb, QiN, eng="act")
                z = slice(64, 128) if d_ == 0 else slice(0, 64)
                memset(qa[z], 0.0); memset(qb[z], 0.0)
                msk = (mle if d_ == 0 else mge).v(lambda a: a.unsqueeze(1).to_broadcast([128, 4, 128]))
                for g0 in range(0, 32, 4):
                    bk = nbank()
                    for gg in range(4):
                        g = g0 + gg
                        mmg(bk[:, gg * 128:(gg + 1) * 128], [(g128(Pr, g), g128(qa, g)), (g128(Pi, g), g128(qb, g))])
                    bv = bk.v(lambda a: a.rearrange("p (g c) -> p g c", g=4))
                    tt(mtmp, bv, msk, ALU.mult)
                    for gg in range(4):
                        g = g0 + gg
                        if d_ == 0:
                            stt(Mt[:, g, :], identF, Dfold[:, g:g + 1], mtmp[:, gg, :], ALU.mult, ALU.add)
                        else:
                            tt(Mt[:, g, :], Mt[:, g, :], mtmp[:, gg, :], ALU.add)
            for g0 in range(0, 32, 4):
                bk = bf(nbank())
                for gg in range(4):
                    for ri, src in enumerate((Pr, Pi)):
                        col = (gg * 2 + ri) * 128
                        tpose(bk[:, col:col + 128], g128(src, g0 + gg), identB)
                cp(PT[:, g0:g0 + 4, :, :], bk.v(lambda a: a.rearrange("p (g r c) -> p g r c", g=4, r=2)), eng=evac_eng())
            cp(A1[:, 0, :], pw[8][0]); cp(A1[:, 1, :], pw[8][0])
            ts(A2[:, 0, :], pw[8][1], -1.0, None, ALU.mult); cp(A2[:, 1, :], pw[8][1])
            scope_end()

            return Qr, QiN, Mt, PT, A1, A2

        print("NOPS at end of consts", Sched.nops)
        if STAGE < 1:
            S.dead = True
        class _Gen:
            pass
        gen = _Gen()

        def alloc_gen(kv=True):
            gen.xt = [newT([128, D]) for i in range(2)]
            gen.xs = [newT([128, D], BF16) for i in range(2)]
            gen.junk = newT([128, D], BF16)
            gen.ssq = [newT([128, 1]) for i in range(2)]
            gen.rstd = [newT([128, 1]) for i in range(2)]
            if kv == "one":
                _kv2 = newT([128, 1024])
                gen.kv_sb = [_kv2, _kv2]
            elif kv:
                gen.kv_sb = [newT([128, 1024]) for i in range(2)]
            else:
                _kv1 = newT([128, 512])
                gen.kv_sb = [_kv1, _kv1]
        _nt = [0]

        def norm_tile(x_in, hT_out, scT, shT):
            _nt[0] += 1
            i = _nt[0] % 2
            act(gen.junk, x_in, AF.Square, accum=gen.ssq[i])
            act(gen.rstd[i], gen.ssq[i], AF.Sqrt, scale=1.0 / D, bias=epsT[:, 0:1])
            recip(gen.rstd[i], gen.rstd[i])
            ts(gen.xs[i], x_in, gen.rstd[i][:, 0:1], None, ALU.mult)
            bk = bf(nbank())
            for kc in range(KC):
                tpose(bk[:, kc * 128:(kc + 1) * 128], gen.xs[i][:, kc * 128:(kc + 1) * 128], identB)
            for kc in range(KC):
                act(hT_out[:, kc, :], bk[:, kc * 128:(kc + 1) * 128], AF.Identity,
                    scale=scT[:, kc:kc + 1], bias=shT[:, kc:kc + 1])

        lq = load(lq_bc, [128, 2, 64]); lk = load(lk_bc, [128, 2, 64])
        lprod = newT([128, 2, 64]); tt(lprod, lq, lk, ALU.mult)
        lsum = newT([128, 2])
        S.op("dve", lambda e: e.tensor_reduce(out=lsum.ap, in_=lprod.ap, axis=AX.X, op=ALU.add),
             reads=[lprod.buf], writes=[lsum.buf])
        lexp = newT([128, 2]); act(lexp, lsum, AF.Exp)
        lamneg = newT([128, 1])
        tt(lamneg, lexp[:, 1:2], lexp[:, 0:1], ALU.subtract)
        ts(lamneg, lamneg, -0.2, None, ALU.add)
        subw = load(subw_bc, [128, 128]); ts(subw, subw, 0.8, None, ALU.mult)

        catS = newT([128, KC, NS_TOK], BF16, "catS")
        pos = load(pos_rc, [128, 32, 2]); fidx = load(fidx_bc, [128, 16])
        freq = newT([128, 16]); act(freq, fidx, AF.Exp, scale=-float(np.log(10000.0)) / 16)
        scope_begin()
        Qr, QiN, Mt, PT, A1, A2 = build_s5_consts()
        scope_begin()
        Uf = newT([128, 32, 128], BF16, "Uf")
        yf = newT([128, 32, 128], BF16, "yf")
        qT = newT([128, 4, NP_TOK], BF16, "qT"); kT = newT([128, 4, NP_TOK], BF16, "kT")
        Vaug = newT([128, 8, 4, 129], BF16, "Vaug"); memset(Vaug, 1.0)
        scope_begin()
        winT = load_win()
        hT = newT([128, KC, NP_TOK], BF16, "hT")
        scope_begin()
        alloc_gen(kv="one")
        _qk1 = newT([128, 1024], BF16, name="qkbf")
        qk_bf = [_qk1, _qk1]
        for t in range(8):
            tok0 = t * 128
            i = t % 2
            S.dma("sp", gen.xt[i].ap, xp[tok0:tok0 + 128, :], writes=[gen.xt[i].buf], stream="x")
            norm_tile(gen.xt[i], hT[:, :, tok0:tok0 + 128], sc1T[:, :, 0], modTT[:, 0, :, 0])
            bq, bkk, bv_ = nbank(), nbank(), nbank()
            for bnk, c0 in ((bq, 512), (bkk, 1024), (bv_, 1536)):
                mmg(bnk, [(hT[:, kc, tok0:tok0 + 128], winT[:, kc, c0:c0 + 512]) for kc in range(KC)])
            cp(gen.kv_sb[i][:, 0:512], bkk, eng="act"); cp(gen.kv_sb[i][:, 512:1024], bv_, eng="dve")
            S.dma("sp", o_ck[tok0:tok0 + 128, :], gen.kv_sb[i].ap[:, 0:512], reads=[gen.kv_sb[i].buf], stream="o")
            S.dma("sp", o_cv[tok0:tok0 + 128, :], gen.kv_sb[i].ap[:, 512:1024], reads=[gen.kv_sb[i].buf], stream="o")
            cp(qk_bf[i][:, 0:512], bq, eng="act"); cp(qk_bf[i][:, 512:1024], gen.kv_sb[i][:, 0:512], eng="dve")
            cp(Vaug[:, t, :, 0:128], gen.kv_sb[i][:, 512:1024].v(lambda a: a.rearrange("p (h e) -> p h e", h=4)), eng="dve")
            bt = bf(nbank())
            for k8 in range(8):
                tpose(bt[:, k8 * 128:(k8 + 1) * 128], qk_bf[i][:, k8 * 128:(k8 + 1) * 128], identB)
            cp(qT[:, :, tok0:tok0 + 128], bt[:, 0:512].v(lambda a: a.rearrange("p (h c) -> p h c", h=4)), eng="act")
            cp(kT[:, :, tok0:tok0 + 128], bt[:, 512:1024].v(lambda a: a.rearrange("p (h c) -> p h c", h=4)), eng="act")

        scope_end()
        hT_cj = hT.v(lambda a: a.rearrange("p k (c j) -> p k c j", j=8))
        u_cm = newT([128, 32, 8, 16], BF16, "u_cm")
        for j in range(8):
            bk = nbank()
            mmg(bk, [(hT_cj[:, kc, :, j], winT[:, kc, 0:512]) for kc in range(KC)])
            cp(u_cm[:, :, j, :], bk.v(lambda a: a.rearrange("p (g h) -> p g h", h=16)), eng=evac_eng())
        for g0 in range(0, 32, 8):
            bk = bf(nbank())
            for gg in range(8):
                g = g0 + gg
                tpose(bk[:, gg * 128:(gg + 1) * 128], g128(u_cm, g), identB)
            cp(Uf[:, g0:g0 + 8, :], bk.v(lambda a: a.rearrange("p (g c) -> p g c", g=8)), eng=evac_eng())
        scope_end()
        scope_begin()
        Sp = newT([128, 128, 2, 32], BF16, "Sp")
        if STAGE < 2:
            S.dead = True
        for g0 in range(0, 32, 2):
            bk = nbank()
            for gg in range(2):
                for ri in range(2):
                    col = (gg * 2 + ri) * 128
                    mmg(bk[:, col:col + 128], [(PT[:, g0 + gg, ri, :], Uf[:, g0 + gg, :])])
            cp(Sp[:, :, :, g0:g0 + 2], bk.v(lambda a: a.rearrange("p (g r c) -> p c r g", g=2, r=2)), eng=evac_eng())
        if STAGE < 3:
            S.dead = True
        Gs = newT([128, 4, 2, 32], F32, "Gs"); memset(Gs, 0.0)
        Tt_ = newT([128, 4, 2, 32], F32, "Tt"); X1 = newT([128, 4, 2, 32]); X2 = newT([128, 4, 2, 32])
        Gst = newT([128, 2, 32, 128], BF16, "Gst")
        X2v = [TT(X2.ap[:, :, ri, :], Buf()) for ri in range(2)]
        Sp_sl = Sp.v(lambda a: a.rearrange("p (s l) r g -> p s l r g", l=32))
        Gst_sl = Gst.v(lambda a: a.rearrange("p r g (s l) -> p s r g l", l=32))
        A1b = A1.v(lambda a: a.unsqueeze(1).to_broadcast([128, 4, 2, 32]))
        for step in range(32):
            for half, l in ((slice(0, 64), step), (slice(64, 128), 31 - step)):
                cp(Gst_sl[half, :, :, :, l], Gs[half], eng="act")
                tt(Tt_[half], Gs[half], Sp_sl[half, :, l, :, :], ALU.add)
                tt(X1[half], Tt_[half], A1b[half], ALU.mult)
                for ri in range(2):
                    tt(X2v[ri][half], Tt_[half, :, 1 - ri, :],
                       A2.v(lambda a: a[:, ri, :].unsqueeze(1).to_broadcast([128, 4, 32]))[half], ALU.mult)
                S.op("dve", lambda e, half=half: e.tensor_tensor(out=Gs[half].ap, in0=X1[half].ap, in1=X2[half].ap,
                                                                 op=ALU.add),
                     reads=[X1.buf, X2v[0].buf, X2v[1].buf], writes=[Gs.buf])
        if STAGE < 4:
            S.dead = True
        hfin = newT([128, 2, 128], F32, "hfin")
        Tt_flat = Tt_.v(lambda a: a.rearrange("p s r g -> p (s r g)"))
        bk = nbank()
        for k2 in range(2):
            tpose(bk[:, k2 * 128:(k2 + 1) * 128], Tt_flat[:, k2 * 128:(k2 + 1) * 128], identF)
        cp(hfin, bk[:, 0:256].v(lambda a: a.rearrange("p (k c) -> p k c", k=2)))
        S.dma("sp", o_st.rearrange("(k p) c -> p k c", p=128), hfin.ap, reads=[hfin.buf], stream="o")
        for g0 in range(0, 32, 4):
            bk = nbank()
            for gg in range(4):
                g = g0 + gg
                mmg(bk[:, gg * 128:(gg + 1) * 128],
                    [(Mt[:, g, :], Uf[:, g, :]), (g128(Qr, g), Gst[:, 0, g, :]), (g128(QiN, g), Gst[:, 1, g, :])])
            cp(yf[:, g0:g0 + 4, :], bk.v(lambda a: a.rearrange("p (g c) -> p g c", g=4)), eng=evac_eng())
        scope_end()
        catT = newT([128, KC, NP_TOK], BF16, "catT")
        scope_begin()
        y_cm = newT([128, 8, 512], F32, "y_cm")
        for g0 in range(0, 32, 8):
            bk = bf(nbank())
            for gg in range(8):
                tpose(bk[:, gg * 128:(gg + 1) * 128], yf[:, g0 + gg, :], identB)
            cp(y_cm[:, :, 16 * g0:16 * g0 + 128].v(lambda a: a.rearrange("p i (g h) -> p g i h", g=8)),
               bk.v(lambda a: a.rearrange("p (g i h) -> p g i h", g=8, i=8)), eng=evac_eng())
        if STAGE < 5:
            S.dead = True
        g_cm = newT([128, 8, 512], BF16, "g_cm")
        gt1 = newT([128, 4, 512]); gt2 = newT([128, 4, 512])
        for hf in range(2):
            ysl = y_cm[:, hf * 4:(hf + 1) * 4, :]
            act(gt1, ysl, AF.Square)
            ts(gt1, gt1, 0.044715, 1.0, ALU.mult, ALU.add)
            tt(gt1, gt1, ysl, ALU.mult)
            act(gt2, gt1, AF.Sigmoid, scale=1.5957691216057308)
            tt(g_cm[:, hf * 4:(hf + 1) * 4, :], ysl, gt2, ALU.mult)
        gT = newT([128, 4, NP_TOK], BF16, "gT")
        gT_v = gT.v(lambda a: a.rearrange("p k (c j) -> p k c j", j=8))
        for i0 in range(0, 8, 2):
            bk = bf(nbank())
            for ii in range(2):
                for k4 in range(4):
                    col = (ii * 4 + k4) * 128
                    tpose(bk[:, col:col + 128], g_cm[:, i0 + ii, k4 * 128:(k4 + 1) * 128], identB)
            for ii in range(2):
                cp(gT_v[:, :, :, i0 + ii],
                   bk[:, ii * 512:(ii + 1) * 512].v(lambda a: a.rearrange("p (k c) -> p k c", k=4)), eng="act")
        sg = [newT([128, 512], name=f"sg{i}") for i in range(2)]
        for m4 in range(4):
            for tb2 in range(2):
                bk = nbank()
                tsl = slice(tb2 * 512, (tb2 + 1) * 512)
                mmg(bk, [(wglu_sb[:, k4, m4 * 128:(m4 + 1) * 128], gT[:, k4, tsl]) for k4 in range(4)])
                act(sg[tb2], bk, AF.Sigmoid)
                tt(catT[:, m4, tsl], gT[:, m4, tsl], sg[tb2], ALU.mult)
        scope_end()

        scope_begin()
        kTm = [newT([128, 4, NP_TOK], BF16, f"kTm{m}") for m in range(2)]
        for m in range(2):
            cp(kTm[m], kT, eng=("act" if m else "dve"))
            z = slice(64, 128) if m == 0 else slice(0, 64)
            memset(kTm[m][z], 0.0)
        PTs = [newT([128, 512], BF16, name=f"PTs{m}") for m in range(2)]
        o_tok = [newT([128, 4, 128], name=f"otok{q}") for q in range(2)]
        rr = newT([128, 2]); sq = newT([128, 4, 128]); ss4 = newT([128, 4]); on = newT([128, 4, 128], BF16)
        for s_ in range(4):
            for hd in range(4):
                for m in range(2):
                    bk = nbank()
                    for kt in range(2):
                        k0 = s_ * 256 + kt * 128
                        mmg(bk[:, kt * 256:(kt + 1) * 256],
                            [(kTm[m][:, hd, k0:k0 + 128], qT[:, hd, s_ * 256:(s_ + 1) * 256])])
                    act(PTs[m], bk, AF.Exp, scale=0.125)
                for qt in range(2):
                    bk = nbank()
                    for m in range(2):
                        mmg(bk[:, m * 129:(m + 1) * 129],
                            [(PTs[m][:, kt * 256 + qt * 128:kt * 256 + qt * 128 + 128], Vaug[:, 2 * s_ + kt, hd, :])
                             for kt in range(2)])
                    recip(rr[:, 0:1], bk[:, 128:129]); recip(rr[:, 1:2], bk[:, 257:258])
                    tt(rr[:, 1:2], rr[:, 1:2], lamneg, ALU.mult)
                    ts(o_tok[qt][:, hd, :], bk[:, 0:128], rr[:, 0:1], None, ALU.mult)
                    stt(o_tok[qt][:, hd, :], bk[:, 129:257], rr[:, 1:2], o_tok[qt][:, hd, :], ALU.mult, ALU.add)
            for qt in range(2):
                tok0 = s_ * 256 + qt * 128
                tt(sq, o_tok[qt], o_tok[qt], ALU.mult)
                S.op("dve", lambda e: e.tensor_reduce(out=ss4.ap, in_=sq.ap, axis=AX.X, op=ALU.add),
                     reads=[sq.buf], writes=[ss4.buf])
                act(ss4, ss4, AF.Sqrt, scale=1.0 / 128, bias=epsT[:, 0:1])
                recip(ss4, ss4)
                tt(sq, o_tok[qt], ss4.v(lambda a: a.unsqueeze(2).to_broadcast([128, 4, 128])), ALU.mult)
                tt(on, sq, subw.v(lambda a: a.unsqueeze(1).to_broadcast([128, 4, 128])), ALU.mult)
                bk = bf(nbank())
                for hd in range(4):
                    tpose(bk[:, hd * 128:(hd + 1) * 128], on[:, hd, :], identB)
                cp(catT[:, 4:8, tok0:tok0 + 128], bk[:, 0:512].v(lambda a: a.rearrange("p (h c) -> p h c", h=4)), eng="act")
        scope_end()

        scope_begin()
        alloc_gen()
        wout_sb = newT([128, KC, D], BF16, "wout_sb")
        for kc in range(KC):
            S.dma("pool", wout_sb.ap[:, kc, :], wout_v[:, kc, :], writes=[wout_sb.buf], stream="w")
        x1t = [newT([128, D], name=f"x1t{i}") for i in range(2)]
        wtmp = newT([128, 512])
        for t in range(8):
            tok0 = t * 128
            i = t % 2
            S.dma("sp", gen.xt[i].ap, xp[tok0:tok0 + 128, :], writes=[gen.xt[i].buf], stream="x")
            for cb in range(2):
                csl = slice(cb * 512, (cb + 1) * 512)
                bk = nbank()
                mmg(bk, [(catT[:, kc, tok0:tok0 + 128], wout_sb[:, kc, csl]) for kc in range(KC)])
                tt(wtmp, bk, gateT[:, 0, 0, csl], ALU.mult)
                tt(x1t[i][:, csl], wtmp, gen.xt[i][:, csl], ALU.add)
            S.dma("sp", x1_d[tok0:tok0 + 128, :], x1t[i].ap, reads=[x1t[i].buf], stream="o")
        S.dead = False
        scope_end()
        scope_end()
        if STAGE < 6:
            S.dead = True
        H0, H1 = slice(0, 64), slice(64, 128)

        yf2 = newT([128, 2, 32, 128], BF16, "yf2")
        scope_begin()
        UfA = newT([128, 32, 512], BF16, "UfA")
        scope_begin()
        alloc_gen()
        winu = newT([128, KC, 512], BF16, "winu")
        for kc in range(KC):
            S.dma("pool", winu.ap[:, kc, :], w_in_v[:, kc, 0:512], writes=[winu.buf], stream="w")
        hTb = newT([128, KC, 1024], BF16, "hTb"); u_cm = newT([128, 32, 8, 16], BF16, "u_cm_s")
        hTb_cj = hTb.v(lambda a: a.rearrange("p k (c j) -> p k c j", j=8))
        for blk in range(4):
            for t in range(8):
                i = t % 2
                r0 = blk * 1024 + t * 128
                S.dma("sp", gen.xt[i].ap, xs_all[r0:r0 + 128, :], writes=[gen.xt[i].buf], stream="x")
                norm_tile(gen.xt[i], hTb[:, :, t * 128:(t + 1) * 128], sc1T[:, :, 1], modTT[:, 0, :, 1])
            for j in range(8):
                bk = nbank()
                mmg(bk, [(hTb_cj[:, kc, :, j], winu[:, kc, :]) for kc in range(KC)])
                cp(u_cm[:, :, j, :], bk.v(lambda a: a.rearrange("p (g h) -> p g h", h=16)), eng=evac_eng())
            for g0 in range(0, 32, 8):
                bk = bf(nbank())
                for gg in range(8):
                    tpose(bk[:, gg * 128:(gg + 1) * 128], g128(u_cm, g0 + gg), identB)
                cp(UfA[:, g0:g0 + 8, blk * 128:(blk + 1) * 128],
                   bk.v(lambda a: a.rearrange("p (g c) -> p g c", g=8)), eng="act")
        scope_end()
        GstO = newT([128, 2, 32, 256], BF16, "GstO")
        SpA = newT([128, 64, 2, 32], BF16, "SpA"); SpB = newT([128, 64, 2, 32], BF16, "SpB")
        h0 = load(h0_l, [128, 2, 32])
        Gs = newT([128, 2, 32]); Tt2 = newT([128, 2, 32]); X1s = newT([128, 2, 32]); X2s = newT([128, 2, 32])

        def compute_Sp(dst, c0):
            for g0 in range(0, 32, 4):
                bk = nbank()
                for gg in range(4):
                    for ri in range(2):
                        col = (gg * 2 + ri) * 64
                        mmg(bk[:, col:col + 64], [(PT[:, g0 + gg, ri, :], UfA[:, g0 + gg, c0:c0 + 64])])
                cp(dst[:, :, :, g0:g0 + 4], bk.v(lambda a: a.rearrange("p (g r c) -> p c r g", g=4, r=2)), eng=evac_eng())

        X1p = newT([128, 2, 32]); Tt2p = newT([128, 2, 32]); Gsp = newT([128, 2, 32])
        X2dT = newT([128, 2, 32]); X2qT = newT([128, 2, 32])
        X2d = [TT(X2dT.ap[:, ri, :], Buf()) for ri in range(2)]
        X2q = [TT(X2qT.ap[:, ri, :], Buf()) for ri in range(2)]

        def a8mul(dst, src, half, eng="dve"):
            xa, xb, xw = (X1s, X2d, X2dT) if eng == "dve" else (X1p, X2q, X2qT)
            tt(xa[half], src[half], A1[half], ALU.mult, eng=eng)
            for ri in range(2):
                tt(xb[ri][half], src[half, 1 - ri, :], A2[half, ri, :], ALU.mult, eng=eng)
            S.op(eng, lambda e: e.tensor_tensor(out=dst[half].ap, in0=xa[half].ap, in1=xw[half].ap, op=ALU.add),
                 reads=[xa.buf, xb[0].buf, xb[1].buf], writes=[dst.buf])

        a8mul(Gsp, h0, H0, eng="pool"); a8mul(Gs, h0, H1)
        for k in range(256):
            if k % 64 == 0:
                compute_Sp(SpA, k); compute_Sp(SpB, 448 - k)
            cp(GstO[H0, :, :, k], Gsp[H0], eng="act")
            tt(Tt2p[H0], Gsp[H0], SpA[H0, k % 64, :, :], ALU.add, eng="pool")
            a8mul(Gsp, Tt2p, H0, eng="pool")
            tt(Tt2[H1], Gs[H1], SpB[H1, 63 - k % 64, :, :], ALU.add)
            a8mul(Gs, Tt2, H1)
        for k in range(256, 512):
            cb_ = 511 - k
            if k % 64 == 0:
                compute_Sp(SpB, 448 - k)
            cp(GstO[H1, :, :, cb_], Gs[H1], eng="act")
            tt(Tt2[H1], Gs[H1], SpB[H1, 63 - k % 64, :, :], ALU.add)
            a8mul(Gs, Tt2, H1)
        for blk in range(2):
            csl = slice(blk * 128, (blk + 1) * 128)
            for g0 in range(0, 32, 4):
                bk = nbank()
                for gg in range(4):
                    g = g0 + gg
                    mmg(bk[:, gg * 128:(gg + 1) * 128],
                        [(Mt[:, g, :], UfA[:, g, csl]), (g128(Qr, g), GstO[:, 0, g, csl]), (g128(QiN, g), GstO[:, 1, g, csl])])
                cp(yf2[:, blk, g0:g0 + 4, :], bk.v(lambda a: a.rearrange("p (g c) -> p g c", g=4)), eng=evac_eng())
        scope_end()
        scope_begin()
        y_cm = newT([128, 8, 512], F32, "y_cm_s"); g_cm = newT([128, 8, 512], BF16, "g_cm_s")
        gt1 = newT([128, 4, 512]); gt2 = newT([128, 4, 512])
        gT = newT([128, 4, 1024], BF16, "gT_s")
        gT_v = gT.v(lambda a: a.rearrange("p k (c j) -> p k c j", j=8))
        sg = [newT([128, 512]) for i in range(2)]
        for blk in range(2):
            for g0 in range(0, 32, 8):
                bk = bf(nbank())
                for gg in range(8):
                    tpose(bk[:, gg * 128:(gg + 1) * 128], yf2[:, blk, g0 + gg, :], identB)
                cp(y_cm[:, :, 16 * g0:16 * g0 + 128].v(lambda a: a.rearrange("p i (g h) -> p g i h", g=8)),
                   bk.v(lambda a: a.rearrange("p (g i h) -> p g i h", g=8, i=8)), eng="act")
            for hf in range(2):
                ysl = y_cm[:, hf * 4:(hf + 1) * 4, :]
                act(gt1, ysl, AF.Square)
                ts(gt1, gt1, 0.044715, 1.0, ALU.mult, ALU.add)
                tt(gt1, gt1, ysl, ALU.mult)
                act(gt2, gt1, AF.Sigmoid, scale=1.5957691216057308)
                tt(g_cm[:, hf * 4:(hf + 1) * 4, :], ysl, gt2, ALU.mult)
            for i0 in range(0, 8, 2):
                bk = bf(nbank())
                for ii in range(2):
                    for k4 in range(4):
                        col = (ii * 4 + k4) * 128
                        tpose(bk[:, col:col + 128], g_cm[:, i0 + ii, k4 * 128:(k4 + 1) * 128], identB)
                for ii in range(2):
                    cp(gT_v[:, :, :, i0 + ii],
                       bk[:, ii * 512:(ii + 1) * 512].v(lambda a: a.rearrange("p (k c) -> p k c", k=4)), eng="act")
            for m4 in range(4):
                for tb2 in range(2):
                    bk = nbank()
                    tsl = slice(tb2 * 512, (tb2 + 1) * 512)
                    osl = slice(blk * 1024 + tb2 * 512, blk * 1024 + (tb2 + 1) * 512)
                    mmg(bk, [(wglu_sb[:, k4, m4 * 128:(m4 + 1) * 128], gT[:, k4, tsl]) for k4 in range(4)])
                    act(sg[tb2], bk, AF.Sigmoid)
                    tt(catS[:, m4, osl], gT[:, m4, tsl], sg[tb2], ALU.mult)
        scope_end()
        scope_end()

        if STAGE < 7:
            S.dead = True
        scope_begin()
        kTa = newT([128, 4, 4608], BF16, "kTa"); Vs = newT([128, 36, 4, 129], BF16, "Vs"); memset(Vs, 1.0)
        qTs = newT([128, 4, NS_TOK], BF16, "qTs")
        scope_begin()
        alloc_gen(kv=False)
        winq = newT([128, KC, 1536], BF16, "winq")
        for kc in range(KC):
            S.dma("pool", winq.ap[:, kc, :], w_in_v[:, kc, 512:2048], writes=[winq.buf], stream="w")
        hTt2 = [newT([128, KC, 128], BF16, "hTt") for _ in range(2)]
        qkf2 = [newT([128, 2, 512], F32, "qkf") for _ in range(2)]
        qkb2 = [newT([128, 2, 512], BF16, "qkb") for _ in range(2)]
        ang4 = newT([128, 2, 2, 16]); kf4 = newT([128, 2, 2, 16])
        SC2 = [newT([128, 2, 2, 16]) for _ in range(2)]
        r1 = newT([128, 16, 2, 16]); r2 = newT([128, 16, 2, 16])
        MAGIC = 12582912.0
        TWO_PI = float(2 * np.pi)
        vbank = {}

        def stA(tile):
            own = tile < 16
            hTt, qkf, qkb = hTt2[tile % 2], qkf2[tile % 2], qkb2[tile % 2]
            if tile < 32:
                i = tile % 2
                S.dma("sp", gen.xt[i].ap, xs_all[tile * 128:(tile + 1) * 128, :], writes=[gen.xt[i].buf], stream="x")
                SC = SC2[tile % 2]
                tt(ang4[:, 0, :, :], pos[:, tile, :].v(lambda a: a.unsqueeze(2).to_broadcast([128, 2, 16])),
                   freq.v(lambda a: a.unsqueeze(1).to_broadcast([128, 2, 16])), ALU.mult)
                ts(ang4[:, 1, :, :], ang4[:, 0, :, :], float(np.pi / 2), None, ALU.add)
                ts(kf4, ang4, 1.0 / TWO_PI, MAGIC, ALU.mult, ALU.add)
                ts(kf4, kf4, -MAGIC, None, ALU.add)
                stt(kf4, kf4, -TWO_PI, ang4, ALU.mult, ALU.add)
                ts(kf4, kf4, -3.1415925, 3.1415925, ALU.max, ALU.min)
                act(SC, kf4, AF.Sin)
                norm_tile(gen.xt[i], hTt, sc1T[:, :, 1], modTT[:, 0, :, 1])
                bkk, bv_ = nbank(), nbank()
                mmg(bkk, [(hTt[:, kc, :], winq[:, kc, 512:1024]) for kc in range(KC)])
                mmg(bv_, [(hTt[:, kc, :], winq[:, kc, 1024:1536]) for kc in range(KC)])
                cp(qkf[:, 1, :], bkk, eng="act")
                vbank[tile] = bv_
                if own:
                    bq = nbank()
                    mmg(bq, [(hTt[:, kc, :], winq[:, kc, 0:512]) for kc in range(KC)])
                    cp(qkf[:, 0, :], bq, eng="act")
            else:
                ct = tile - 32
                S.dma("sp", qkf.ap[:, 1, :], ck_in[ct * 128:(ct + 1) * 128, :], writes=[qkf.buf], stream="x")
                S.dma("sp", gen.xt[tile % 2].ap[:, 0:512], cv_in[ct * 128:(ct + 1) * 128, :], writes=[gen.xt[tile % 2].buf], stream="x")

        def stB(tile):
            own = tile < 16
            hTt, qkf, qkb = hTt2[tile % 2], qkf2[tile % 2], qkb2[tile % 2]
            if tile < 32:
                SC = SC2[tile % 2]
                cp(Vs[:, tile, :, 0:128], vbank.pop(tile).v(lambda a: a.rearrange("p (h e) -> p h e", h=4)), eng="dve")
                a0 = 0 if own else 1
                na = 2 - a0
                xv = qkf[:, a0:2, :].v(lambda a: a.rearrange("p a (b r x f) -> p (a b) r x f", r=2, x=2, f=16))
                ov = qkb[:, a0:2, :].v(lambda a: a.rearrange("p a (b r x f) -> p (a b) r x f", r=2, x=2, f=16))
                A_ = na * 8
                COS = SC[:, 1, :, :].v(lambda a: a.unsqueeze(1).to_broadcast([128, A_, 2, 16]))
                SIN = SC[:, 0, :, :].v(lambda a: a.unsqueeze(1).to_broadcast([128, A_, 2, 16]))
                x1 = xv[:, :, :, 0, :]; x2 = xv[:, :, :, 1, :]
                tt(r1[:, 0:A_], x1, COS, ALU.mult); tt(r2[:, 0:A_], x2, SIN, ALU.mult)
                tt(ov[:, :, :, 0, :], r1[:, 0:A_], r2[:, 0:A_], ALU.subtract)
                tt(r1[:, 0:A_], x2, COS, ALU.mult); tt(r2[:, 0:A_], x1, SIN, ALU.mult)
                tt(ov[:, :, :, 1, :], r1[:, 0:A_], r2[:, 0:A_], ALU.add)
            else:
                cp(qkb[:, 1, :], qkf[:, 1, :])
                cp(Vs[:, tile, :, 0:128], gen.xt[tile % 2][:, 0:512].v(lambda a: a.rearrange("p (h e) -> p h e", h=4)))
            bt = bf(nbank())
            for k8 in range(4 if not own else 8):
                src = qkb[:, 1, (k8 % 4) * 128:(k8 % 4 + 1) * 128] if k8 < 4 else qkb[:, 0, (k8 - 4) * 128:(k8 - 3) * 128]
                tpose(bt[:, k8 * 128:(k8 + 1) * 128], src, identB)
            cp(kTa[:, :, tile * 128:(tile + 1) * 128], bt[:, 0:512].v(lambda a: a.rearrange("p (h c) -> p h c", h=4)), eng="act")
            if own:
                cp(qTs[:, :, tile * 128:(tile + 1) * 128], bt[:, 512:1024].v(lambda a: a.rearrange("p (h c) -> p h c", h=4)), eng="act")

        stA(0)
        for tile in range(36):
            if tile + 1 < 36:
                stA(tile + 1)
            stB(tile)
        scope_end()
        qTm = [newT([128, 512], BF16, name=f"qTm{m}") for m in range(2)]
        PTs = [newT([128, 512], BF16, name=f"PTss{i}") for i in range(4)]
        Osb = [newT([128, 4, 129], name=f"Osb{m}") for m in range(2)]
        o_tok = newT([128, 4, 4, 128], F32, "o_tok_s")
        rr = newT([128, 2]); sq = newT([128, 4, 128]); ss4 = newT([128, 4]); on = newT([128, 4, 128], BF16)
        obank = [TT(banks[i][:, :], bbuf[i]) for i in range(4)]
        _sbk = [0]
        def sbank():
            _sbk[0] = (_sbk[0] + 1) % 4
            i = 4 + _sbk[0]
            return TT(banks[i][:, :], bbuf[i])
        _pt = [0]
        for qb in range(4):
            for hd in range(4):
                for m in range(2):
                    cp(qTm[m], qTs[:, hd, qb * 512:(qb + 1) * 512], eng=("act" if m else "dve"))
                    z = H1 if m == 0 else H0
                    memset(qTm[m][z], 0.0)
                    sbks = {}
                    Pbuf = {}
                    for it in range(36 + 3):
                        if it < 36:
                            kt = it
                            sbks[kt] = sbank()
                            mmg(sbks[kt], [(kTa[:, hd, kt * 128:(kt + 1) * 128], qTm[m])])
                        if 0 <= it - 2 < 36:
                            kt = it - 2
                            _pt[0] = (_pt[0] + 1) % 4
                            Pbuf[kt] = PTs[_pt[0]]
                            act(Pbuf[kt], sbks[kt], AF.Exp, scale=0.125)
                        if 0 <= it - 3 < 36:
                            kt = it - 3
                            P_ = Pbuf[kt]
                            for qt in range(4):
                                S.op("pe", lambda e, qt=qt, P_=P_, kt=kt: e.matmul(
                                    obank[qt].ap[:, 0:129], lhsT=P_.ap[:, qt * 128:(qt + 1) * 128],
                                    rhs=Vs.ap[:, kt, hd, :], start=(kt == 0), stop=(kt == 35)),
                                    reads=[P_.buf, Vs.buf], writes=[obank[qt].buf])
                    for qt in range(4):
                        cp(Osb[m][:, qt, :], obank[qt][:, 0:129], eng=("act" if qt % 2 else "dve"))
                for qt in range(4):
                    recip(rr[:, 0:1], Osb[0][:, qt, 128:129]); recip(rr[:, 1:2], Osb[1][:, qt, 128:129])
                    tt(rr[:, 1:2], rr[:, 1:2], lamneg, ALU.mult)
                    ts(o_tok[:, qt, hd, :], Osb[0][:, qt, 0:128], rr[:, 0:1], None, ALU.mult)
                    stt(o_tok[:, qt, hd, :], Osb[1][:, qt, 0:128], rr[:, 1:2], o_tok[:, qt, hd, :], ALU.mult, ALU.add)
            for qt in range(4):
                tok0 = qb * 512 + qt * 128
                ot = o_tok[:, qt, :, :]
                tt(sq, ot, ot, ALU.mult)
                S.op("dve", lambda e: e.tensor_reduce(out=ss4.ap, in_=sq.ap, axis=AX.X, op=ALU.add),
                     reads=[sq.buf], writes=[ss4.buf])
                act(ss4, ss4, AF.Sqrt, scale=1.0 / 128, bias=epsT[:, 0:1])
                recip(ss4, ss4)
                tt(sq, ot, ss4.v(lambda a: a.unsqueeze(2).to_broadcast([128, 4, 128])), ALU.mult)
                tt(on, sq, subw.v(lambda a: a.unsqueeze(1).to_broadcast([128, 4, 128])), ALU.mult)
                bk = TT(banks[4 + qt][:, :].bitcast(BF16), bbuf[4 + qt])
                for hd in range(4):
                    tpose(bk[:, hd * 128:(hd + 1) * 128], on[:, hd, :], identB)
                cp(catS[:, 4:8, tok0:tok0 + 128], bk[:, 0:512].v(lambda a: a.rearrange("p (h c) -> p h c", h=4)), eng="act")
        scope_end()

        scope_begin()
        alloc_gen()
        wout_sb = newT([128, KC, D], BF16, "wout_sb2")
        for kc in range(KC):
            S.dma("pool", wout_sb.ap[:, kc, :], wout_v[:, kc, :], writes=[wout_sb.buf], stream="w")
        x1t = [newT([128, D]) for i in range(2)]
        wtmp = newT([128, 512])
        for t in range(16):
            tok0 = t * 128
            i = t % 2
            S.dma("sp", gen.xt[i].ap, xs_all[tok0:tok0 + 128, :], writes=[gen.xt[i].buf], stream="x")
            for cb in range(2):
                csl = slice(cb * 512, (cb + 1) * 512)
                bk = nbank()
                mmg(bk, [(catS[:, kc, tok0:tok0 + 128], wout_sb[:, kc, csl]) for kc in range(KC)])
                tt(wtmp, bk, gateT[:, 1, 0, csl], ALU.mult)
                tt(x1t[i][:, csl], wtmp, gen.xt[i][:, csl], ALU.add)
            S.dma("sp", x1_d[NP_TOK + tok0:NP_TOK + tok0 + 128, :], x1t[i].ap, reads=[x1t[i].buf], stream="o")
        S.dead = False
        scope_end()
        scope_end()

        NT = NT_MOE
        scope_begin()
        h2T = newT([128, KC, NT * 128], BF16, "h2T")
        acc = newT([128, NT, D], F32, "acc")
        gates = newT([128, NT, 65], F32, "gates"); memset(gates, 1.0)
        _xt1 = newT([128, D])
        gen.xt = [_xt1, _xt1]
        _xs1 = newT([128, D], BF16)
        gen.xs = [_xs1, _xs1]
        gen.junk = newT([128, D], BF16)
        gen.ssq = [newT([128, 1]) for i in range(2)]
        gen.rstd = [newT([128, 1]) for i in range(2)]
        wr_sb = newT([128, KC, 64], BF16, "wr_sb")
        S.dma("pool", wr_sb.ap, w_router_d.rearrange("(kc p) n -> p kc n", p=128), writes=[wr_sb.buf], stream="w")
        rbias = load(rbias_bc, [128, 64])
        sco = newT([128, 64]); bia = newT([128, 64]); eq = newT([128, 64]); mk1 = newT([128, 64])
        gm1 = newT([128, 8]); gm2 = newT([128, 8]); gsc = newT([128, 8]); top8 = newT([128, 8])
        gmask = newT([128, 8]); pen = newT([128, 8]); sel = newT([128, 64]); den = newT([128, 1])
        def g88(x):
            return x.v(lambda a: a.rearrange("p (g e) -> p g e", e=8))
        def b88(x):
            return x.v(lambda a: a.unsqueeze(2).to_broadcast([128, 8, 8]))
        def prologue(t):
            cond = 0 if t < 8 else 1
            i = t % 2
            S.dma("sp", gen.xt[i].ap, x1_d[t * 128:(t + 1) * 128, :], writes=[gen.xt[i].buf], stream="x")
            norm_tile(gen.xt[i], h2T[:, :, t * 128:(t + 1) * 128], sc2T[:, :, cond], modTT[:, 2, :, cond])
            bk = nbank()
            mmg(bk[:, 0:64], [(h2T[:, kc, t * 128:(t + 1) * 128], wr_sb[:, kc, :]) for kc in range(KC)])
            act(sco, bk[:, 0:64], AF.Sigmoid)
            tt(bia, sco, rbias, ALU.add)
            S.op("dve", lambda e: e.tensor_reduce(out=gm1.ap, in_=g88(bia).ap, axis=AX.X, op=ALU.max),
                 reads=[bia.buf], writes=[gm1.buf])
            tt(g88(eq), g88(bia), b88(gm1), ALU.is_equal)
            stt(mk1, eq, -1e9, bia, ALU.mult, ALU.add)
            S.op("dve", lambda e: e.tensor_reduce(out=gm2.ap, in_=g88(mk1).ap, axis=AX.X, op=ALU.max),
                 reads=[mk1.buf], writes=[gm2.buf])
            tt(gsc, gm1, gm2, ALU.add)
            S.op("dve", lambda e: e.max(out=top8.ap, in_=gsc.ap), reads=[gsc.buf], writes=[top8.buf])
            ts(gmask, gsc, top8[:, 3:4], None, ALU.is_ge)
            ts(pen, gmask, -1.0, 1e9, ALU.add, ALU.mult)
            tt(g88(mk1), g88(bia), b88(gmask), ALU.mult)
            tt(g88(mk1), g88(mk1), b88(pen), ALU.add)
            S.op("dve", lambda e: e.max(out=top8.ap, in_=mk1.ap), reads=[mk1.buf], writes=[top8.buf])
            ts(sel, mk1, top8[:, 7:8], None, ALU.is_ge)
            tt(sel, sel, sco, ALU.mult)
            S.op("dve", lambda e: e.tensor_reduce(out=den.ap, in_=sel.ap, axis=AX.X, op=ALU.add),
                 reads=[sel.buf], writes=[den.buf])
            recip(den, den)
            ts(gates[:, t, 0:64], sel, den[:, 0:1], 2.5, ALU.mult, ALU.mult)
        scope_begin()
        wgu = [newT([128, KC, 512], BF16, f"wgu{i}") for i in range(2)]
        wd = [newT([128, 2, D], BF16, f"wd{i}") for i in range(2)]
        sgl = [newT([128, 512], BF16, name=f"sgl{i}") for i in range(2)]
        actT = [newT([128, 512], BF16, name=f"actT{i}") for i in range(2)]
        NE = NE_MOE
        NB_ = NT // 2
        items = [(e_, blk) for e_ in range(NE) for blk in range(NB_)]

        def load_expert(e_):
            wb = e_ % 2
            S.dma("pool", wgu[wb].ap[:, :, 0:256], weg[e_].rearrange("(kc p) f -> p kc f", p=128),
                  writes=[wgu[wb].buf], stream="w")
            S.dma("pool", wgu[wb].ap[:, :, 256:512], weu[e_].rearrange("(kc p) f -> p kc f", p=128),
                  writes=[wgu[wb].buf], stream="w")
            S.dma("pool", wd[wb].ap, wed[e_].rearrange("(c p) f -> p c f", p=128),
                  writes=[wd[wb].buf], stream="w")

        ugb = {}

        def UG(idx):
            e_, blk = items[idx]
            wb = e_ % 2
            tsl = slice(blk * 256, (blk + 1) * 256)
            bA, bB = nbank(), nbank()
            for ffc in range(2):
                mmg(bA[:, ffc * 256:(ffc + 1) * 256],
                    [(wgu[wb][:, kc, ffc * 128:(ffc + 1) * 128], h2T[:, kc, tsl]) for kc in range(KC)])
            for ffc in range(2):
                mmg(bB[:, ffc * 256:(ffc + 1) * 256],
                    [(wgu[wb][:, kc, 256 + ffc * 128:256 + (ffc + 1) * 128], h2T[:, kc, tsl]) for kc in range(KC)])
            ugb[idx] = (bA, bB)

        def MID(idx):
            bA, bB = ugb.pop(idx)
            i = idx % 2
            act(sgl[i], bA, AF.Silu)
            tt(actT[i], sgl[i], bB, ALU.mult)

        def DOWN(idx):
            e_, blk = items[idx]
            wb = e_ % 2
            i = idx % 2
            for t2 in range(2):
                tile_ = blk * 2 + t2
                for cb in range(2):
                    csl = slice(cb * 512, (cb + 1) * 512)
                    bC = nbank()
                    mmg(bC, [(actT[i][:, ffc * 256 + t2 * 128:ffc * 256 + t2 * 128 + 128], wd[wb][:, ffc, csl])
                             for ffc in range(2)])
                    if e_ == 0:
                        ts(acc[:, tile_, csl], bC, gates[:, tile_, e_:e_ + 1], None, ALU.mult)
                    else:
                        stt(acc[:, tile_, csl], bC, gates[:, tile_, e_:e_ + 1], acc[:, tile_, csl], ALU.mult, ALU.add)

        load_expert(0)
        if NE > 1:
            load_expert(1)
        prologue(0); prologue(1)
        UG(0)
        for idx in range(len(items)):
            e_, blk = items[idx]
            MID(idx)
            if idx + 1 < len(items):
                if items[idx + 1][0] == 0:
                    prologue(2 * items[idx + 1][1]); prologue(2 * items[idx + 1][1] + 1)
                UG(idx + 1)
            DOWN(idx)
            if blk == NB_ - 1 and e_ + 2 < NE:
                load_expert(e_ + 2)
        scope_end()
        fwb = load(fw_bc, [128, D])
        _yt1 = newT([128, D], name="ytile")
        ytile = [_yt1, _yt1]
        for t in range(NT):
            cond = 0 if t < 8 else 1
            i = t % 2
            S.dma("sp", gen.xt[i].ap, x1_d[t * 128:(t + 1) * 128, :], writes=[gen.xt[i].buf], stream="x")
            tt(ytile[i], acc[:, t, :], gateT[:, cond, 1, :], ALU.mult)
            tt(gen.xt[i], gen.xt[i], ytile[i], ALU.add)
            act(gen.junk, gen.xt[i], AF.Square, accum=gen.ssq[i])
            act(gen.rstd[i], gen.ssq[i], AF.Sqrt, scale=1.0 / D, bias=epsT[:, 0:1])
            recip(gen.rstd[i], gen.rstd[i])
            stt(ytile[i], gen.xt[i], gen.rstd[i][:, 0:1], fwb, ALU.mult, ALU.mult)
            S.dma("sp", o_y[t * 128:(t + 1) * 128, :], ytile[i].ap, reads=[ytile[i].buf], stream="o")
        scope_end()
        S.finish()
    return nc


_NC_CACHE = {}
_DBG = {}


def kernel(x_prompt, x_sample, c, cache_k, cache_v, state_ssm_re, state_ssm_im,
           c_ctx, w_ada, b_ada, norm1_w, w_in, ssm_lambda_re, ssm_lambda_im, ssm_log_dt,
           ssm_b_re, ssm_b_im, ssm_c_re, ssm_c_im, ssm_d, ssm_w_glu,
           diff_lambda_q, diff_lambda_k, diff_subln_w, w_out, norm2_w,
           w_router, router_bias, w_exp_gate, w_exp_up, w_exp_down,
           w_sh_gate, w_sh_up, w_sh_down, final_norm_w):
    f32 = np.float32
    A = lambda a: np.ascontiguousarray(np.asarray(a, dtype=f32))
    x_prompt = A(x_prompt); x_sample = A(x_sample); c = A(c); c_ctx = A(c_ctx)

    if "nc" not in _NC_CACHE:
        _NC_CACHE["nc"] = build_program()
    nc = _NC_CACHE["nc"]

    def fm(v):
        return np.ascontiguousarray(np.asarray(v, f32).reshape(KC, 128).T)

    jj = np.arange(128) // 16
    shared = {
        "w_ada": A(w_ada)[0],
        "b_ada_bc": np.ascontiguousarray(np.broadcast_to(A(b_ada)[0][None, :], (128, 6 * D))),
        "n1w": fm(A(norm1_w)[0]),
        "w_in": A(w_in)[0],
        "ident": np.eye(128, dtype=f32),
        "w_out": A(w_out)[0], "w_glu": A(ssm_w_glu)[0],
        "dfold_l": np.ascontiguousarray(np.tile(A(ssm_d)[0].reshape(32, 16).T, (8, 1))),
        "mle_l": (jj[:, None] <= jj[None, :]).astype(f32), "mge_l": (jj[:, None] >= jj[None, :]).astype(f32),
        "n2w": fm(A(norm2_w)[0]), "w_router": A(w_router)[0],
        "rbias_bc": np.ascontiguousarray(np.broadcast_to(A(router_bias)[0][None], (128, 64))),
        "fw_bc": np.ascontiguousarray(np.broadcast_to(A(final_norm_w)[None], (128, D))),
        "weg": np.concatenate([A(w_exp_gate)[0], A(w_sh_gate)], axis=0),
        "weu": np.concatenate([A(w_exp_up)[0], A(w_sh_up)], axis=0),
        "wed": np.concatenate([A(w_exp_down)[0], A(w_sh_down)], axis=0),
        "lq_bc": np.ascontiguousarray(np.broadcast_to(A(diff_lambda_q)[0][None], (128, 2, 64))),
        "lk_bc": np.ascontiguousarray(np.broadcast_to(A(diff_lambda_k)[0][None], (128, 2, 64))),
        "subw_bc": np.ascontiguousarray(np.broadcast_to(A(diff_subln_w)[0][None], (128, 128))),
    }
    def s5l(rev):
        ds = [1, 0] if rev else [0, 1]
        C = np.ascontiguousarray
        o = {}
        o["lam_re_l"] = C(A(ssm_lambda_re)[0][ds].transpose(0, 2, 1).reshape(128, 32))
        o["lam_im_l"] = C(A(ssm_lambda_im)[0][ds].transpose(0, 2, 1).reshape(128, 32))
        o["logdt_l"] = C(np.repeat(A(ssm_log_dt)[0][ds][:, None, :], 64, axis=1).reshape(128, 32))
        o["bre_l"] = C(A(ssm_b_re)[0][ds].transpose(0, 2, 1, 3).reshape(128, 32, 16))
        o["bim_l"] = C(A(ssm_b_im)[0][ds].transpose(0, 2, 1, 3).reshape(128, 32, 16))
        o["cre_l"] = C(A(ssm_c_re)[0][ds].transpose(0, 3, 1, 2).reshape(128, 32, 16))
        o["cim_l"] = C(A(ssm_c_im)[0][ds].transpose(0, 3, 1, 2).reshape(128, 32, 16))
        return o
    s5maps = [s5l(False), s5l(True)]
    in_maps = []
    for i in range(NCORES):
        b, rev = i // 2, (i % 2 == 1)
        xpi = x_prompt[4 * i:4 * i + 4]
        if rev:
            xpi = xpi[:, ::-1]
        cond2 = np.stack([c_ctx, c[b]], axis=0)
        condT = np.ascontiguousarray(cond2.reshape(2, KC, 128).transpose(2, 1, 0))
        m = dict(shared)
        m["xp"] = np.ascontiguousarray(xpi.reshape(NP_TOK, D))
        m["condT"] = condT
        m.update(s5maps[i % 2])
        xsb = x_sample[b]
        idx = np.arange(4096)
        if rev:
            xsb = xsb[::-1]
            idx = idx[::-1]
        m["xs_all"] = np.ascontiguousarray(xsb)
        rc = np.stack([(idx // 64).astype(f32), (idx % 64).astype(f32)], axis=-1)
        m["pos_rc"] = np.ascontiguousarray(rc.reshape(32, 128, 2).transpose(1, 0, 2))
        m["fidx_bc"] = np.ascontiguousarray(np.broadcast_to(np.arange(16, dtype=f32)[None], (128, 16)))
        m["ck_in"] = np.ascontiguousarray(A(cache_k)[b, 0].reshape(512, 512))
        m["cv_in"] = np.ascontiguousarray(A(cache_v)[b, 0].reshape(512, 512))
        ds_ = [1, 0] if rev else [0, 1]
        hr = A(state_ssm_re)[b, 0][ds_].transpose(0, 2, 1).reshape(128, 32)
        hi = A(state_ssm_im)[b, 0][ds_].transpose(0, 2, 1).reshape(128, 32)
        m["h0_l"] = np.ascontiguousarray(np.stack([hr, hi], axis=1))
        in_maps.append(m)

    res = run_bass_kernel_spmd(nc, in_maps, core_ids=list(range(NCORES)))
    R = res.results

    y_prompt = np.zeros((32, 256, D), f32)
    y_sample = np.zeros((4, 4096, D), f32)
    new_ck = np.zeros((32, 1, 256, 4, 128), f32)
    new_cv = np.zeros((32, 1, 256, 4, 128), f32)
    new_re = np.zeros((32, 1, 2, 32, 64), f32)
    new_im = np.zeros((32, 1, 2, 32, 64), f32)
    for i in range(NCORES):
        rev = (i % 2 == 1)
        ck = np.asarray(R[i]["o_ck"]).reshape(4, 256, 4, 128)
        cv = np.asarray(R[i]["o_cv"]).reshape(4, 256, 4, 128)
        if rev:
            ck, cv = ck[:, ::-1], cv[:, ::-1]
        new_ck[4 * i:4 * i + 4, 0] = ck
        new_cv[4 * i:4 * i + 4, 0] = cv
        stt_ = np.asarray(R[i]["o_st"]).reshape(4, 2, 32, 2, 64)
        if rev:
            stt_ = stt_[:, :, :, ::-1]
        new_re[4 * i:4 * i + 4, 0] = stt_[:, 0].transpose(0, 2, 1, 3)
        new_im[4 * i:4 * i + 4, 0] = stt_[:, 1].transpose(0, 2, 1, 3)
        yy = np.asarray(R[i]["o_y"])
        yp = yy[:NP_TOK].reshape(4, 256, D)
        if rev:
            yp = yp[:, ::-1]
        y_prompt[4 * i:4 * i + 4] = yp
        ys = yy[NP_TOK:]
        if rev:
            y_sample[i // 2, 2048:] = ys[::-1]
        else:
            y_sample[i // 2, :2048] = ys
    return (y_prompt, y_sample, new_ck, new_cv, new_re, new_im)
```
